# Optimizing a Trainium2 kernel written in Bass

```python
import math
import jax, jax.numpy as jnp
from jax import lax
import numpy as np

D_MODEL = 2048
BATCH = 16
SEQ = 2048
DEPTH = 1

N_Q_HEADS = 16
N_KV_GROUPS = 4
HEADS_PER_GROUP = N_Q_HEADS // N_KV_GROUPS
HEAD_DIM = 64
ATTN_WIDTH = N_Q_HEADS * HEAD_DIM
KV_WIDTH = N_KV_GROUPS * HEAD_DIM
SCALE = HEAD_DIM ** -0.5
CMP_BLOCK = 32
CMP_STRIDE = 16
CMP_HIDDEN = 256
SEL_BLOCK = 64
N_SELECT = 16
WINDOW = 512
Q_BLOCK = 128
SEL_Q_CHUNK = 32
N_BRANCH = 3
RNN_WIDTH = D_MODEL - ATTN_WIDTH
RNN_BLOCKS = 16
RNN_BLOCK_DIM = RNN_WIDTH // RNN_BLOCKS
RNN_CONV_WIDTH = 4
RG_LRU_C = 8.0
N_BUCKETS = 32
MAX_DISTANCE = 128
D_FF = 5632
FFN_CONV_WIDTH = 3
NORM_EPS = 1e-6
NEG_INF = -1e30
FORCE_BONUS = 1e3
IN_WIDTH = ATTN_WIDTH + 6 * KV_WIDTH + N_BRANCH * N_Q_HEADS + 2 * RNN_WIDTH

kernel_name = "hybrid_nsa_rglru_convffn"


def rms_norm(x, g):
    xf = x.astype(jnp.float32)
    y = xf * lax.rsqrt(jnp.mean(xf * xf, axis=-1, keepdims=True) + NORM_EPS)
    return (y * g.astype(jnp.float32)).astype(x.dtype)


def t5_bucket(dist):
    n = jnp.maximum(dist, 0)
    max_exact = N_BUCKETS // 2
    nf = jnp.maximum(n, 1).astype(jnp.float32)
    large = max_exact + (jnp.log(nf / max_exact) / math.log(MAX_DISTANCE / max_exact)
                         * (N_BUCKETS - max_exact)).astype(jnp.int32)
    large = jnp.minimum(large, N_BUCKETS - 1)
    return jnp.where(n < max_exact, n, large)


def causal_depthwise_conv(x, w, b):
    k = w.shape[0]
    y = lax.conv_general_dilated(x, w[:, None, :].astype(x.dtype), window_strides=(1,),
                                 padding=[(k - 1, 0)], dimension_numbers=('NWC', 'WIO', 'NWC'),
                                 feature_group_count=x.shape[-1])
    return y + b.astype(x.dtype)


def compress_tokens(t, pe, w1, w2):
    B, S, G, _ = t.shape
    n_cmp = (S - CMP_BLOCK) // CMP_STRIDE + 1
    idx = jnp.arange(n_cmp)[:, None] * CMP_STRIDE + jnp.arange(CMP_BLOCK)[None, :]
    blk = t[:, idx] + pe[None, None, :, None, :]
    blk = jnp.swapaxes(blk, 2, 3).reshape(B, n_cmp, G, CMP_BLOCK * HEAD_DIM)
    return jax.nn.gelu(blk @ w1) @ w2


def compressed_attention(q, k_cmp, v_cmp, bias_table):
    B, S, G, R, _ = q.shape
    n_cmp = k_cmp.shape[1]
    t = jnp.arange(S)[:, None]
    blk_end = jnp.arange(n_cmp)[None, :] * CMP_STRIDE + CMP_BLOCK - 1
    dist = t - blk_end
    mask = dist >= 0
    bias = jnp.moveaxis(bias_table[t5_bucket(dist)], -1, 0).reshape(G, R, S, n_cmp)
    s = jnp.einsum('bsgrd,bcgd->bgrsc', q, k_cmp) * SCALE
    logits = jnp.where(mask, s.astype(jnp.float32) + bias.astype(jnp.float32), NEG_INF)
    has_any = jnp.any(mask, axis=-1)[:, None].astype(jnp.float32)
    p = jax.nn.softmax(logits, axis=-1) * has_any
    o = jnp.einsum('bgrsc,bcgd->bsgrd', p.astype(v_cmp.dtype), v_cmp)
    return o, p


def select_blocks(p_cmp):
    S, n_cmp = p_cmp.shape[3], p_cmp.shape[4]
    n_sb = S // SEL_BLOCK
    c = np.arange(n_cmp)[:, None]
    j = np.arange(n_sb)[None, :]
    lo = np.maximum(c * CMP_STRIDE, j * SEL_BLOCK)
    hi = np.minimum(c * CMP_STRIDE + CMP_BLOCK, (j + 1) * SEL_BLOCK)
    overlap = jnp.asarray(np.maximum(hi - lo, 0) / CMP_BLOCK, dtype=jnp.float32)
    imp = jnp.einsum('bgrsc,cj->bgsj', p_cmp, overlap)
    t = jnp.arange(S)[:, None]
    jj = jnp.arange(n_sb)[None, :]
    cur = t // SEL_BLOCK
    valid = jj <= cur
    forced = ((jj == 0) | (jj == cur) | (jj == cur - 1)).astype(jnp.float32)
    score = jnp.where(valid, imp + FORCE_BONUS * forced, -1.0)
    n_sel = min(N_SELECT, n_sb)
    top_val, top_idx = lax.top_k(score, n_sel)
    return top_idx, top_val >= 0.0


def selected_attention(q, k_slc, v_slc, top_idx, top_valid, bias_table):
    B, S, G, R, hd = q.shape
    n_sb = S // SEL_BLOCK
    n_sel = top_idx.shape[-1]
    kb = k_slc.reshape(B, n_sb, SEL_BLOCK, G, hd).transpose(0, 3, 1, 2, 4)
    vb = v_slc.reshape(B, n_sb, SEL_BLOCK, G, hd).transpose(0, 3, 1, 2, 4)
    n_chunks = S // SEL_Q_CHUNK
    qc = jnp.moveaxis(q.reshape(B, n_chunks, SEL_Q_CHUNK, G, R, hd), 1, 0)
    ic = jnp.moveaxis(top_idx.reshape(B, G, n_chunks, SEL_Q_CHUNK, n_sel), 2, 0)
    vc = jnp.moveaxis(top_valid.reshape(B, G, n_chunks, SEL_Q_CHUNK, n_sel), 2, 0)
    tc = jnp.arange(S).reshape(n_chunks, SEL_Q_CHUNK)
    bias_g = bias_table.reshape(N_BUCKETS, G, R).transpose(1, 0, 2)
    bi = jnp.arange(B)[:, None, None, None]
    gi = jnp.arange(G)[None, :, None, None]

    def chunk(args):
        q_c, idx, val, t = args
        C = t.shape[0]
        kg = kb[bi, gi, idx]
        vg = vb[bi, gi, idx]
        tok = idx[..., None] * SEL_BLOCK + jnp.arange(SEL_BLOCK)
        dist = t[None, None, :, None, None] - tok
        mask = (dist >= 0) & val[..., None]
        bias = bias_g[gi[..., None], t5_bucket(dist)]
        s = jnp.einsum('bcgrd,bgcnld->bgrcnl', q_c, kg) * SCALE
        logits = s.astype(jnp.float32) + jnp.moveaxis(bias, -1, 2).astype(jnp.float32)
        logits = jnp.where(mask[:, :, None], logits, NEG_INF)
        p = jax.nn.softmax(logits.reshape(B, G, R, C, n_sel * SEL_BLOCK), axis=-1)
        return jnp.einsum('bgrcm,bgcmd->bcgrd', p.astype(vg.dtype),
                          vg.reshape(B, G, C, n_sel * SEL_BLOCK, hd))

    out = lax.map(chunk, (qc, ic, vc, tc))
    return jnp.moveaxis(out, 0, 1).reshape(B, S, G, R, hd)


def window_attention(q, k_win, v_win, bias_table):
    B, S, G, R, hd = q.shape
    n_qb = S // Q_BLOCK
    span = Q_BLOCK + WINDOW
    kp = jnp.pad(k_win, ((0, 0), (WINDOW, 0), (0, 0), (0, 0)))
    vp = jnp.pad(v_win, ((0, 0), (WINDOW, 0), (0, 0), (0, 0)))
    i = jnp.arange(Q_BLOCK)[:, None]
    j = jnp.arange(span)[None, :]
    rel = WINDOW + i - j
    band = (rel >= 0) & (rel < WINDOW)
    bias = jnp.moveaxis(bias_table[t5_bucket(rel)], -1, 0).reshape(G, R, Q_BLOCK, span)
    qb = jnp.moveaxis(q.reshape(B, n_qb, Q_BLOCK, G, R, hd), 1, 0)

    def block(args):
        q_b, b_idx = args
        start = b_idx * Q_BLOCK
        kw = lax.dynamic_slice_in_dim(kp, start, span, axis=1)
        vw = lax.dynamic_slice_in_dim(vp, start, span, axis=1)
        in_seq = (start + jnp.arange(span)) >= WINDOW
        mask = band & in_seq[None, :]
        s = jnp.einsum('bigrd,bjgd->bgrij', q_b, kw) * SCALE
        logits = jnp.where(mask, s.astype(jnp.float32) + bias.astype(jnp.float32), NEG_INF)
        p = jax.nn.softmax(logits, axis=-1)
        return jnp.einsum('bgrij,bjgd->bigrd', p.astype(vw.dtype), vw)

    out = lax.map(block, (qb, jnp.arange(n_qb)))
    return jnp.moveaxis(out, 0, 1).reshape(B, S, G, R, hd)


def rg_lru(x, w_a, b_a, w_x, b_x, lam):
    B, S, _ = x.shape
    xb = x.reshape(B, S, RNN_BLOCKS, RNN_BLOCK_DIM)
    r = jax.nn.sigmoid(jnp.einsum('bshi,hij->bshj', xb, w_a).reshape(B, S, RNN_WIDTH) + b_a)
    gi = jax.nn.sigmoid(jnp.einsum('bshi,hij->bshj', xb, w_x).reshape(B, S, RNN_WIDTH) + b_x)
    log_a = (-RG_LRU_C * jax.nn.softplus(-lam.astype(jnp.float32))) * r.astype(jnp.float32)
    a = jnp.exp(log_a)
    mult = jnp.sqrt(-jnp.expm1(2.0 * log_a))
    u = mult * (gi * x).astype(jnp.float32)

    def combine(left, right):
        a_l, u_l = left
        a_r, u_r = right
        return a_l * a_r, a_r * u_l + u_r

    _, h = lax.associative_scan(combine, (a, u), axis=1)
    return h.astype(x.dtype)


def setup_inputs(seed: int = 0) -> dict:
    key = jax.random.key(seed)
    ks = jax.random.split(key, 32)
    f32 = jnp.float32
    L = DEPTH

    def nrm(k, shape, scale):
        return jax.random.normal(k, shape, f32) * scale

    def gain(k, shape):
        return 1.0 + 0.02 * jax.random.normal(k, shape, f32)

    u = jax.random.uniform(ks[18], (L, RNN_WIDTH), f32, 0.9, 0.999)
    a = u ** (1.0 / RG_LRU_C)
    lam = jnp.log(a) - jnp.log1p(-a)
    return {
        "x": jax.random.normal(ks[0], (BATCH, SEQ, D_MODEL), f32),
        "mix_norm_g": gain(ks[1], (L, D_MODEL)),
        "w_in": nrm(ks[2], (L, D_MODEL, IN_WIDTH), D_MODEL ** -0.5),
        "b_gate": nrm(ks[3], (L, N_BRANCH * N_Q_HEADS), 0.02),
        "cmp_pe_k": nrm(ks[4], (L, CMP_BLOCK, HEAD_DIM), 0.5),
        "cmp_pe_v": nrm(ks[5], (L, CMP_BLOCK, HEAD_DIM), 0.5),
        "cmp_k_w1": nrm(ks[6], (L, CMP_BLOCK * HEAD_DIM, CMP_HIDDEN), (CMP_BLOCK * HEAD_DIM) ** -0.5),
        "cmp_k_w2": nrm(ks[7], (L, CMP_HIDDEN, HEAD_DIM), CMP_HIDDEN ** -0.5),
        "cmp_v_w1": nrm(ks[8], (L, CMP_BLOCK * HEAD_DIM, CMP_HIDDEN), (CMP_BLOCK * HEAD_DIM) ** -0.5),
        "cmp_v_w2": nrm(ks[9], (L, CMP_HIDDEN, HEAD_DIM), CMP_HIDDEN ** -0.5),
        "rel_bias": nrm(ks[10], (N_BUCKETS, N_Q_HEADS), 0.5),
        "rnn_conv_w": nrm(ks[11], (L, RNN_CONV_WIDTH, RNN_WIDTH), RNN_CONV_WIDTH ** -0.5),
        "rnn_conv_b": nrm(ks[12], (L, RNN_WIDTH), 0.02),
        "rg_a_w": nrm(ks[13], (L, RNN_BLOCKS, RNN_BLOCK_DIM, RNN_BLOCK_DIM), RNN_BLOCK_DIM ** -0.5),
        "rg_a_b": nrm(ks[14], (L, RNN_WIDTH), 0.02),
        "rg_x_w": nrm(ks[15], (L, RNN_BLOCKS, RNN_BLOCK_DIM, RNN_BLOCK_DIM), RNN_BLOCK_DIM ** -0.5),
        "rg_x_b": nrm(ks[16], (L, RNN_WIDTH), 0.02),
        "rg_lambda": lam,
        "attn_out_g": gain(ks[19], (L, ATTN_WIDTH)),
        "rnn_out_g": gain(ks[20], (L, RNN_WIDTH)),
        "w_out": nrm(ks[21], (L, D_MODEL, D_MODEL), D_MODEL ** -0.5),
        "ffn_norm_g": gain(ks[22], (L, D_MODEL)),
        "w_ffn_gate": nrm(ks[23], (L, D_MODEL, D_FF), D_MODEL ** -0.5),
        "w_ffn_up": nrm(ks[24], (L, D_MODEL, D_FF), D_MODEL ** -0.5),
        "ffn_conv_w": nrm(ks[25], (L, FFN_CONV_WIDTH, D_FF), FFN_CONV_WIDTH ** -0.5),
        "ffn_conv_b": nrm(ks[26], (L, D_FF), 0.02),
        "w_ffn_down": nrm(ks[27], (L, D_FF, D_MODEL), D_FF ** -0.5),
        "final_norm_g": gain(ks[28], (D_MODEL,)),
    }


def reference(x, mix_norm_g, w_in, b_gate, cmp_pe_k, cmp_pe_v, cmp_k_w1, cmp_k_w2, cmp_v_w1, cmp_v_w2,
              rel_bias, rnn_conv_w, rnn_conv_b, rg_a_w, rg_a_b, rg_x_w, rg_x_b, rg_lambda,
              attn_out_g, rnn_out_g, w_out, ffn_norm_g, w_ffn_gate, w_ffn_up, ffn_conv_w, ffn_conv_b,
              w_ffn_down, final_norm_g):
    B, S, _ = x.shape
    G, R, hd = N_KV_GROUPS, HEADS_PER_GROUP, HEAD_DIM
    sizes = [ATTN_WIDTH] + [KV_WIDTH] * 6 + [N_BRANCH * N_Q_HEADS, RNN_WIDTH, RNN_WIDTH]
    offsets = np.cumsum(sizes)[:-1].tolist()
    h = x
    for layer in range(DEPTH):
        y = rms_norm(h, mix_norm_g[layer])
        proj = y @ w_in[layer]
        q, kc, vc, ks_, vs_, kw, vw, gate, rx, ry = jnp.split(proj, offsets, axis=-1)
        q = q.reshape(B, S, G, R, hd)
        kc = kc.reshape(B, S, G, hd)
        vc = vc.reshape(B, S, G, hd)
        ks_ = ks_.reshape(B, S, G, hd)
        vs_ = vs_.reshape(B, S, G, hd)
        kw = kw.reshape(B, S, G, hd)
        vw = vw.reshape(B, S, G, hd)
        k_cmp = compress_tokens(kc, cmp_pe_k[layer], cmp_k_w1[layer], cmp_k_w2[layer])
        v_cmp = compress_tokens(vc, cmp_pe_v[layer], cmp_v_w1[layer], cmp_v_w2[layer])
        o_cmp, p_cmp = compressed_attention(q, k_cmp, v_cmp, rel_bias)
        top_idx, top_valid = select_blocks(p_cmp)
        o_slc = selected_attention(q, ks_, vs_, top_idx, top_valid, rel_bias)
        o_win = window_attention(q, kw, vw, rel_bias)
        g = jax.nn.sigmoid(gate + b_gate[layer]).reshape(B, S, G, R, N_BRANCH)
        o_attn = (g[..., 0:1] * o_cmp + g[..., 1:2] * o_slc + g[..., 2:3] * o_win).reshape(B, S, ATTN_WIDTH)
        xr = causal_depthwise_conv(rx, rnn_conv_w[layer], rnn_conv_b[layer])
        hr = rg_lru(xr, rg_a_w[layer], rg_a_b[layer], rg_x_w[layer], rg_x_b[layer], rg_lambda[layer])
        o_rnn = hr * jax.nn.gelu(ry)
        mixed = jnp.concatenate([rms_norm(o_attn, attn_out_g[layer]),
                                 rms_norm(o_rnn, rnn_out_g[layer])], axis=-1)
        h = h + mixed @ w_out[layer]
        y = rms_norm(h, ffn_norm_g[layer])
        u = jax.nn.gelu(causal_depthwise_conv(y @ w_ffn_gate[layer], ffn_conv_w[layer], ffn_conv_b[layer]))
        h = h + (u * (y @ w_ffn_up[layer])) @ w_ffn_down[layer]
    return rms_norm(h, final_norm_g)
```

```python
import math
from contextlib import ExitStack

import numpy as np
import ml_dtypes

import concourse.bass as bass
import concourse.mybir as mybir
from concourse.bass_utils import run_bass_kernel_spmd

F32 = mybir.dt.float32
BF16 = mybir.dt.bfloat16
AF = mybir.ActivationFunctionType
ALU = mybir.AluOpType

N_CORES = 8
D = 2048
S = 2048
NSEQ = 2
NH = 16
NG = 4
HD = 64
INW = 4656
DFF = 5632
NFC = DFF // 128
NCMP = 127
EPS = 1e-6
NEG = -30000.0


class Buf:
    __slots__ = ("name", "w", "r")

    def __init__(self, name=""):
        self.name = name
        self.w = {}
        self.r = {}


def bufs(n, name=""):
    return [Buf(f"{name}{i}") for i in range(n)]


class MK:
    ENG = ("pe", "act", "dve", "pool", "sp")

    def __init__(self, nc, es):
        self.nc = nc
        self.q = {e: [] for e in self.ENG}
        self.semh = {}
        self.prog = {}
        for e in ("pe", "act", "dve", "pool"):
            h = es.enter_context(nc.semaphore("prog_" + e))
            self.prog[e] = [h, 0]
            self.semh[("p", e)] = h
        self.seen = {e: {} for e in self.ENG}
        self.dsem = {}
        for qn, n in (("sp", 10), ("pool", 6), ("act", 2)):
            lst = []
            for i in range(n):
                h = es.enter_context(nc.semaphore(f"d_{qn}{i}"))
                lst.append([h, 0])
                self.semh[("d", qn, i)] = h
            self.dsem[qn] = lst
        self.drr = {qn: 0 for qn in self.dsem}
        self.ninstr = {e: 0 for e in self.ENG}

    def _wait(self, eng, key, v):
        if eng == "pe" and key == ("p", "pe"):
            return
        seen = self.seen[eng]
        if seen.get(key, 0) < v:
            seen[key] = v
            h = self.semh[key]
            self.q[eng].append(lambda e, h=h, v=v: e.wait_ge(h, v))
            self.ninstr[eng] += 1

    def _waits(self, eng, reads, writes):
        need = {}
        for b in reads:
            for k, v in b.w.items():
                if need.get(k, 0) < v:
                    need[k] = v
        for b in writes:
            for k, v in b.w.items():
                if need.get(k, 0) < v:
                    need[k] = v
            for k, v in b.r.items():
                if need.get(k, 0) < v:
                    need[k] = v
        for k, v in need.items():
            self._wait(eng, k, v)

    def op(self, eng, fn, reads=(), writes=()):
        self._waits(eng, reads, writes)
        p = self.prog[eng]
        p[1] += 1
        v = p[1]
        key = ("p", eng)
        h = p[0]
        self.q[eng].append(lambda e, fn=fn, h=h: fn(e).then_inc(h, 1))
        self.ninstr[eng] += 1
        for b in reads:
            if b.r.get(key, 0) < v:
                b.r[key] = v
        for b in writes:
            b.w = {key: v}
            b.r = {}

    def dma(self, qn, out, in_, reads=(), writes=()):
        self._waits(qn, reads, writes)
        lst = self.dsem[qn]
        i = self.drr[qn]
        self.drr[qn] = (i + 1) % len(lst)
        s = lst[i]
        key = ("d", qn, i)
        if s[1] > 0:
            self._wait(qn, key, s[1])
        s[1] += 16
        v = s[1]
        h = s[0]
        self.q[qn].append(lambda e, h=h, out=out, in_=in_: e.dma_start(out=out, in_=in_).then_inc(h, 16))
        self.ninstr[qn] += 1
        for b in reads:
            if b.r.get(key, 0) < v:
                b.r[key] = v
        for b in writes:
            b.w = {key: v}
            b.r = {}

    def barrier(self):
        for eng in self.ENG:
            for e2, p in self.prog.items():
                if p[1] > 0:
                    if eng == e2 and eng == "pe":
                        continue
                    seen = self.seen[eng]
                    key = ("p", e2)
                    if seen.get(key, 0) < p[1]:
                        seen[key] = p[1]
                        self.q[eng].append(lambda e, h=p[0], v=p[1]: e.wait_ge(h, v))
            for qn, lst in self.dsem.items():
                for i, s in enumerate(lst):
                    if s[1] > 0:
                        key = ("d", qn, i)
                        seen = self.seen[eng]
                        if seen.get(key, 0) < s[1]:
                            seen[key] = s[1]
                            self.q[eng].append(lambda e, h=s[0], v=s[1]: e.wait_ge(h, v))

    def flush(self, final=False):
        nc = self.nc
        if final:
            for qn, lst in self.dsem.items():
                for i, s in enumerate(lst):
                    if s[1] > 0:
                        self._wait(qn, ("d", qn, i), s[1])
        q = self.q
        with nc.Block() as block:
            @block.tensor
            def _(e):
                for f in q["pe"]:
                    f(e)

            @block.scalar
            def _(e):
                for f in q["act"]:
                    f(e)

            @block.vector
            def _(e):
                for f in q["dve"]:
                    f(e)

            @block.gpsimd
            def _(e):
                for f in q["pool"]:
                    f(e)

            @block.sync
            def _(e):
                for f in q["sp"]:
                    f(e)
        self.q = {e: [] for e in self.ENG}


def t5_bucket_np(dist):
    n = np.maximum(dist, 0)
    max_exact = 16
    nf = np.maximum(n, 1).astype(np.float32)
    large = max_exact + (np.log(nf / np.float32(max_exact)) / np.float32(math.log(128 / max_exact))
                         * np.float32(32 - max_exact)).astype(np.int32)
    large = np.minimum(large, 31)
    return np.where(n < max_exact, n, large)


GW = 384


def host_constants():
    c = {}
    c["ident_bf"] = np.eye(128, dtype=np.float32).astype(ml_dtypes.bfloat16)
    c["ones_bf"] = np.ones((128, 128), dtype=np.float32).astype(ml_dtypes.bfloat16)
    n = np.arange(GW)
    bk = t5_bucket_np(n - 127)
    oh = np.zeros((32, GW), np.float32)
    oh[bk, n] = 1.0
    oh[31, :] -= 1.0
    c["oh"] = oh
    j = np.arange(128)[:, None]
    i = np.arange(128)[None, :]
    c["mask_d0"] = (i >= j).astype(np.float32)
    c["mask_d4"] = (j > i).astype(np.float32)
    e = np.zeros((32, S), np.float32)
    e[np.arange(S) // 64, np.arange(S)] = 1.0
    c["e_rows"] = e.astype(ml_dtypes.bfloat16)
    cc = np.arange(NCMP)[:, None]
    jj = np.arange(32)[None, :]
    lo = np.maximum(cc * 16, jj * 64)
    hi = np.minimum(cc * 16 + 32, (jj + 1) * 64)
    c["overlap"] = (np.maximum(hi - lo, 0) / 32.0).astype(np.float32)
    t = np.arange(S)[:, None]
    jb = np.arange(32)[None, :]
    cur = t // 64
    valid = jb <= cur
    forced = ((jb == 0) | (jb == cur) | (jb == cur - 1))
    val = valid.astype(np.float32)
    add = np.where(valid, 1000.0 * forced, -1.0).astype(np.float32)
    c["sel_val"] = np.ascontiguousarray(val.reshape(16, 128, 32).transpose(1, 0, 2))
    c["sel_add"] = np.ascontiguousarray(add.reshape(16, 128, 32).transpose(1, 0, 2))
    zc = np.zeros((16, 272), np.float32)
    zc[np.arange(16), np.arange(16) + 128] = 1.0
    c["zc"] = zc.astype(ml_dtypes.bfloat16)
    k = np.arange(16)[:, None]
    dist = i - 16 * (k - 8) - 31
    c["cmp_valid"] = (dist >= 0).astype(np.float32)
    oh2 = np.zeros((32, 16 * 128), np.float32)
    bk2 = t5_bucket_np(dist.reshape(-1))
    oh2[bk2, np.arange(16 * 128)] = 1.0
    oh2[31, :] -= 1.0
    c["oh_cmp"] = oh2
    return c


def dram_in(nc, name, shape, dt=F32):
    return nc.dram_tensor(name, list(shape), dt, kind="ExternalInput").ap()


class Prog:
    pass


def build(phases=("p0", "p1", "p2", "p3", "p4"), debug=False, nseq=NSEQ):
    nc = bass.Bass("TRN2", target_bir_lowering=False)
    P = Prog()
    P.nc = nc
    P.nseq = nseq
    kind_scr = "ExternalOutput" if debug else "Internal"

    def scr(name, shape, dt):
        return nc.dram_tensor(name, list(shape), dt, kind=kind_scr).ap()

    I = {}
    I["xT"] = dram_in(nc, "xT", [NSEQ, D, S])
    I["w_in"] = dram_in(nc, "w_in", [D, INW])
    I["w_out"] = dram_in(nc, "w_out", [D, D])
    I["w_g"] = dram_in(nc, "w_g", [D, DFF])
    I["w_u"] = dram_in(nc, "w_u", [D, DFF])
    I["w_d"] = dram_in(nc, "w_d", [DFF, D])
    I["g_mix"] = dram_in(nc, "g_mix", [128, 16])
    I["g_ffn"] = dram_in(nc, "g_ffn", [128, 16])
    I["g_fin"] = dram_in(nc, "g_fin", [128, 16])
    I["b_gate_bc"] = dram_in(nc, "b_gate_bc", [128, 48])
    I["g_attn_bc"] = dram_in(nc, "g_attn_bc", [128, 1024])
    I["rnn_vec"] = dram_in(nc, "rnn_vec", [128, 8, 10])
    I["rg_a_w"] = dram_in(nc, "rg_a_w", [16, 64, 64])
    I["rg_x_w"] = dram_in(nc, "rg_x_w", [16, 64, 64])
    I["ffn_vec"] = dram_in(nc, "ffn_vec", [128, NFC, 4])
    I["rel_bias"] = dram_in(nc, "rel_bias", [32, 16])
    I["cmp_w1"] = dram_in(nc, "cmp_w1", [2, 64, 32, 256])
    I["cmp_w2"] = dram_in(nc, "cmp_w2", [2, 128, 2, 64])
    I["cmp_peT"] = dram_in(nc, "cmp_peT", [2, 64, 32])
    I["ident_bf"] = dram_in(nc, "ident_bf", [128, 128], BF16)
    I["ones_bf"] = dram_in(nc, "ones_bf", [128, 128], BF16)
    I["oh"] = dram_in(nc, "oh", [32, GW])
    I["mask_d0"] = dram_in(nc, "mask_d0", [128, 128])
    I["mask_d4"] = dram_in(nc, "mask_d4", [128, 128])
    I["e_rows"] = dram_in(nc, "e_rows", [32, S], BF16)
    I["overlap"] = dram_in(nc, "overlap", [NCMP, 32])
    I["sel_val"] = dram_in(nc, "sel_val", [128, 16, 32])
    I["sel_add"] = dram_in(nc, "sel_add", [128, 16, 32])
    I["zc"] = dram_in(nc, "zc", [16, 272], BF16)
    I["cmp_valid"] = dram_in(nc, "cmp_valid", [16, 128])
    P.I = I

    outT = nc.dram_tensor("outT", [NSEQ, D, S], F32, kind="ExternalOutput").ap()
    P.outT = outT

    W = {}
    W["w_in"] = scr("w_in_b", [D, INW], BF16)
    W["w_out"] = scr("w_out_b", [D, D], BF16)
    W["w_g"] = scr("w_g_b", [D, DFF], BF16)
    W["w_u"] = scr("w_u_b", [D, DFF], BF16)
    W["w_d"] = scr("w_d_b", [DFF, D], BF16)
    P.W = W
    X = {}
    X["QT"] = scr("QT", [NSEQ, 1024, S], BF16)
    X["KC"] = scr("KC", [NSEQ, 512, S], BF16)
    X["KS"] = scr("KS", [NSEQ, 256, S], BF16)
    X["KW"] = scr("KW", [NSEQ, 256, S], BF16)
    X["RX"] = scr("RX", [NSEQ, 1024, S], F32)
    X["RY"] = scr("RY", [NSEQ, 1024, S], F32)
    X["VSW"] = scr("VSW", [NSEQ, 16, 128, 2, 4, 65], BF16)
    X["GATE"] = scr("GATE", [NSEQ, 16, 128, 48], F32)
    X["MIXR"] = scr("MIXR", [NSEQ, 1024, S], BF16)
    X["RSTDR"] = scr("RSTDR", [NSEQ, 128, S], F32)
    X["OATT"] = scr("OATT", [NSEQ, 16, 128, 1024], F32)
    X["MIXA"] = scr("MIXA", [NSEQ, 1024, S], BF16)
    X["RTAB"] = scr("RTAB", [16, 128, GW], F32)
    if debug:
        X["DBGDN"] = scr("DBGDN", [128, 16, 2, 128], BF16)
        X["DBGMB"] = scr("DBGMB", [16, 16, 128], BF16)
    P.X = X

    es = ExitStack()
    with es:
        mk = MK(nc, es)
        P.mk = mk
        P.wb = {k: Buf("wb_" + k) for k in W}
        P.xb = {k: [Buf(f"{k}{s}") for s in range(NSEQ)] for k in X}

        cst = ExitStack()
        with cst:
            C = {}
            C["ident"] = cst.enter_context(nc.sbuf_tensor("c_ident", [128, 128], BF16))
            C["ones"] = cst.enter_context(nc.sbuf_tensor("c_ones", [128, 128], BF16))
            P.C = C
            P.cb = Buf("consts")
            mk.dma("sp", C["ident"][:], I["ident_bf"][:, :], writes=[P.cb])
            mk.dma("sp", C["ones"][:], I["ones_bf"][:, :], writes=[P.cb])
            P.cb.w = dict(P.cb.w)

            if "p0" in phases:
                phase0(P)
            if "p1" in phases:
                for s in range(nseq):
                    phase1(P, s)
            if "p2" in phases:
                for s in range(nseq):
                    phase2(P, s)
            if "p3" in phases:
                phase3_setup(P)
                for s in range(nseq):
                    phase3(P, s)
            if "p4" in phases:
                phase4(P, nseq)
            mk.flush(final=True)
    return nc


def phase0(P):
    mk, I, W = P.mk, P.I, P.W
    for name, rows, cols in (("w_in", D, INW), ("w_out", D, D), ("w_g", D, DFF), ("w_u", D, DFF), ("w_d", DFF, D)):
        ncp = -(-cols // 2048)
        while cols % ncp:
            ncp += 1
        cw = cols // ncp
        rb = 512
        evs = {}
        for r0 in range(0, rows, rb):
            for cp in range(ncp):
                b = Buf()
                mk.dma("pool", W[name][r0:r0 + rb, cp * cw:(cp + 1) * cw], I[name][r0:r0 + rb, cp * cw:(cp + 1) * cw],
                       writes=[b])
                for k, v in b.w.items():
                    evs[k] = max(evs.get(k, 0), v)
        P.wb[name].w = evs
    mk.flush()


def act_copy(out, in_, scale=1.0):
    return lambda e: e.activation(out=out, in_=in_, func=AF.Copy, scale=float(scale))


def dve_scale(out, in_, scale=1.0):
    return lambda e: e.tensor_scalar(out=out, in0=in_, scalar1=float(scale), scalar2=None, op0=ALU.mult)


def evac(mk, idx, out, in_, reads, writes, scale=1.0):
    if idx % 2 == 0:
        mk.op("act", act_copy(out, in_, scale), reads=reads, writes=writes)
    else:
        mk.op("dve", dve_scale(out, in_, scale), reads=reads, writes=writes)


def rms_rstd(mk, ps_ap, ps_buf, rt_ap, rt_buf, rstd_ap, rstd_buf, n):
    mk.op("act", lambda e: e.activation(out=rt_ap, in_=ps_ap, func=AF.Sqrt, scale=1.0 / n, bias=P_EPS[0]),
          reads=[ps_buf, P_EPS[1]], writes=[rt_buf])
    mk.op("dve", lambda e: e.reciprocal(out=rstd_ap, in_=rt_ap), reads=[rt_buf], writes=[rstd_buf])


P_EPS = [None, None]


def phase1(P, s):
    nc, mk, I, W, X, C = P.nc, P.mk, P.I, P.W, P.X, P.C
    mk.barrier()
    with ExitStack() as ts:
        def sb(name, shape, dt):
            return ts.enter_context(nc.sbuf_tensor(f"p1_{s}_{name}", shape, dt))

        yT = sb("yT", [128, 16, S], BF16)
        xin = [sb(f"xin{i}", [128, 16, 256], F32) for i in range(2)]
        sq = [sb(f"sq{i}", [128, 16, 256], BF16) for i in range(2)]
        rt = sb("rt", [128, 256], F32)
        rstd = sb("rstd", [128, 256], F32)
        gmix = sb("gmix", [128, 16], F32)
        epst = sb("eps", [128, 1], F32)
        WB = [sb(f"WB{i}", [128, 16, 512], BF16) for i in range(2)]
        Wtok = sb("Wtok", [128, 16, 560], BF16)
        stb = [sb(f"stb{i}", [128, S], BF16) for i in range(2)]
        stf = [sb(f"stf{i}", [128, S], F32) for i in range(2)]
        Vst = [sb(f"Vst{i}", [128, 2, 4, 65], BF16) for i in range(2)]
        gtmp = [sb(f"gtmp{i}", [128, 48], F32) for i in range(2)]
        gst = [sb(f"gst{i}", [128, 48], F32) for i in range(2)]
        bgate = sb("bgate", [128, 48], F32)
        ps = [ts.enter_context(nc.psum_tensor(f"p1_{s}_ps{i}", [128, 512], F32)) for i in range(8)]
        psb = bufs(8, "ps")

        b_small = Buf("small")
        mk.dma("sp", gmix[:], I["g_mix"][:, :], writes=[b_small])
        b_bg = Buf("bgate")
        mk.dma("sp", bgate[:], I["b_gate_bc"][:, :], writes=[b_bg])
        b_eps = Buf("eps")
        mk.op("dve", lambda e: e.memset(epst[:], EPS), writes=[b_eps])
        P_EPS[0] = epst[:]
        P_EPS[1] = b_eps
        b_vst = bufs(2, "vst")
        for i in range(2):
            mk.op("dve", lambda e, i=i: e.memset(Vst[i][:], 1.0), writes=[b_vst[i]])

        b_xin = bufs(2, "xin")
        b_sq = bufs(2, "sq")
        b_rt = Buf("rt")
        b_rstd = Buf("rstd")
        b_yT = bufs(8, "yT")
        xsrc = I["xT"][s].rearrange("(c p) t -> p c t", p=128)
        pcnt = 0
        for j in range(8):
            k = j % 2
            t0 = j * 256
            mk.dma("sp", xin[k][:], xsrc[:, :, t0:t0 + 256], writes=[b_xin[k]])
            mk.op("act", lambda e, k=k: e.activation(out=sq[k][:], in_=xin[k][:], func=AF.Square),
                  reads=[b_xin[k]], writes=[b_sq[k]])
            pb = 6 + (j % 2)
            for c in range(16):
                mk.op("pe", lambda e, k=k, c=c, pb=pb: e.matmul(ps[pb][:, 0:256], lhsT=C["ones"][:], rhs=sq[k][:, c, :],
                                                                 start=(c == 0), stop=(c == 15)),
                      reads=[b_sq[k], P.cb], writes=[psb[pb]])
            rms_rstd(mk, ps[pb][:, 0:256], psb[pb], rt[:], b_rt, rstd[:], b_rstd, float(D))
            for c in range(16):
                mk.op("dve", lambda e, k=k, c=c, t0=t0: e.scalar_tensor_tensor(
                    out=yT[:, c, t0:t0 + 256], in0=xin[k][:, c, :], scalar=gmix[:, c:c + 1], in1=rstd[:],
                    op0=ALU.mult, op1=ALU.mult), reads=[b_xin[k], b_rstd, b_small], writes=[b_yT[j]])

        wcols = [[(0, 512)], [(512, 1024)], [(1024, 1536)], [(1536, 1792), (2048, 2304)],
                 [(2608, 3120)], [(3120, 3632)], [(3632, 4144)], [(4144, 4656)]]
        b_WB = bufs(2, "WB")
        wsrc = W["w_in"].rearrange("(c p) n -> p c n", p=128)

        def load_w(w):
            k = w % 2
            o = 0
            for (c0, c1) in wcols[w]:
                mk.dma("sp", WB[k][:, :, o:o + (c1 - c0)], wsrc[:, :, c0:c1], reads=[P.wb["w_in"]], writes=[b_WB[k]])
                o += c1 - c0

        b_Wtok = Buf("Wtok")
        b_stb = [bufs(4, f"stb{i}_") for i in range(2)]
        b_stf = [bufs(4, f"stf{i}_") for i in range(2)]
        load_w(0)
        nb = 0
        nbf = 0
        ecnt = 0
        for w in range(8):
            if w + 1 < 8:
                load_w(w + 1)
            elif True:
                o = 0
                for (c0, c1) in ((1792, 2048), (2304, 2560), (2560, 2608)):
                    mk.dma("sp", Wtok[:, :, o:o + (c1 - c0)], wsrc[:, :, c0:c1], reads=[P.wb["w_in"]], writes=[b_Wtok])
                    o += c1 - c0
            k = w % 2
            for m in range(4):
                ch = 4 * w + m
                isf = ch >= 16
                if isf:
                    sidx = nbf % 2
                    nbf += 1
                    stage, sbufs_ = stf[sidx], b_stf[sidx]
                else:
                    sidx = nb % 2
                    nb += 1
                    stage, sbufs_ = stb[sidx], b_stb[sidx]
                for tg in range(4):
                    pb = pcnt % 6
                    pcnt += 1
                    for c in range(16):
                        mk.op("pe", lambda e, k=k, c=c, m=m, tg=tg, pb=pb: e.matmul(
                            ps[pb][:], lhsT=WB[k][:, c, m * 128:(m + 1) * 128], rhs=yT[:, c, tg * 512:(tg + 1) * 512],
                            start=(c == 0), stop=(c == 15)),
                            reads=[b_WB[k], b_yT[2 * tg], b_yT[2 * tg + 1]], writes=[psb[pb]])
                    evac(mk, ecnt, stage[:, tg * 512:(tg + 1) * 512], ps[pb][:], [psb[pb]], [sbufs_[tg]],
                         scale=(0.125 if ch < 8 else 1.0))
                    ecnt += 1
                if ch < 8:
                    dst = X["QT"][s, ch * 128:(ch + 1) * 128, :]
                    db = P.xb["QT"][s]
                elif ch < 12:
                    dst = X["KC"][s, (ch - 8) * 128:(ch - 7) * 128, :]
                    db = P.xb["KC"][s]
                elif ch < 14:
                    dst = X["KS"][s, (ch - 12) * 128:(ch - 11) * 128, :]
                    db = P.xb["KS"][s]
                elif ch < 16:
                    dst = X["KW"][s, (ch - 14) * 128:(ch - 13) * 128, :]
                    db = P.xb["KW"][s]
                elif ch < 24:
                    dst = X["RX"][s, (ch - 16) * 128:(ch - 15) * 128, :]
                    db = P.xb["RX"][s]
                else:
                    dst = X["RY"][s, (ch - 24) * 128:(ch - 23) * 128, :]
                    db = P.xb["RY"][s]
                dmab = Buf()
                mk.dma("pool", dst, stage[:], reads=sbufs_, writes=[dmab])
                for kk, vv in dmab.w.items():
                    db.w[kk] = max(db.w.get(kk, 0), vv)

        b_gtmp = bufs(2, "gtmp")
        b_gst = bufs(2, "gst")
        for tt in range(16):
            k = tt % 2
            pv = pcnt % 6
            pcnt += 1
            pg = 6 + (tt % 2)
            for c in range(16):
                lhs = yT[:, c, tt * 128:(tt + 1) * 128]
                mk.op("pe", lambda e, c=c, pv=pv, lhs=lhs: e.matmul(ps[pv][:], lhsT=lhs, rhs=Wtok[:, c, 0:512],
                                                                     start=(c == 0), stop=(c == 15)),
                      reads=[b_Wtok, b_yT[tt // 2]], writes=[psb[pv]])
                mk.op("pe", lambda e, c=c, pg=pg, lhs=lhs: e.matmul(ps[pg][:, 0:48], lhsT=lhs, rhs=Wtok[:, c, 512:560],
                                                                     start=(c == 0), stop=(c == 15)),
                      reads=[b_Wtok, b_yT[tt // 2]], writes=[psb[pg]])
            evac(mk, tt, Vst[k][:, :, :, 0:64], ps[pv][:].rearrange("p (a g d) -> p a g d", a=2, g=4),
                 [psb[pv]], [b_vst[k]])
            mk.op("dve", lambda e, k=k, pg=pg: e.tensor_tensor(out=gtmp[k][:], in0=ps[pg][:, 0:48], in1=bgate[:],
                                                                op=ALU.add),
                  reads=[psb[pg], b_bg], writes=[b_gtmp[k]])
            mk.op("act", lambda e, k=k: e.activation(out=gst[k][:], in_=gtmp[k][:], func=AF.Sigmoid),
                  reads=[b_gtmp[k]], writes=[b_gst[k]])
            for (dst, src, sbuf_, key) in ((X["VSW"][s, tt], Vst[k][:], b_vst[k], "VSW"),
                                           (X["GATE"][s, tt], gst[k][:], b_gst[k], "GATE")):
                dmab = Buf()
                mk.dma("pool", dst, src, reads=[sbuf_], writes=[dmab])
                db = P.xb[key][s]
                for kk, vv in dmab.w.items():
                    db.w[kk] = max(db.w.get(kk, 0), vv)
        mk.flush()


def pc(v, nchunk):
    return np.ascontiguousarray(np.asarray(v, np.float32).reshape(nchunk, 128).T)


def make_in_maps(inp, n_cores=N_CORES):
    f = lambda k: np.asarray(inp[k], np.float32)
    shared = {}
    shared["w_in"] = np.ascontiguousarray(f("w_in")[0])
    shared["w_out"] = np.ascontiguousarray(f("w_out")[0])
    shared["w_g"] = np.ascontiguousarray(f("w_ffn_gate")[0])
    shared["w_u"] = np.ascontiguousarray(f("w_ffn_up")[0])
    shared["w_d"] = np.ascontiguousarray(f("w_ffn_down")[0])
    shared["g_mix"] = pc(f("mix_norm_g")[0], 16)
    shared["g_ffn"] = pc(f("ffn_norm_g")[0], 16)
    shared["g_fin"] = pc(f("final_norm_g"), 16)
    shared["b_gate_bc"] = np.ascontiguousarray(np.broadcast_to(f("b_gate")[0][None, :], (128, 48)))
    shared["g_attn_bc"] = np.ascontiguousarray(np.broadcast_to(f("attn_out_g")[0][None, :], (128, 1024)))
    rv = np.zeros((128, 8, 10), np.float32)
    cw = f("rnn_conv_w")[0]
    for k in range(4):
        rv[:, :, k] = pc(cw[k], 8)
    rv[:, :, 4] = pc(f("rnn_conv_b")[0], 8)
    rv[:, :, 5] = pc(f("rg_a_b")[0], 8)
    rv[:, :, 6] = pc(f("rg_x_b")[0], 8)
    rv[:, :, 7] = pc(f("rg_lambda")[0], 8)
    rv[:, :, 8] = pc(f("rnn_out_g")[0], 8)
    shared["rnn_vec"] = rv
    shared["rg_a_w"] = np.ascontiguousarray(f("rg_a_w")[0])
    shared["rg_x_w"] = np.ascontiguousarray(f("rg_x_w")[0])
    fv = np.zeros((128, NFC, 4), np.float32)
    fw = f("ffn_conv_w")[0]
    for k in range(3):
        fv[:, :, k] = pc(fw[k], NFC)
    fv[:, :, 3] = pc(f("ffn_conv_b")[0], NFC)
    shared["ffn_vec"] = fv
    shared["rel_bias"] = np.ascontiguousarray(f("rel_bias"))
    w1 = np.stack([f("cmp_k_w1")[0], f("cmp_v_w1")[0]])
    shared["cmp_w1"] = np.ascontiguousarray(w1.reshape(2, 32, 64, 256).transpose(0, 2, 1, 3))
    w2 = np.stack([f("cmp_k_w2")[0], f("cmp_v_w2")[0]])
    shared["cmp_w2"] = np.ascontiguousarray(w2.reshape(2, 2, 128, 64).transpose(0, 2, 1, 3))
    pe = np.stack([f("cmp_pe_k")[0], f("cmp_pe_v")[0]])
    shared["cmp_peT"] = np.ascontiguousarray(pe.transpose(0, 2, 1))
    shared.update(host_constants())
    x = f("x")
    maps = []
    for c in range(n_cores):
        m = dict(shared)
        m["xT"] = np.ascontiguousarray(x[c * NSEQ:(c + 1) * NSEQ].transpose(0, 2, 1))
        maps.append(m)
    return maps


_NC_CACHE = {}


def kernel(**inputs):
    if "nc" not in _NC_CACHE:
        _NC_CACHE["nc"] = build()
    nc = _NC_CACHE["nc"]
    maps = make_in_maps(inputs)
    res = run_bass_kernel_spmd(nc, maps, core_ids=list(range(N_CORES)))
    outs = [np.asarray(r["outT"]).transpose(0, 2, 1) for r in res.results]
    return np.ascontiguousarray(np.concatenate(outs, axis=0).astype(np.float32))


def phase2(P, s):
    nc, mk, I, W, X, C = P.nc, P.mk, P.I, P.W, P.X, P.C
    mk.barrier()
    with ExitStack() as ts:
        def sb(name, shape, dt):
            return ts.enter_context(nc.sbuf_tensor(f"p2_{s}_{name}", shape, dt))

        rv = sb("rv", [128, 8, 10], F32)
        cl = sb("cl", [128, 8, 2], F32)
        tmp8 = sb("tmp8", [128, 8], F32)
        cst1 = sb("cst1", [128, 2], F32)
        BDa = sb("BDa", [128, 8, 128], BF16)
        BDx = sb("BDx", [128, 8, 128], BF16)
        rxp = [sb(f"rxp{i}", [128, 3 + S], F32) for i in range(2)]
        ryt = [sb(f"ryt{i}", [128, S], F32) for i in range(2)]
        xr = sb("xr", [128, S], F32)
        xrb = sb("xrb", [128, S], BF16)
        rr = sb("rr", [128, S], F32)
        gi = sb("gi", [128, S], F32)
        aa = sb("aa", [128, S], F32)
        mm = sb("mm", [128, S], F32)
        hh = sb("hh", [128, S], F32)
        sqo = sb("sqo", [128, S], BF16)
        mst = [sb(f"mst{i}", [128, S], BF16) for i in range(2)]
        rstdr = sb("rstdr", [128, S], F32)
        ps = [ts.enter_context(nc.psum_tensor(f"p2_{s}_ps{i}", [128, 512], F32)) for i in range(8)]
        psb = bufs(8, "ps")

        b_rv = Buf("rv")
        mk.dma("sp", rv[:], I["rnn_vec"][:, :, :], writes=[b_rv])
        b_cst = Buf("cst")
        mk.op("dve", lambda e: e.memset(cst1[:, 0:1], 1.0), writes=[b_cst])
        mk.op("dve", lambda e: e.memset(cst1[:, 1:2], EPS), writes=[b_cst])
        b_cl = Buf("cl")
        b_t8 = Buf("t8")
        mk.op("act", lambda e: e.activation(out=tmp8[:], in_=rv[:, :, 7], func=AF.Exp, scale=-1.0),
              reads=[b_rv], writes=[b_t8])
        mk.op("act", lambda e: e.activation(out=tmp8[:], in_=tmp8[:], func=AF.Ln, bias=cst1[:, 0:1]),
              reads=[b_t8, b_cst], writes=[b_t8])
        mk.op("dve", lambda e: e.tensor_scalar(out=cl[:, :, 0], in0=tmp8[:], scalar1=-8.0, scalar2=None, op0=ALU.mult),
              reads=[b_t8], writes=[b_cl])
        mk.op("dve", lambda e: e.tensor_scalar(out=cl[:, :, 1], in0=tmp8[:], scalar1=-16.0, scalar2=None, op0=ALU.mult),
              reads=[b_t8], writes=[b_cl])
        b_bd = Buf("bd")
        mk.op("dve", lambda e: e.memset(BDa[:], 0.0), writes=[b_bd])
        mk.op("dve", lambda e: e.memset(BDx[:], 0.0), writes=[b_bd])
        for (bd, key) in ((BDa, "rg_a_w"), (BDx, "rg_x_w")):
            src = I[key].rearrange("(c two) i j -> two i c j", two=2)
            mk.dma("pool", bd[0:64, :, 0:64], src[0], writes=[b_bd])
            mk.dma("pool", bd[64:128, :, 64:128], src[1], writes=[b_bd])
        b_rxp = bufs(2, "rxp")
        b_ry = bufs(2, "ry")
        for i in range(2):
            mk.op("dve", lambda e, i=i: e.memset(rxp[i][:, 0:3], 0.0), writes=[b_rxp[i]])
        b_xr, b_xrb, b_rr, b_gi, b_aa, b_mm, b_hh, b_sqo = (Buf(n) for n in
                                                              ("xr", "xrb", "rr", "gi", "aa", "mm", "hh", "sqo"))
        b_mst = bufs(2, "mst")

        def load(c):
            k = c % 2
            mk.dma("sp", rxp[k][:, 3:3 + S], X["RX"][s, c * 128:(c + 1) * 128, :], reads=[P.xb["RX"][s]],
                   writes=[b_rxp[k]])
            mk.dma("sp", ryt[k][:], X["RY"][s, c * 128:(c + 1) * 128, :], reads=[P.xb["RY"][s]], writes=[b_ry[k]])

        load(0)
        for c in range(8):
            k = c % 2
            if c + 1 < 8:
                load(c + 1)
            mk.op("act", lambda e, k=k, c=c: e.activation(out=xr[:], in_=rxp[k][:, 3:3 + S], func=AF.Identity,
                                                          scale=rv[:, c, 3:4], bias=rv[:, c, 4:5]),
                  reads=[b_rxp[k], b_rv], writes=[b_xr])
            for kk in range(3):
                mk.op("dve", lambda e, k=k, c=c, kk=kk: e.scalar_tensor_tensor(
                    out=xr[:], in0=rxp[k][:, kk:kk + S], scalar=rv[:, c, kk:kk + 1], in1=xr[:],
                    op0=ALU.mult, op1=ALU.add), reads=[b_rxp[k], b_rv, b_xr], writes=[b_xr])
            mk.op("act", act_copy(xrb[:], xr[:]), reads=[b_xr], writes=[b_xrb])
            for tg in range(4):
                sl = slice(tg * 512, (tg + 1) * 512)
                pr = tg % 2
                pg = 2 + tg % 2
                mk.op("pe", lambda e, c=c, sl=sl, pr=pr: e.matmul(ps[pr][:], lhsT=BDa[:, c, :], rhs=xrb[:, sl],
                                                                 start=True, stop=True),
                      reads=[b_bd, b_xrb], writes=[psb[pr]])
                mk.op("pe", lambda e, c=c, sl=sl, pg=pg: e.matmul(ps[pg][:], lhsT=BDx[:, c, :], rhs=xrb[:, sl],
                                                                 start=True, stop=True),
                      reads=[b_bd, b_xrb], writes=[psb[pg]])
                mk.op("act", lambda e, c=c, sl=sl, pr=pr: e.activation(out=rr[:, sl], in_=ps[pr][:], func=AF.Sigmoid,
                                                                      bias=rv[:, c, 5:6]),
                      reads=[psb[pr], b_rv], writes=[b_rr])
                mk.op("act", lambda e, c=c, sl=sl, pg=pg: e.activation(out=gi[:, sl], in_=ps[pg][:], func=AF.Sigmoid,
                                                                      bias=rv[:, c, 6:7]),
                      reads=[psb[pg], b_rv], writes=[b_gi])
            mk.op("act", lambda e, c=c: e.activation(out=aa[:], in_=rr[:], func=AF.Exp, scale=cl[:, c, 0:1]),
                  reads=[b_rr, b_cl], writes=[b_aa])
            mk.op("act", lambda e, c=c: e.activation(out=mm[:], in_=rr[:], func=AF.Exp, scale=cl[:, c, 1:2]),
                  reads=[b_rr, b_cl], writes=[b_mm])
            mk.op("act", lambda e: e.activation(out=mm[:], in_=mm[:], func=AF.Sqrt, scale=-1.0, bias=cst1[:, 0:1]),
                  reads=[b_mm, b_cst], writes=[b_mm])
            mk.op("dve", lambda e: e.tensor_tensor(out=gi[:], in0=gi[:], in1=xr[:], op=ALU.mult),
                  reads=[b_gi, b_xr], writes=[b_gi])
            mk.op("dve", lambda e: e.tensor_tensor(out=gi[:], in0=gi[:], in1=mm[:], op=ALU.mult),
                  reads=[b_gi, b_mm], writes=[b_gi])
            mk.op("dve", lambda e: e.tensor_tensor_scan(out=hh[:], data0=aa[:], data1=gi[:], initial=0.0,
                                                        op0=ALU.mult, op1=ALU.add),
                  reads=[b_aa, b_gi], writes=[b_hh])
            mk.op("act", lambda e, k=k: e.activation(out=ryt[k][:], in_=ryt[k][:], func=AF.Gelu_apprx_tanh),
                  reads=[b_ry[k]], writes=[b_ry[k]])
            mk.op("dve", lambda e, k=k: e.tensor_tensor(out=hh[:], in0=hh[:], in1=ryt[k][:], op=ALU.mult),
                  reads=[b_hh, b_ry[k]], writes=[b_hh])
            mk.op("act", lambda e: e.activation(out=sqo[:], in_=hh[:], func=AF.Square), reads=[b_hh], writes=[b_sqo])
            for tg in range(4):
                sl = slice(tg * 512, (tg + 1) * 512)
                mk.op("pe", lambda e, c=c, sl=sl, tg=tg: e.matmul(ps[4 + tg][:], lhsT=C["ones"][:], rhs=sqo[:, sl],
                                                                 start=(c == 0), stop=(c == 7)),
                      reads=[b_sqo, P.cb], writes=[psb[4 + tg]])
            mk.op("dve", lambda e, k=k, c=c: e.tensor_scalar(out=mst[k][:], in0=hh[:], scalar1=rv[:, c, 8:9],
                                                             scalar2=None, op0=ALU.mult),
                  reads=[b_hh, b_rv], writes=[b_mst[k]])
            dmab = Buf()
            mk.dma("pool", X["MIXR"][s, c * 128:(c + 1) * 128, :], mst[k][:], reads=[b_mst[k]], writes=[dmab])
            db = P.xb["MIXR"][s]
            for kk_, vv in dmab.w.items():
                db.w[kk_] = max(db.w.get(kk_, 0), vv)
        b_rs = Buf("rstdr")
        for tg in range(4):
            sl = slice(tg * 512, (tg + 1) * 512)
            mk.op("act", lambda e, sl=sl, tg=tg: e.activation(out=rstdr[:, sl], in_=ps[4 + tg][:], func=AF.Sqrt,
                                                              scale=1.0 / 1024.0, bias=cst1[:, 1:2]),
                  reads=[psb[4 + tg], b_cst], writes=[b_rs])
        mk.op("dve", lambda e: e.reciprocal(out=rstdr[:], in_=rstdr[:]), reads=[b_rs], writes=[b_rs])
        mk.dma("pool", X["RSTDR"][s], rstdr[:], reads=[b_rs], writes=[P.xb["RSTDR"][s]])
        mk.flush()


def merge_ev(dst_buf, src_buf):
    for kk, vv in src_buf.w.items():
        dst_buf.w[kk] = max(dst_buf.w.get(kk, 0), vv)


def phase3_setup(P):
    pass


def phase3(P, s):
    if s == 0:
        phase3_all(P)


def phase3_all(P):
    nc, mk, I, W, X, C = P.nc, P.mk, P.I, P.W, P.X, P.C
    nseq = P.nseq
    mk.barrier()
    with ExitStack() as ts:
        def sb(name, shape, dt):
            return ts.enter_context(nc.sbuf_tensor(f"p3_{name}", shape, dt))

        DNb = sb("DNb", [128, 16, 2, 128], BF16)
        DN4b = sb("DN4b", [128, 4, 128], BF16)
        Mb = sb("Mb", [16, 16, 128], BF16)
        zc = sb("zc", [16, 272], BF16)
        W1 = sb("W1", [64, 2, 32, 256], BF16)
        W2 = sb("W2", [128, 2, 2, 64], BF16)
        peT = sb("peT", [64, 2, 34], BF16)
        pebias = sb("pebias", [128, 2, 2], F32)
        VAL = sb("VAL", [128, 16, 32], F32)
        ADD = sb("ADD", [128, 16, 32], F32)
        gbc = sb("gbc", [128, 1024], F32)
        KsA = sb("KsA", [96, S], BF16)
        VCA = sb("VCA", [128, 4, 97], BF16)
        NSP = [sb(f"NSP{i}", [128, 96], BF16) for i in range(2)]
        cst1 = sb("cst1", [128, 2], F32)
        ts2 = ExitStack()

        def sb2(name, shape, dt):
            return ts2.enter_context(nc.sbuf_tensor(f"p3t_{name}", shape, dt))

        tab = sb2("tab", [32, 16], F32)
        tabb = [sb2(f"tabb{i}", [32, 128], F32) for i in range(2)]
        oh = sb2("oh", [32, GW], F32)
        rst = [sb2(f"rst{i}", [128, GW], F32) for i in range(2)]
        dn01 = sb2("dn01", [128, 16, 2, 128], F32)
        m0 = sb2("m0", [128, 128], F32)
        negm0 = sb2("negm0", [128, 128], F32)
        m4 = sb2("m4", [128, 128], F32)
        mbf = sb2("mbf", [16, 16, 128], F32)
        cvm = sb2("cvm", [16, 128], F32)
        negcv = sb2("negcv", [16, 128], F32)
        ps = [ts.enter_context(nc.psum_tensor(f"p3_ps{i}", [128, 512], F32)) for i in range(8)]
        psb = bufs(8, "ps")

        b_c = Buf("p3c")
        for (t_, src) in ((tab, I["rel_bias"]), (oh, I["oh"]), (m0, I["mask_d0"]), (m4, I["mask_d4"]),
                          (cvm, I["cmp_valid"]), (zc, I["zc"]), (VAL, I["sel_val"]), (ADD, I["sel_add"]),
                          (gbc, I["g_attn_bc"])):
            b = Buf()
            mk.dma("sp", t_[:], src, writes=[b])
            merge_ev(b_c, b)
        b = Buf()
        mk.dma("sp", KsA[64:96, :], I["e_rows"][:, :], writes=[b])
        merge_ev(b_c, b)
        for i in range(2):
            b = Buf()
            mk.op("dve", lambda e, i=i: e.memset(NSP[i][:], 0.0), writes=[b])
            merge_ev(b_c, b)
        b = Buf()
        mk.op("dve", lambda e: e.memset(cst1[:, 0:1], 1.0), writes=[b])
        mk.op("dve", lambda e: e.memset(cst1[:, 1:2], EPS), writes=[b])
        merge_ev(b_c, b)
        b_vca = Buf("vca")
        mk.op("dve", lambda e: e.memset(VCA[:], 1.0), writes=[b_vca])
        for g in range(4):
            mk.dma("pool", VCA[0:NCMP, g, 65:97], I["overlap"][:, :], writes=[b_vca])
        b_w1 = Buf("w1")
        mk.dma("pool", W1[:, 0], I["cmp_w1"][0], writes=[b_w1])
        mk.dma("pool", W1[:, 1], I["cmp_w1"][1], writes=[b_w1])
        mk.dma("pool", W2[:], I["cmp_w2"].rearrange("kv p m e -> p kv m e"), writes=[b_w1])
        mk.op("dve", lambda e: e.memset(peT[:], 0.0), writes=[b_w1])
        mk.dma("pool", peT[:, :, 0:32], I["cmp_peT"].rearrange("kv d l -> d kv l"), writes=[b_w1])

        b_tabb = bufs(2, "tabb")
        b_rst = bufs(2, "rst")
        b_rtab = Buf("rtab")
        for h in range(16):
            k = h % 2
            mk.op("dve", lambda e, k=k, h=h: e.tensor_copy(out=tabb[k][:], in_=tab[:, h:h + 1].to_broadcast([32, 128])),
                  reads=[b_c], writes=[b_tabb[k]])
            mk.op("pe", lambda e, k=k: e.matmul(ps[k][:, 0:GW], lhsT=tabb[k][:], rhs=oh[:], start=True, stop=True),
                  reads=[b_tabb[k], b_c], writes=[psb[k]])
            mk.op("act", act_copy(rst[k][:], ps[k][:, 0:GW]), reads=[psb[k]], writes=[b_rst[k]])
            b = Buf()
            mk.dma("sp", X["RTAB"][h], rst[k][:], reads=[b_rst[k]], writes=[b])
            merge_ev(b_rtab, b)
        b_dn = Buf("dn")
        rt_t = X["RTAB"].tensor
        mk.dma("sp", dn01[:], bass.AP(rt_t, 127, [[GW - 1, 128], [128 * GW, 16], [128, 2], [1, 128]]),
               reads=[b_rtab], writes=[b_dn])
        b_mb = Buf("mbf")
        mk.dma("sp", mbf[:], bass.AP(rt_t, 224, [[GW - 16, 16], [128 * GW, 16], [1, 128]]), reads=[b_rtab],
               writes=[b_mb])
        b_m = Buf("masks")
        mk.op("dve", lambda e: e.tensor_scalar(out=negm0[:], in0=m0[:], scalar1=-1.0, scalar2=-NEG, op0=ALU.add,
                                               op1=ALU.mult), reads=[b_c], writes=[b_m])
        mk.op("dve", lambda e: e.tensor_scalar(out=m4[:], in0=m4[:], scalar1=-1.0, scalar2=-NEG, op0=ALU.add,
                                               op1=ALU.mult), reads=[b_c], writes=[b_m])
        mk.op("dve", lambda e: e.tensor_scalar(out=negcv[:], in0=cvm[:], scalar1=-1.0, scalar2=-NEG, op0=ALU.add,
                                               op1=ALU.mult), reads=[b_c], writes=[b_m])
        b_DN = Buf("DN")
        mk.op("dve", lambda e: e.tensor_tensor(out=dn01[:, :, 0, :], in0=dn01[:, :, 0, :],
                                               in1=m0[:].unsqueeze(1).to_broadcast([128, 16, 128]), op=ALU.mult),
              reads=[b_dn, b_c], writes=[b_dn])
        mk.op("dve", lambda e: e.tensor_tensor(out=DNb[:, :, 0, :], in0=dn01[:, :, 0, :],
                                               in1=negm0[:].unsqueeze(1).to_broadcast([128, 16, 128]), op=ALU.add),
              reads=[b_dn, b_m], writes=[b_DN])
        mk.op("dve", lambda e: e.tensor_copy(out=DNb[:, :, 1, :], in_=dn01[:, :, 1, :]), reads=[b_dn], writes=[b_DN])
        mk.op("dve", lambda e: e.tensor_copy(out=DN4b[:], in_=m4[:].unsqueeze(1).to_broadcast([128, 4, 128])),
              reads=[b_m], writes=[b_DN])
        mk.op("dve", lambda e: e.tensor_tensor(out=mbf[:], in0=mbf[:],
                                               in1=cvm[:].unsqueeze(1).to_broadcast([16, 16, 128]), op=ALU.mult),
              reads=[b_mb, b_c], writes=[b_mb])
        mk.op("dve", lambda e: e.tensor_tensor(out=Mb[:], in0=mbf[:],
                                               in1=negcv[:].unsqueeze(1).to_broadcast([16, 16, 128]), op=ALU.add),
              reads=[b_mb, b_m], writes=[b_DN])
        b_pb = Buf("pebias")
        for kv in range(2):
            for mc in range(2):
                pb = 2 + mc
                for l in range(32):
                    mk.op("pe", lambda e, kv=kv, mc=mc, l=l, pb=pb: e.matmul(
                        ps[pb][:, 0:2], lhsT=W1[:, kv, l, mc * 128:(mc + 1) * 128], rhs=peT[:, kv, l:l + 2],
                        start=(l == 0), stop=(l == 31)), reads=[b_w1], writes=[psb[pb]])
                mk.op("act", act_copy(pebias[:, kv, mc:mc + 1], ps[pb][:, 0:1]), reads=[psb[pb]], writes=[b_pb])

        if "DBGDN" in X:
            mk.dma("sp", X["DBGDN"], DNb[:], reads=[b_DN], writes=[Buf()])
            mk.dma("sp", X["DBGMB"], Mb[:], reads=[b_DN], writes=[Buf()])
        mk.flush()
        ts2.close()
        L = dict(locals())
        for s in range(nseq):
            attention_seq(P, s, L)


def attention_seq(P, s, L):
    nc, mk, I, W, X, C = P.nc, P.mk, P.I, P.W, P.X, P.C
    ps, psb = L["ps"], L["psb"]
    b_c, b_DN, b_w1, b_pb, b_vca = L["b_c"], L["b_DN"], L["b_w1"], L["b_pb"], L["b_vca"]
    W1, W2, pebias, VCA, KsA, zc, Mb, DNb, DN4b = (L[k] for k in ("W1", "W2", "pebias", "VCA", "KsA", "zc", "Mb",
                                                                 "DNb", "DN4b"))
    VAL, ADD, gbc, NSP, cst1 = L["VAL"], L["ADD"], L["gbc"], L["NSP"], L["cst1"]
    ident = C["ident"]
    with ExitStack() as ts:
        def sb(name, shape, dt):
            return ts.enter_context(nc.sbuf_tensor(f"p3s_{s}_{name}", shape, dt))

        big = sb("big", [128, 16384], BF16)
        KCt = big[0:64, :].rearrange("p (kv g t) -> p kv g t", kv=2, g=4)
        HT = sb("HT", [128, 2, 2, 508], BF16)
        KCMP = sb("KCMP", [64, 4, NCMP], BF16)
        Gt = sb("Gt", [128, 16, 48], F32)
        QA = sb("QA", [96, 4, S], BF16)
        KwT = sb("KwT", [64, S], BF16)
        Vsw = sb("Vsw", [128, 16, 2, 65], BF16)
        PTc = [sb(f"PTc{i}", [128, 512], BF16) for i in range(2)]
        PTs = [big[:, i * 8192:(i + 1) * 8192].rearrange("p (k n) -> p k n", k=16) for i in range(2)]
        PTw = [sb(f"PTw{i}", [128, 5, 512], BF16) for i in range(2)]
        ocmp = sb("ocmp", [128, 16, 4, 64], F32)
        oacc = [sb(f"oacc{i}", [128, 4, 64], F32) for i in range(2)]
        rs = sb("rs", [128, 4], F32)
        rinv = sb("rinv", [128, 4], F32)
        coef = sb("coef", [128, 4], F32)
        imp = sb("imp", [128, 32], F32)
        score = sb("score", [128, 32], F32)
        sc2 = sb("sc2", [128, 32], F32)
        m8a = sb("m8a", [128, 8], F32)
        m8b = sb("m8b", [128, 8], F32)
        thr = sb("thr", [128, 1], F32)
        selt = sb("selt", [128, 32], F32)
        oat = [sb(f"oat{i}", [128, 1024], F32) for i in range(2)]
        junk = sb("junk", [128, 1024], BF16)
        ssq = sb("ssq", [128, 1], F32)
        rstd = sb("rstd", [128, 1], F32)
        mtok = [sb(f"mtok{i}", [128, 1024], BF16) for i in range(2)]
        mixst = [sb(f"mixst{i}", [128, 8, 128], BF16) for i in range(2)]
        pst = [ts.enter_context(nc.psum_tensor(f"p3s_{s}_pst{i}", [128, 8, 128], BF16)) for i in range(0)]

        mk.barrier()
        b_kct = Buf("kct")
        mk.dma("sp", KCt, X["KC"][s].rearrange("(kv g d) t -> d kv g t", kv=2, g=4), reads=[P.xb["KC"][s]],
               writes=[b_kct])
        b_G = Buf("G")
        mk.dma("sp", Gt[:], X["GATE"][s].rearrange("tt p e -> p tt e"), reads=[P.xb["GATE"][s]], writes=[b_G])
        b_ht = bufs(4, "ht")
        for kv in range(2):
            for mc in range(2):
                pb = (2 * kv + mc) % 4
                for l in range(32):
                    mk.op("pe", lambda e, kv=kv, mc=mc, l=l, pb=pb: e.matmul(
                        ps[pb][:, 0:508].rearrange("p (g c) -> p g c", g=4),
                        lhsT=W1[:, kv, l, mc * 128:(mc + 1) * 128], rhs=KCt[:, kv, :, l:l + 2017:16],
                        start=(l == 0), stop=(l == 31)), reads=[b_w1, b_kct], writes=[psb[pb]])
                mk.op("act", lambda e, kv=kv, mc=mc, pb=pb: e.activation(
                    out=HT[:, kv, mc, :], in_=ps[pb][:, 0:508], func=AF.Gelu_apprx_tanh, bias=pebias[:, kv, mc:mc + 1]),
                    reads=[psb[pb], b_pb], writes=[b_ht[2 * kv + mc]])
        b_kcmp = Buf("kcmp")
        for mc in range(2):
            mk.op("pe", lambda e, mc=mc: e.matmul(ps[4][0:64, 0:508], lhsT=W2[:, 0, mc, :], rhs=HT[:, 0, mc, :],
                                                  start=(mc == 0), stop=(mc == 1)),
                  reads=[b_w1, b_ht[0], b_ht[1]], writes=[psb[4]])
        mk.op("act", act_copy(KCMP[:], ps[4][0:64, 0:508].rearrange("p (g c) -> p g c", g=4)), reads=[psb[4]],
              writes=[b_kcmp])
        b_vc = Buf("vcmp")
        merge_ev(b_vc, b_vca)
        for g in range(4):
            pb = 5 + g % 2
            for mc in range(2):
                mk.op("pe", lambda e, mc=mc, g=g, pb=pb: e.matmul(
                    ps[pb][0:NCMP, 0:64], lhsT=HT[:, 1, mc, g * NCMP:(g + 1) * NCMP], rhs=W2[:, 1, mc, :],
                    start=(mc == 0), stop=(mc == 1)), reads=[b_w1, b_ht[2], b_ht[3]], writes=[psb[pb]])
            bb = Buf()
            mk.op("dve", lambda e, g=g, pb=pb: e.tensor_copy(out=VCA[0:NCMP, g, 0:64], in_=ps[pb][0:NCMP, 0:64]),
                  reads=[psb[pb], b_vca], writes=[bb])
            merge_ev(b_vc, bb)
        b_vca.r = {}
        L["b_vca_last"] = b_vc

        mk.barrier()
        b_qa = Buf("qa")
        b_qsel = bufs(16, "qsel")
        b_ks = Buf("ks")
        b_kw = Buf("kw")
        b_v = Buf("v")
        b_ptc = bufs(2, "ptc")
        b_pts = [bufs(16, f"pts{i}_") for i in range(2)]
        b_ptw = [bufs(5, f"ptw{i}_") for i in range(2)]
        b_ocmp = bufs(16, "ocmp")
        b_oacc = bufs(2, "oacc")
        b_t = Buf("dvetmp")
        b_nsp = bufs(2, "nsp")
        for i in range(2):
            merge_ev(b_nsp[i], b_c)
        scnt = [0]

        def sbank():
            b = scnt[0] % 4
            scnt[0] += 1
            return b

        for g in range(4):
            mk.dma("sp", QA[0:64, :, :], X["QT"][s, g * 256:(g + 1) * 256, :].rearrange("(r d) t -> d r t", r=4),
                   reads=[P.xb["QT"][s]], writes=[b_qa] + b_qsel)
            mk.dma("sp", KsA[0:64, :], X["KS"][s, g * 64:(g + 1) * 64, :], reads=[P.xb["KS"][s]], writes=[b_ks])
            mk.dma("sp", KwT[:], X["KW"][s, g * 64:(g + 1) * 64, :], reads=[P.xb["KW"][s]], writes=[b_kw])
            for a_ in range(2):
                mk.dma("sp", Vsw[:, :, a_, :], X["VSW"][s][:, :, a_, g, :].rearrange("tt p e -> p tt e"),
                       reads=[P.xb["VSW"][s]], writes=[b_v])

            for qt in range(16):
                nk = min(NCMP, 8 * qt + 7)
                qs = slice(qt * 128, (qt + 1) * 128)
                sbk = sbank()
                k2 = qt % 2
                off = 136 - 8 * qt
                mk.op("pe", lambda e, g=g, nk=nk, qs=qs, sbk=sbk: e.matmul(
                    ps[sbk][0:nk, :].rearrange("p (r i) -> p r i", r=4), lhsT=KCMP[:, g, 0:nk], rhs=QA[0:64, :, qs],
                    start=True, stop=False), reads=[b_kcmp, b_qa], writes=[psb[sbk]])
                mk.op("pe", lambda e, g=g, nk=nk, off=off, sbk=sbk: e.matmul(
                    ps[sbk][0:nk, :].rearrange("p (r i) -> p r i", r=4), lhsT=zc[0:16, off:off + nk],
                    rhs=Mb[0:16, 4 * g:4 * g + 4, :], start=False, stop=True), reads=[b_c, b_DN], writes=[psb[sbk]])
                mk.op("act", lambda e, nk=nk, sbk=sbk, k2=k2: e.activation(out=PTc[k2][0:nk, :], in_=ps[sbk][0:nk, :],
                                                                          func=AF.Exp),
                      reads=[psb[sbk]], writes=[b_ptc[k2]])
                ob = 4 + k2
                for r in range(4):
                    mk.op("pe", lambda e, r=r, nk=nk, g=g, k2=k2, ob=ob: e.matmul(
                        ps[ob][:, r * 128:r * 128 + 97], lhsT=PTc[k2][0:nk, r * 128:(r + 1) * 128], rhs=VCA[0:nk, g, :],
                        start=True, stop=True), reads=[b_ptc[k2], b_vc], writes=[psb[ob]])
                O = ps[ob][:, :].rearrange("p (r e) -> p r e", r=4)
                mk.op("dve", lambda e, O=O: e.tensor_scalar(out=rs[:], in0=O[:, :, 64], scalar1=1e-30, scalar2=None,
                                                            op0=ALU.max), reads=[psb[ob]], writes=[b_t])
                mk.op("dve", lambda e: e.reciprocal(out=rinv[:], in_=rs[:]), reads=[b_t], writes=[b_t])
                mk.op("dve", lambda e, O=O: e.tensor_scalar(out=imp[:], in0=O[:, 0, 65:97], scalar1=rinv[:, 0:1],
                                                            scalar2=None, op0=ALU.mult),
                      reads=[psb[ob], b_t], writes=[b_t])
                for r in range(1, 4):
                    mk.op("dve", lambda e, O=O, r=r: e.scalar_tensor_tensor(
                        out=imp[:], in0=O[:, r, 65:97], scalar=rinv[:, r:r + 1], in1=imp[:], op0=ALU.mult,
                        op1=ALU.add), reads=[psb[ob], b_t], writes=[b_t])
                mk.op("dve", lambda e, qt=qt, g=g: e.tensor_tensor(out=coef[:], in0=rinv[:],
                                                                  in1=Gt[:, qt, g * 12:g * 12 + 12:3], op=ALU.mult),
                      reads=[b_t, b_G], writes=[b_t])
                for r in range(4):
                    mk.op("dve", lambda e, O=O, r=r, qt=qt: e.tensor_scalar(
                        out=ocmp[:, qt, r, :], in0=O[:, r, 0:64], scalar1=coef[:, r:r + 1], scalar2=None,
                        op0=ALU.mult), reads=[psb[ob], b_t], writes=[b_ocmp[qt]])
                mk.op("dve", lambda e, qt=qt: e.tensor_tensor(out=score[:], in0=imp[:], in1=VAL[:, qt, :], op=ALU.mult),
                      reads=[b_t, b_c], writes=[b_t])
                mk.op("dve", lambda e, qt=qt: e.tensor_tensor(out=score[:], in0=score[:], in1=ADD[:, qt, :], op=ALU.add),
                      reads=[b_t, b_c], writes=[b_t])
                mk.op("dve", lambda e: e.max(out=m8a[:], in_=score[:]), reads=[b_t], writes=[b_t])
                mk.op("dve", lambda e: e.match_replace(out=sc2[:], in_to_replace=m8a[:], in_values=score[:],
                                                       imm_value=-1e30), reads=[b_t], writes=[b_t])
                mk.op("dve", lambda e: e.max(out=m8b[:], in_=sc2[:]), reads=[b_t], writes=[b_t])
                mk.op("dve", lambda e: e.tensor_scalar(out=thr[:], in0=m8b[:, 7:8], scalar1=0.0, scalar2=None,
                                                       op0=ALU.max), reads=[b_t], writes=[b_t])
                mk.op("dve", lambda e: e.tensor_scalar(out=selt[:], in0=score[:], scalar1=thr[:, 0:1], scalar2=None,
                                                       op0=ALU.is_ge), reads=[b_t], writes=[b_t])
                mk.op("dve", lambda e, k2=k2: e.tensor_scalar(out=NSP[k2][:, 64:96], in0=selt[:], scalar1=-1.0,
                                                              scalar2=-NEG, op0=ALU.add, op1=ALU.mult),
                      reads=[b_t], writes=[b_nsp[k2]])
                tb = 6 + k2
                mk.op("pe", lambda e, k2=k2, tb=tb: e.matmul(ps[tb][0:96, 0:128], lhsT=NSP[k2][:, 0:96], rhs=ident[:],
                                                             start=True, stop=True),
                      reads=[b_nsp[k2], P.cb], writes=[psb[tb]])
                mk.op("act", lambda e, tb=tb, qs=qs: e.activation(
                    out=QA[64:96, :, qs], in_=ps[tb][64:96, 0:128].unsqueeze(1).to_broadcast([32, 4, 128]),
                    func=AF.Copy), reads=[psb[tb]], writes=[b_qsel[qt]])

            def qk(qt):
                k2 = qt % 2
                qs = slice(qt * 128, (qt + 1) * 128)
                for kt in range(0, qt + 1):
                    sbk = sbank()
                    ks_ = slice(kt * 128, (kt + 1) * 128)
                    near = kt >= qt - 1
                    mk.op("pe", lambda e, sbk=sbk, ks_=ks_, qs=qs, near=near: e.matmul(
                        ps[sbk][:, :].rearrange("p (r i) -> p r i", r=4), lhsT=KsA[0:96, ks_], rhs=QA[0:96, :, qs],
                        start=True, stop=(not near)), reads=[b_ks, b_c, b_qa, b_qsel[qt]], writes=[psb[sbk]])
                    if near:
                        mk.op("pe", lambda e, sbk=sbk, g=g, dl=qt - kt: e.matmul(
                            ps[sbk][:, :].rearrange("p (r i) -> p r i", r=4), lhsT=ident[:],
                            rhs=DNb[:, 4 * g:4 * g + 4, dl, :], start=False, stop=True),
                            reads=[P.cb, b_DN], writes=[psb[sbk]])
                    mk.op("act", lambda e, sbk=sbk, k2=k2, kt=kt: e.activation(out=PTs[k2][:, kt, :], in_=ps[sbk][:, :],
                                                                              func=AF.Exp),
                          reads=[psb[sbk]], writes=[b_pts[k2][kt]])
                for wi, kt in enumerate(range(max(0, qt - 4), qt + 1)):
                    sbk = sbank()
                    ks_ = slice(kt * 128, (kt + 1) * 128)
                    dl = qt - kt
                    sp_ = dl in (0, 1, 4)
                    mk.op("pe", lambda e, sbk=sbk, ks_=ks_, qs=qs, sp_=sp_: e.matmul(
                        ps[sbk][:, :].rearrange("p (r i) -> p r i", r=4), lhsT=KwT[0:64, ks_], rhs=QA[0:64, :, qs],
                        start=True, stop=(not sp_)), reads=[b_kw, b_qa], writes=[psb[sbk]])
                    if sp_:
                        rhs = DN4b[:] if dl == 4 else DNb[:, 4 * g:4 * g + 4, dl, :]
                        mk.op("pe", lambda e, sbk=sbk, rhs=rhs: e.matmul(
                            ps[sbk][:, :].rearrange("p (r i) -> p r i", r=4), lhsT=ident[:], rhs=rhs, start=False,
                            stop=True), reads=[P.cb, b_DN], writes=[psb[sbk]])
                    mk.op("act", lambda e, sbk=sbk, k2=k2, wi=wi: e.activation(out=PTw[k2][:, wi, :], in_=ps[sbk][:, :],
                                                                              func=AF.Exp),
                          reads=[psb[sbk]], writes=[b_ptw[k2][wi]])

            def pv(qt):
                k2 = qt % 2
                obs = 4 + 2 * k2
                obw = 5 + 2 * k2
                for r in range(4):
                    for kt in range(0, qt + 1):
                        mk.op("pe", lambda e, r=r, kt=kt, k2=k2, obs=obs, qt=qt: e.matmul(
                            ps[obs][:, r * 128:r * 128 + 65], lhsT=PTs[k2][:, kt, r * 128:(r + 1) * 128],
                            rhs=Vsw[:, kt, 0, :], start=(kt == 0), stop=(kt == qt)),
                            reads=[b_pts[k2][kt], b_v], writes=[psb[obs]])
                kts = list(range(max(0, qt - 4), qt + 1))
                for r in range(4):
                    for wi, kt in enumerate(kts):
                        mk.op("pe", lambda e, r=r, kt=kt, wi=wi, k2=k2, obw=obw: e.matmul(
                            ps[obw][:, r * 128:r * 128 + 65], lhsT=PTw[k2][:, wi, r * 128:(r + 1) * 128],
                            rhs=Vsw[:, kt, 1, :], start=(wi == 0), stop=(wi == len(kts) - 1)),
                            reads=[b_ptw[k2][wi], b_v], writes=[psb[obw]])
                for bi, ob in ((1, obs), (2, obw)):
                    O = ps[ob][:, :].rearrange("p (r e) -> p r e", r=4)
                    mk.op("dve", lambda e, O=O: e.reciprocal(out=rinv[:], in_=O[:, :, 64]), reads=[psb[ob]], writes=[b_t])
                    mk.op("dve", lambda e, qt=qt, bi=bi, g=g: e.tensor_tensor(
                        out=coef[:], in0=rinv[:], in1=Gt[:, qt, g * 12 + bi:g * 12 + 12:3], op=ALU.mult),
                        reads=[b_t, b_G], writes=[b_t])
                    for r in range(4):
                        src1 = ocmp[:, qt, r, :] if bi == 1 else oacc[k2][:, r, :]
                        mk.op("dve", lambda e, O=O, r=r, src1=src1, k2=k2: e.scalar_tensor_tensor(
                            out=oacc[k2][:, r, :], in0=O[:, r, 0:64], scalar=coef[:, r:r + 1], in1=src1, op0=ALU.mult,
                            op1=ALU.add), reads=[psb[ob], b_t, b_ocmp[qt], b_oacc[k2]], writes=[b_oacc[k2]])
                bb = Buf()
                mk.dma("pool", X["OATT"][s, qt, :, g * 256:(g + 1) * 256], oacc[k2][:].rearrange("p r e -> p (r e)"),
                       reads=[b_oacc[k2]], writes=[bb])
                merge_ev(P.xb["OATT"][s], bb)

            qk(0)
            for qt in range(16):
                if qt + 1 < 16:
                    qk(qt + 1)
                pv(qt)

        b_oat = bufs(2, "oat")
        b_n = Buf("normtmp")
        b_mtok = bufs(2, "mtok")
        b_mixst = bufs(2, "mixst")
        for qt in range(16):
            k2 = qt % 2
            mk.dma("sp", oat[k2][:], X["OATT"][s, qt], reads=[P.xb["OATT"][s]], writes=[b_oat[k2]])
            mk.op("act", lambda e, k2=k2: e.activation(out=junk[:], in_=oat[k2][:], func=AF.Square, accum_out=ssq[:]),
                  reads=[b_oat[k2]], writes=[b_n])
            mk.op("act", lambda e: e.activation(out=rstd[:], in_=ssq[:], func=AF.Sqrt, scale=1.0 / 1024.0,
                                                bias=cst1[:, 1:2]), reads=[b_n, b_c], writes=[b_n])
            mk.op("dve", lambda e: e.reciprocal(out=rstd[:], in_=rstd[:]), reads=[b_n], writes=[b_n])
            mk.op("dve", lambda e, k2=k2: e.scalar_tensor_tensor(out=mtok[k2][:], in0=oat[k2][:], scalar=rstd[:, 0:1],
                                                                 in1=gbc[:], op0=ALU.mult, op1=ALU.mult),
                  reads=[b_oat[k2], b_n, b_c], writes=[b_mtok[k2]])
            tb = 2 * k2
            for c in range(8):
                mk.op("pe", lambda e, c=c, k2=k2, tb=tb: e.matmul(
                    ps[tb + c // 4][:, (c % 4) * 128:(c % 4 + 1) * 128], lhsT=mtok[k2][:, c * 128:(c + 1) * 128],
                    rhs=ident[:], start=True, stop=True), reads=[b_mtok[k2], P.cb], writes=[psb[tb + c // 4]])
            for hh_ in range(2):
                evac(mk, hh_, mixst[k2][:, 4 * hh_:4 * hh_ + 4, :],
                     ps[tb + hh_][:, :].rearrange("p (c t) -> p c t", c=4), [psb[tb + hh_]], [b_mixst[k2]])
            bb = Buf()
            mk.dma("pool", X["MIXA"][s, :, qt * 128:(qt + 1) * 128].rearrange("(c p) t -> p c t", p=128), mixst[k2][:],
                   reads=[b_mixst[k2]], writes=[bb])
            merge_ev(P.xb["MIXA"][s], bb)
        mk.flush()


def phase4(P, nseq):
    nc, mk, I, W, X, C = P.nc, P.mk, P.I, P.W, P.X, P.C
    mk.barrier()
    TG = 512
    with ExitStack() as ts:
        def sb(name, shape, dt):
            return ts.enter_context(nc.sbuf_tensor(f"p4_{name}", shape, dt))

        hx = sb("hx", [128, 16, TG], F32)
        mixT = sb("mixT", [128, 16, TG], BF16)
        rstr = sb("rstr", [128, TG], F32)
        y2T = sb("y2T", [128, 16, TG], BF16)
        actT = sb("actT", [128, NFC, TG], BF16)
        W16 = [sb(f"W16_{i}", [128, 16, 256], BF16) for i in range(4)]
        WD = [sb(f"WD_{i}", [128, NFC, 128], BF16) for i in range(2)]
        gpre = [sb(f"gpre{i}", [128, TG + 2], F32) for i in range(2)]
        cv = [sb(f"cv{i}", [128, TG], F32) for i in range(2)]
        ge = [sb(f"ge{i}", [128, TG], F32) for i in range(2)]
        GC = sb("GC", [128, NFC, 2], F32)
        rt = sb("rt", [128, TG], F32)
        rstd = sb("rstd", [128, TG], F32)
        fv = sb("fv", [128, NFC, 4], F32)
        gffn = sb("gffn", [128, 16], F32)
        gfin = sb("gfin", [128, 16], F32)
        epst = sb("eps", [128, 1], F32)
        ps = [ts.enter_context(nc.psum_tensor(f"p4_ps{i}", [128, 512], F32)) for i in range(8)]
        psb = bufs(8, "ps")
        ones = C["ones"]

        b_c = Buf("p4c")
        for (t_, src) in ((fv, I["ffn_vec"]), (gffn, I["g_ffn"]), (gfin, I["g_fin"])):
            b = Buf()
            mk.dma("pool", t_[:], src, writes=[b])
            merge_ev(b_c, b)
        b = Buf()
        mk.op("dve", lambda e: e.memset(epst[:], EPS), writes=[b])
        merge_ev(b_c, b)
        P_EPS[0] = epst[:]
        P_EPS[1] = b_c

        b_hx = bufs(16, "hx")
        b_mix = bufs(16, "mix")
        b_rstr = Buf("rstr")
        b_y2 = bufs(16, "y2")
        b_act = bufs(NFC, "act")
        b_w16 = bufs(4, "w16")
        b_wd = bufs(2, "wd")
        b_gpre = bufs(2, "gpre")
        b_cv = bufs(2, "cv")
        b_ge = bufs(2, "ge")
        b_gc = bufs(NFC, "gc")
        b_rt = Buf("rt")
        b_rstd = Buf("rstd")

        groups = [(s, j) for s in range(nseq) for j in range(S // TG)]
        wo_src = W["w_out"].rearrange("(c p) n -> p c n", p=128)
        wg_src = W["w_g"].rearrange("(c p) n -> p c n", p=128)
        wu_src = W["w_u"].rearrange("(c p) n -> p c n", p=128)
        wd_src = W["w_d"].rearrange("(f p) n -> p f n", p=128)
        items16 = []
        for gi_ in range(len(groups)):
            for dcp in range(8):
                items16.append((wo_src[:, :, dcp * 256:(dcp + 1) * 256], P.wb["w_out"]))
            for fp in range(NFC // 2):
                items16.append((wg_src[:, :, fp * 256:(fp + 1) * 256], P.wb["w_g"]))
                items16.append((wu_src[:, :, fp * 256:(fp + 1) * 256], P.wb["w_u"]))
        st16 = [0]

        def need16(n):
            while st16[0] <= min(n + 3, len(items16) - 1):
                i = st16[0]
                src, wb_ = items16[i]
                mk.dma("sp", W16[i % 4][:], src, reads=[wb_], writes=[b_w16[i % 4]])
                st16[0] += 1

        itemsd = []
        for gi_ in range(len(groups)):
            for dc in range(16):
                itemsd.append(wd_src[:, :, dc * 128:(dc + 1) * 128])
        std = [0]

        def needd(n):
            while std[0] <= min(n + 1, len(itemsd) - 1):
                i = std[0]
                mk.dma("pool", WD[i % 2][:], itemsd[i], reads=[P.wb["w_d"]], writes=[b_wd[i % 2]])
                std[0] += 1

        pcnt = [0]

        def bank():
            b = pcnt[0] % 8
            pcnt[0] += 1
            return b

        def norm_stats(n_feat):
            pb = bank()
            for c in range(16):
                mk.op("act", lambda e, c=c: e.activation(out=mixT[:, c, :], in_=hx[:, c, :], func=AF.Square),
                      reads=[b_hx[c]], writes=[b_mix[c]])
                mk.op("pe", lambda e, c=c, pb=pb: e.matmul(ps[pb][:, :], lhsT=ones[:], rhs=mixT[:, c, :], start=(c == 0),
                                                           stop=(c == 15)), reads=[b_mix[c], P.cb], writes=[psb[pb]])
            rms_rstd(mk, ps[pb][:, :], psb[pb], rt[:], b_rt, rstd[:], b_rstd, float(n_feat))

        i16 = 0
        idn = 0
        import os
        P4S = int(os.environ.get("P4_STOP", "9"))
        CPE = os.environ.get("P4_CPE", "dve")
        if P4S < 9:
            groups = groups[:1]
        for gi_, (s, j) in enumerate(groups):
            t0 = j * TG
            tsl = slice(t0, t0 + TG)
            need16(i16)
            xsrc = I["xT"][s].rearrange("(c p) t -> p c t", p=128)
            for hf in range(2):
                cs = slice(hf * 8, hf * 8 + 8)
                mk.dma("pool", hx[:, cs, :], xsrc[:, cs, tsl], writes=b_hx[hf * 8:hf * 8 + 8])
            mk.dma("pool", mixT[:, 0:8, :], X["MIXA"][s].rearrange("(c p) t -> p c t", p=128)[:, :, tsl],
                   reads=[P.xb["MIXA"][s]], writes=b_mix[0:8])
            mk.dma("pool", mixT[:, 8:16, :], X["MIXR"][s].rearrange("(c p) t -> p c t", p=128)[:, :, tsl],
                   reads=[P.xb["MIXR"][s]], writes=b_mix[8:16])
            mk.dma("pool", rstr[:], X["RSTDR"][s][:, tsl], reads=[P.xb["RSTDR"][s]], writes=[b_rstr])
            for c in range(8, 16):
                mk.op("dve", lambda e, c=c: e.tensor_tensor(out=mixT[:, c, :], in0=mixT[:, c, :], in1=rstr[:],
                                                            op=ALU.mult), reads=[b_mix[c], b_rstr], writes=[b_mix[c]])
            if j == 0:
                for fc in range(NFC):
                    mk.op("pool", lambda e, fc=fc: e.memset(GC[:, fc, :], 0.0), writes=[b_gc[fc]])
            if P4S <= 1:
                break
            for dcp in range(8):
                need16(i16)
                slot = i16 % 4
                for dd in range(2):
                    dc = 2 * dcp + dd
                    pb = bank()
                    for c in range(16):
                        mk.op("pe", lambda e, c=c, pb=pb, slot=slot, dd=dd: e.matmul(
                            ps[pb][:, :], lhsT=W16[slot][:, c, dd * 128:(dd + 1) * 128], rhs=mixT[:, c, :],
                            start=(c == 0), stop=(c == 15)), reads=[b_w16[slot], b_mix[c]], writes=[psb[pb]])
                    mk.op("dve", lambda e, dc=dc, pb=pb: e.tensor_tensor(out=hx[:, dc, :], in0=ps[pb][:, :],
                                                                        in1=hx[:, dc, :], op=ALU.add),
                          reads=[psb[pb], b_hx[dc]], writes=[b_hx[dc]])
                i16 += 1
            if P4S <= 2:
                break
            norm_stats(D)
            for c in range(16):
                mk.op("dve", lambda e, c=c: e.scalar_tensor_tensor(out=y2T[:, c, :], in0=hx[:, c, :],
                                                                   scalar=gffn[:, c:c + 1], in1=rstd[:],
                                                                   op0=ALU.mult, op1=ALU.mult),
                      reads=[b_hx[c], b_rstd, b_c], writes=[b_y2[c]])
            if P4S <= 3:
                break
            for fp in range(NFC // 2):
                need16(i16)
                sg = i16 % 4
                su = (i16 + 1) % 4
                for ff in range(2):
                    fc = 2 * fp + ff
                    k = fc % 2
                    pg = bank()
                    pu = bank()
                    for c in range(16):
                        mk.op("pe", lambda e, c=c, pg=pg, sg=sg, ff=ff: e.matmul(
                            ps[pg][:, :], lhsT=W16[sg][:, c, ff * 128:(ff + 1) * 128], rhs=y2T[:, c, :],
                            start=(c == 0), stop=(c == 15)), reads=[b_w16[sg], b_y2[c]], writes=[psb[pg]])
                    for c in range(16):
                        mk.op("pe", lambda e, c=c, pu=pu, su=su, ff=ff: e.matmul(
                            ps[pu][:, :], lhsT=W16[su][:, c, ff * 128:(ff + 1) * 128], rhs=y2T[:, c, :],
                            start=(c == 0), stop=(c == 15)), reads=[b_w16[su], b_y2[c]], writes=[psb[pu]])
                    mk.op(CPE, lambda e, k=k, fc=fc: e.tensor_copy(out=gpre[k][:, 0:2], in_=GC[:, fc, :]),
                          reads=[b_gc[fc]], writes=[b_gpre[k]])
                    mk.op("act", act_copy(gpre[k][:, 2:TG + 2], ps[pg][:, :]), reads=[psb[pg]], writes=[b_gpre[k]])
                    mk.op("act", lambda e, k=k, fc=fc, pg=pg: e.activation(
                        out=cv[k][:], in_=ps[pg][:, :], func=AF.Identity, scale=fv[:, fc, 2:3], bias=fv[:, fc, 3:4]),
                        reads=[psb[pg], b_c], writes=[b_cv[k]])
                    mk.op(CPE, lambda e, k=k, fc=fc: e.tensor_copy(out=GC[:, fc, :], in_=gpre[k][:, TG:TG + 2]),
                          reads=[b_gpre[k]], writes=[b_gc[fc]])
                    for kk in (1, 0):
                        mk.op("dve", lambda e, k=k, fc=fc, kk=kk: e.scalar_tensor_tensor(
                            out=cv[k][:], in0=gpre[k][:, kk:kk + TG], scalar=fv[:, fc, kk:kk + 1], in1=cv[k][:],
                            op0=ALU.mult, op1=ALU.add), reads=[b_gpre[k], b_cv[k], b_c], writes=[b_cv[k]])
                    mk.op("act", lambda e, k=k: e.activation(out=ge[k][:], in_=cv[k][:], func=AF.Gelu_apprx_tanh),
                          reads=[b_cv[k]], writes=[b_ge[k]])
                    mk.op("dve", lambda e, k=k, fc=fc, pu=pu: e.tensor_tensor(out=actT[:, fc, :], in0=ps[pu][:, :],
                                                                              in1=ge[k][:], op=ALU.mult),
                          reads=[psb[pu], b_ge[k]], writes=[b_act[fc]])
                i16 += 2
            if P4S <= 4:
                break
            for dc in range(16):
                needd(idn)
                slot = idn % 2
                pb = bank()
                for fc in range(NFC):
                    mk.op("pe", lambda e, fc=fc, pb=pb, slot=slot: e.matmul(
                        ps[pb][:, :], lhsT=WD[slot][:, fc, :], rhs=actT[:, fc, :], start=(fc == 0),
                        stop=(fc == NFC - 1)), reads=[b_wd[slot], b_act[fc]], writes=[psb[pb]])
                mk.op("dve", lambda e, dc=dc, pb=pb: e.tensor_tensor(out=hx[:, dc, :], in0=ps[pb][:, :],
                                                                    in1=hx[:, dc, :], op=ALU.add),
                      reads=[psb[pb], b_hx[dc]], writes=[b_hx[dc]])
                idn += 1
            if P4S <= 5:
                break
            norm_stats(D)
            for c in range(16):
                mk.op("dve", lambda e, c=c: e.scalar_tensor_tensor(out=hx[:, c, :], in0=hx[:, c, :],
                                                                   scalar=gfin[:, c:c + 1], in1=rstd[:],
                                                                   op0=ALU.mult, op1=ALU.mult),
                      reads=[b_hx[c], b_rstd, b_c], writes=[b_hx[c]])
            osrc = P.outT[s].rearrange("(c p) t -> p c t", p=128)
            for hf in range(2):
                cs = slice(hf * 8, hf * 8 + 8)
                mk.dma("pool", osrc[:, cs, tsl], hx[:, cs, :], reads=b_hx[hf * 8:hf * 8 + 8], writes=[Buf()])
        mk.flush()
```

```python
import math
from contextlib import ExitStack

import numpy as np
import ml_dtypes

import concourse.bass as bass
import concourse.mybir as mybir
from concourse.bass_utils import run_bass_kernel_spmd

F32 = mybir.dt.float32
BF16 = mybir.dt.bfloat16
AF = mybir.ActivationFunctionType
ALU = mybir.AluOpType

N_CORES = 8
D = 2048
S = 2048
NSEQ = 2
NH = 16
NG = 4
HD = 64
INW = 4656
DFF = 5632
NFC = DFF // 128
NCMP = 127
EPS = 1e-6
NEG = -30000.0


class Buf:
    __slots__ = ("name", "w", "r")

    def __init__(self, name=""):
        self.name = name
        self.w = {}
        self.r = {}


def bufs(n, name=""):
    return [Buf(f"{name}{i}") for i in range(n)]


class MK:
    ENG = ("pe", "act", "dve", "pool", "sp")

    def __init__(self, nc, es):
        self.nc = nc
        self.q = {e: [] for e in self.ENG}
        self.semh = {}
        self.prog = {}
        for e in ("pe", "act", "dve", "pool"):
            h = es.enter_context(nc.semaphore("prog_" + e))
            self.prog[e] = [h, 0]
            self.semh[("p", e)] = h
        self.seen = {e: {} for e in self.ENG}
        self.dsem = {}
        for qn, n in (("sp", 10), ("pool", 6), ("act", 2), ("cast", 8)):
            lst = []
            for i in range(n):
                h = es.enter_context(nc.semaphore(f"d_{qn}{i}"))
                lst.append([h, 0])
                self.semh[("d", qn, i)] = h
            self.dsem[qn] = lst
        self.drr = {qn: 0 for qn in self.dsem}
        self.ninstr = {e: 0 for e in self.ENG}

    def _wait(self, eng, key, v):
        if eng == "pe" and key == ("p", "pe"):
            return
        seen = self.seen[eng]
        if seen.get(key, 0) < v:
            seen[key] = v
            h = self.semh[key]
            self.q[eng].append(lambda e, h=h, v=v: e.wait_ge(h, v))
            self.ninstr[eng] += 1

    def _waits(self, eng, reads, writes):
        need = {}
        for b in reads:
            for k, v in b.w.items():
                if need.get(k, 0) < v:
                    need[k] = v
        for b in writes:
            for k, v in b.w.items():
                if need.get(k, 0) < v:
                    need[k] = v
            for k, v in b.r.items():
                if need.get(k, 0) < v:
                    need[k] = v
        for k, v in need.items():
            self._wait(eng, k, v)

    def op(self, eng, fn, reads=(), writes=()):
        self._waits(eng, reads, writes)
        p = self.prog[eng]
        p[1] += 1
        v = p[1]
        key = ("p", eng)
        h = p[0]
        self.q[eng].append(lambda e, fn=fn, h=h: fn(e).then_inc(h, 1))
        self.ninstr[eng] += 1
        for b in reads:
            if b.r.get(key, 0) < v:
                b.r[key] = v
        for b in writes:
            b.w = {key: v}
            b.r = {}

    def dma(self, qn, out, in_, reads=(), writes=(), sems=None):
        self._waits(qn, reads, writes)
        sn = sems or qn
        lst = self.dsem[sn]
        i = self.drr[sn]
        self.drr[sn] = (i + 1) % len(lst)
        s = lst[i]
        key = ("d", sn, i)
        if s[1] > 0:
            self._wait(qn, key, s[1])
        s[1] += 16
        v = s[1]
        h = s[0]
        self.q[qn].append(lambda e, h=h, out=out, in_=in_: e.dma_start(out=out, in_=in_).then_inc(h, 16))
        self.ninstr[qn] += 1
        for b in reads:
            if b.r.get(key, 0) < v:
                b.r[key] = v
        for b in writes:
            b.w = {key: v}
            b.r = {}

    def barrier(self):
        for eng in self.ENG:
            for e2, p in self.prog.items():
                if p[1] > 0:
                    if eng == e2 and eng == "pe":
                        continue
                    seen = self.seen[eng]
                    key = ("p", e2)
                    if seen.get(key, 0) < p[1]:
                        seen[key] = p[1]
                        self.q[eng].append(lambda e, h=p[0], v=p[1]: e.wait_ge(h, v))
            for qn, lst in self.dsem.items():
                if qn == "cast":
                    continue
                for i, s in enumerate(lst):
                    if s[1] > 0:
                        key = ("d", qn, i)
                        seen = self.seen[eng]
                        if seen.get(key, 0) < s[1]:
                            seen[key] = s[1]
                            self.q[eng].append(lambda e, h=s[0], v=s[1]: e.wait_ge(h, v))

    def flush(self, final=False):
        nc = self.nc
        if final:
            for qn, lst in self.dsem.items():
                for i, s in enumerate(lst):
                    if s[1] > 0:
                        self._wait("pool" if qn == "cast" else qn, ("d", qn, i), s[1])
        q = self.q
        with nc.Block() as block:
            @block.tensor
            def _(e):
                for f in q["pe"]:
                    f(e)

            @block.scalar
            def _(e):
                for f in q["act"]:
                    f(e)

            @block.vector
            def _(e):
                for f in q["dve"]:
                    f(e)

            @block.gpsimd
            def _(e):
                for f in q["pool"]:
                    f(e)

            @block.sync
            def _(e):
                for f in q["sp"]:
                    f(e)
        self.q = {e: [] for e in self.ENG}


def t5_bucket_np(dist):
    n = np.maximum(dist, 0)
    max_exact = 16
    nf = np.maximum(n, 1).astype(np.float32)
    large = max_exact + (np.log(nf / np.float32(max_exact)) / np.float32(math.log(128 / max_exact))
                         * np.float32(32 - max_exact)).astype(np.int32)
    large = np.minimum(large, 31)
    return np.where(n < max_exact, n, large)


GW = 384


def host_constants():
    c = {}
    c["ident_bf"] = np.eye(128, dtype=np.float32).astype(ml_dtypes.bfloat16)
    c["ones_bf"] = np.ones((128, 128), dtype=np.float32).astype(ml_dtypes.bfloat16)
    n = np.arange(GW)
    bk = t5_bucket_np(n - 127)
    oh = np.zeros((32, GW), np.float32)
    oh[bk, n] = 1.0
    oh[31, :] -= 1.0
    c["oh"] = oh
    j = np.arange(128)[:, None]
    i = np.arange(128)[None, :]
    c["mask_d0"] = (i >= j).astype(np.float32)
    c["mask_d4"] = (j > i).astype(np.float32)
    e = np.zeros((32, S), np.float32)
    e[np.arange(S) // 64, np.arange(S)] = 1.0
    c["e_rows"] = e.astype(ml_dtypes.bfloat16)
    cc = np.arange(NCMP)[:, None]
    jj = np.arange(32)[None, :]
    lo = np.maximum(cc * 16, jj * 64)
    hi = np.minimum(cc * 16 + 32, (jj + 1) * 64)
    c["overlap"] = (np.maximum(hi - lo, 0) / 32.0).astype(np.float32)
    t = np.arange(S)[:, None]
    jb = np.arange(32)[None, :]
    cur = t // 64
    valid = jb <= cur
    forced = ((jb == 0) | (jb == cur) | (jb == cur - 1))
    val = valid.astype(np.float32)
    add = np.where(valid, 1000.0 * forced, -1.0).astype(np.float32)
    c["sel_val"] = np.ascontiguousarray(val.reshape(16, 128, 32).transpose(1, 0, 2))
    c["sel_add"] = np.ascontiguousarray(add.reshape(16, 128, 32).transpose(1, 0, 2))
    zc = np.zeros((16, 272), np.float32)
    zc[np.arange(16), np.arange(16) + 128] = 1.0
    c["zc"] = zc.astype(ml_dtypes.bfloat16)
    k = np.arange(16)[:, None]
    dist = i - 16 * (k - 8) - 31
    c["cmp_valid"] = (dist >= 0).astype(np.float32)
    oh2 = np.zeros((32, 16 * 128), np.float32)
    bk2 = t5_bucket_np(dist.reshape(-1))
    oh2[bk2, np.arange(16 * 128)] = 1.0
    oh2[31, :] -= 1.0
    c["oh_cmp"] = oh2
    return c


def dram_in(nc, name, shape, dt=F32):
    return nc.dram_tensor(name, list(shape), dt, kind="ExternalInput").ap()


class Prog:
    pass


def build(phases=("p0", "p1", "p2", "p3", "p4"), debug=False, nseq=NSEQ):
    nc = bass.Bass("TRN2", target_bir_lowering=False)
    P = Prog()
    P.nc = nc
    P.nseq = nseq
    kind_scr = "ExternalOutput" if debug else "Internal"

    def scr(name, shape, dt):
        return nc.dram_tensor(name, list(shape), dt, kind=kind_scr).ap()

    I = {}
    I["xT"] = dram_in(nc, "xT", [NSEQ, D, S])
    I["w_in"] = dram_in(nc, "w_in", [D, INW])
    I["w_out"] = dram_in(nc, "w_out", [D, D])
    I["w_g"] = dram_in(nc, "w_g", [D, DFF])
    I["w_u"] = dram_in(nc, "w_u", [D, DFF])
    I["w_d"] = dram_in(nc, "w_d", [DFF, D])
    I["g_mix"] = dram_in(nc, "g_mix", [128, 16])
    I["g_ffn"] = dram_in(nc, "g_ffn", [128, 16])
    I["g_fin"] = dram_in(nc, "g_fin", [128, 16])
    I["b_gate_bc"] = dram_in(nc, "b_gate_bc", [128, 48])
    I["g_attn_bc"] = dram_in(nc, "g_attn_bc", [128, 1024])
    I["rnn_vec"] = dram_in(nc, "rnn_vec", [128, 8, 10])
    I["rg_a_w"] = dram_in(nc, "rg_a_w", [16, 64, 64])
    I["rg_x_w"] = dram_in(nc, "rg_x_w", [16, 64, 64])
    I["ffn_vec"] = dram_in(nc, "ffn_vec", [128, NFC, 4])
    I["rel_bias"] = dram_in(nc, "rel_bias", [32, 16])
    I["cmp_w1"] = dram_in(nc, "cmp_w1", [2, 64, 32, 256])
    I["cmp_w2"] = dram_in(nc, "cmp_w2", [2, 128, 2, 64])
    I["cmp_peT"] = dram_in(nc, "cmp_peT", [2, 64, 32])
    I["ident_bf"] = dram_in(nc, "ident_bf", [128, 128], BF16)
    I["ones_bf"] = dram_in(nc, "ones_bf", [128, 128], BF16)
    I["oh"] = dram_in(nc, "oh", [32, GW])
    I["mask_d0"] = dram_in(nc, "mask_d0", [128, 128])
    I["mask_d4"] = dram_in(nc, "mask_d4", [128, 128])
    I["e_rows"] = dram_in(nc, "e_rows", [32, S], BF16)
    I["overlap"] = dram_in(nc, "overlap", [NCMP, 32])
    I["sel_val"] = dram_in(nc, "sel_val", [128, 16, 32])
    I["sel_add"] = dram_in(nc, "sel_add", [128, 16, 32])
    I["zc"] = dram_in(nc, "zc", [16, 272], BF16)
    I["cmp_valid"] = dram_in(nc, "cmp_valid", [16, 128])
    P.I = I

    outT = nc.dram_tensor("outT", [NSEQ, D, S], F32, kind="ExternalOutput").ap()
    P.outT = outT

    W = {}
    W["w_in"] = scr("w_in_b", [D, INW], BF16)
    W["w_out"] = scr("w_out_b", [D, D], BF16)
    W["w_g"] = scr("w_g_b", [D, DFF], BF16)
    W["w_u"] = scr("w_u_b", [D, DFF], BF16)
    W["w_d"] = scr("w_d_b", [DFF, D], BF16)
    P.W = W
    X = {}
    X["QT"] = scr("QT", [NSEQ, 1024, S], BF16)
    X["KC"] = scr("KC", [NSEQ, 512, S], BF16)
    X["KS"] = scr("KS", [NSEQ, 256, S], BF16)
    X["KW"] = scr("KW", [NSEQ, 256, S], BF16)
    X["RX"] = scr("RX", [NSEQ, 1024, S], F32)
    X["RY"] = scr("RY", [NSEQ, 1024, S], F32)
    X["VSW"] = scr("VSW", [NSEQ, 16, 128, 2, 4, 65], BF16)
    X["GATE"] = scr("GATE", [NSEQ, 16, 128, 48], F32)
    X["MIXR"] = scr("MIXR", [NSEQ, 1024, S], BF16)
    X["RSTDR"] = scr("RSTDR", [NSEQ, 128, S], F32)
    X["OATT"] = scr("OATT", [NSEQ, 16, 128, 1024], F32)
    X["MIXA"] = scr("MIXA", [NSEQ, 1024, S], BF16)
    X["RTAB"] = scr("RTAB", [16, 128, GW], F32)
    if debug:
        X["DBGDN"] = scr("DBGDN", [128, 16, 2, 128], BF16)
        X["DBGMB"] = scr("DBGMB", [16, 16, 128], BF16)
    P.X = X

    es = ExitStack()
    with es:
        mk = MK(nc, es)
        P.mk = mk
        P.wb = {k: Buf("wb_" + k) for k in W}
        P.xb = {k: [Buf(f"{k}{s}") for s in range(NSEQ)] for k in X}

        cst = ExitStack()
        with cst:
            C = {}
            C["ident"] = cst.enter_context(nc.sbuf_tensor("c_ident", [128, 128], BF16))
            C["ones"] = cst.enter_context(nc.sbuf_tensor("c_ones", [128, 128], BF16))
            P.C = C
            P.cb = Buf("consts")
            mk.dma("sp", C["ident"][:], I["ident_bf"][:, :], writes=[P.cb])
            mk.dma("sp", C["ones"][:], I["ones_bf"][:, :], writes=[P.cb])
            P.cb.w = dict(P.cb.w)

            if "p0" in phases:
                phase0(P)
            if "p1" in phases:
                for s in range(nseq):
                    phase1(P, s)
            if "p2" in phases:
                for s in range(nseq):
                    phase2(P, s)
            if "p3" in phases:
                phase3_setup(P)
                for s in range(nseq):
                    phase3(P, s)
            if "p4" in phases:
                phase4(P, nseq)
            mk.flush(final=True)
    return nc


def phase0(P):
    mk, I, W = P.mk, P.I, P.W
    P.cast_jobs = []
    for name, rows, cols in (("w_in", D, INW), ("w_out", D, D), ("w_g", D, DFF), ("w_u", D, DFF), ("w_d", DFF, D)):
        ncp = -(-cols // 2048)
        while cols % ncp:
            ncp += 1
        cw = cols // ncp
        rb = 512
        for r0 in range(0, rows, rb):
            for cp in range(ncp):
                job = (name, W[name][r0:r0 + rb, cp * cw:(cp + 1) * cw], I[name][r0:r0 + rb, cp * cw:(cp + 1) * cw])
                if name == "w_in":
                    issue_cast(P, job)
                else:
                    P.cast_jobs.append(job)
    mk.flush()


def issue_cast(P, job, gate=None):
    mk = P.mk
    name, dst, src = job
    if gate is not None:
        mk._waits("pool", [gate], [])
    b = Buf()
    mk.dma("pool", dst, src, writes=[b], sems="cast")
    evs = P.wb[name].w
    for k, v in b.w.items():
        evs[k] = max(evs.get(k, 0), v)


def issue_casts(P, n, gate=None):
    for _ in range(n):
        if P.cast_jobs:
            issue_cast(P, P.cast_jobs.pop(0), gate)


def act_copy(out, in_, scale=1.0):
    return lambda e: e.activation(out=out, in_=in_, func=AF.Copy, scale=float(scale))


def dve_scale(out, in_, scale=1.0):
    return lambda e: e.tensor_scalar(out=out, in0=in_, scalar1=float(scale), scalar2=None, op0=ALU.mult)


def evac(mk, idx, out, in_, reads, writes, scale=1.0):
    if idx % 2 == 0:
        mk.op("act", act_copy(out, in_, scale), reads=reads, writes=writes)
    else:
        mk.op("dve", dve_scale(out, in_, scale), reads=reads, writes=writes)


def rms_rstd(mk, ps_ap, ps_buf, rt_ap, rt_buf, rstd_ap, rstd_buf, n):
    mk.op("act", lambda e: e.activation(out=rt_ap, in_=ps_ap, func=AF.Sqrt, scale=1.0 / n, bias=P_EPS[0]),
          reads=[ps_buf, P_EPS[1]], writes=[rt_buf])
    mk.op("dve", lambda e: e.reciprocal(out=rstd_ap, in_=rt_ap), reads=[rt_buf], writes=[rstd_buf])


P_EPS = [None, None]


def phase1(P, s):
    nc, mk, I, W, X, C = P.nc, P.mk, P.I, P.W, P.X, P.C
    mk.barrier()
    with ExitStack() as ts:
        def sb(name, shape, dt):
            return ts.enter_context(nc.sbuf_tensor(f"p1_{s}_{name}", shape, dt))

        yT = sb("yT", [128, 16, S], BF16)
        xin = [sb(f"xin{i}", [128, 16, 256], F32) for i in range(2)]
        sq = [sb(f"sq{i}", [128, 16, 256], BF16) for i in range(2)]
        rt = sb("rt", [128, 256], F32)
        rstd = sb("rstd", [128, 256], F32)
        gmix = sb("gmix", [128, 16], F32)
        epst = sb("eps", [128, 1], F32)
        WB = [sb(f"WB{i}", [128, 16, 512], BF16) for i in range(2)]
        Wtok = sb("Wtok", [128, 16, 560], BF16)
        stb = [sb(f"stb{i}", [128, S], BF16) for i in range(2)]
        stf = [sb(f"stf{i}", [128, S], F32) for i in range(2)]
        Vst = [sb(f"Vst{i}", [128, 2, 4, 65], BF16) for i in range(2)]
        gtmp = [sb(f"gtmp{i}", [128, 48], F32) for i in range(2)]
        gst = [sb(f"gst{i}", [128, 48], F32) for i in range(2)]
        bgate = sb("bgate", [128, 48], F32)
        ps = [ts.enter_context(nc.psum_tensor(f"p1_{s}_ps{i}", [128, 512], F32)) for i in range(8)]
        psb = bufs(8, "ps")

        b_small = Buf("small")
        mk.dma("sp", gmix[:], I["g_mix"][:, :], writes=[b_small])
        b_bg = Buf("bgate")
        mk.dma("sp", bgate[:], I["b_gate_bc"][:, :], writes=[b_bg])
        b_eps = Buf("eps")
        mk.op("dve", lambda e: e.memset(epst[:], EPS), writes=[b_eps])
        P_EPS[0] = epst[:]
        P_EPS[1] = b_eps
        b_vst = bufs(2, "vst")
        for i in range(2):
            mk.op("dve", lambda e, i=i: e.memset(Vst[i][:], 1.0), writes=[b_vst[i]])

        b_xin = bufs(2, "xin")
        b_sq = bufs(2, "sq")
        b_rt = Buf("rt")
        b_rstd = Buf("rstd")
        b_yT = bufs(8, "yT")
        xsrc = I["xT"][s].rearrange("(c p) t -> p c t", p=128)
        pcnt = 0
        for j in range(8):
            k = j % 2
            t0 = j * 256
            mk.dma("sp", xin[k][:], xsrc[:, :, t0:t0 + 256], writes=[b_xin[k]])
            mk.op("act", lambda e, k=k: e.activation(out=sq[k][:], in_=xin[k][:], func=AF.Square),
                  reads=[b_xin[k]], writes=[b_sq[k]])
            pb = 6 + (j % 2)
            for c in range(16):
                mk.op("pe", lambda e, k=k, c=c, pb=pb: e.matmul(ps[pb][:, 0:256], lhsT=C["ones"][:], rhs=sq[k][:, c, :],
                                                                 start=(c == 0), stop=(c == 15)),
                      reads=[b_sq[k], P.cb], writes=[psb[pb]])
            rms_rstd(mk, ps[pb][:, 0:256], psb[pb], rt[:], b_rt, rstd[:], b_rstd, float(D))
            for c in range(16):
                mk.op("dve", lambda e, k=k, c=c, t0=t0: e.scalar_tensor_tensor(
                    out=yT[:, c, t0:t0 + 256], in0=xin[k][:, c, :], scalar=gmix[:, c:c + 1], in1=rstd[:],
                    op0=ALU.mult, op1=ALU.mult), reads=[b_xin[k], b_rstd, b_small], writes=[b_yT[j]])

        wcols = [[(0, 512)], [(512, 1024)], [(1024, 1536)], [(1536, 1792), (2048, 2304)],
                 [(2608, 3120)], [(3120, 3632)], [(3632, 4144)], [(4144, 4656)]]
        b_WB = bufs(2, "WB")
        wsrc = W["w_in"].rearrange("(c p) n -> p c n", p=128)

        def load_w(w):
            k = w % 2
            o = 0
            for (c0, c1) in wcols[w]:
                mk.dma("sp", WB[k][:, :, o:o + (c1 - c0)], wsrc[:, :, c0:c1], reads=[P.wb["w_in"]], writes=[b_WB[k]])
                o += c1 - c0

        b_Wtok = Buf("Wtok")
        b_stb = [bufs(4, f"stb{i}_") for i in range(2)]
        b_stf = [bufs(4, f"stf{i}_") for i in range(2)]
        load_w(0)
        nb = 0
        nbf = 0
        ecnt = 0
        for w in range(8):
            if w + 1 < 8:
                load_w(w + 1)
            elif True:
                o = 0
                for (c0, c1) in ((1792, 2048), (2304, 2560), (2560, 2608)):
                    mk.dma("sp", Wtok[:, :, o:o + (c1 - c0)], wsrc[:, :, c0:c1], reads=[P.wb["w_in"]], writes=[b_Wtok])
                    o += c1 - c0
            k = w % 2
            for m in range(4):
                ch = 4 * w + m
                isf = ch >= 16
                if isf:
                    sidx = nbf % 2
                    nbf += 1
                    stage, sbufs_ = stf[sidx], b_stf[sidx]
                else:
                    sidx = nb % 2
                    nb += 1
                    stage, sbufs_ = stb[sidx], b_stb[sidx]
                for tg in range(4):
                    pb = pcnt % 6
                    pcnt += 1
                    for c in range(16):
                        mk.op("pe", lambda e, k=k, c=c, m=m, tg=tg, pb=pb: e.matmul(
                            ps[pb][:], lhsT=WB[k][:, c, m * 128:(m + 1) * 128], rhs=yT[:, c, tg * 512:(tg + 1) * 512],
                            start=(c == 0), stop=(c == 15)),
                            reads=[b_WB[k], b_yT[2 * tg], b_yT[2 * tg + 1]], writes=[psb[pb]])
                    evac(mk, ecnt, stage[:, tg * 512:(tg + 1) * 512], ps[pb][:], [psb[pb]], [sbufs_[tg]],
                         scale=(0.125 if ch < 8 else 1.0))
                    ecnt += 1
                if ch < 8:
                    dst = X["QT"][s, ch * 128:(ch + 1) * 128, :]
                    db = P.xb["QT"][s]
                elif ch < 12:
                    dst = X["KC"][s, (ch - 8) * 128:(ch - 7) * 128, :]
                    db = P.xb["KC"][s]
                elif ch < 14:
                    dst = X["KS"][s, (ch - 12) * 128:(ch - 11) * 128, :]
                    db = P.xb["KS"][s]
                elif ch < 16:
                    dst = X["KW"][s, (ch - 14) * 128:(ch - 13) * 128, :]
                    db = P.xb["KW"][s]
                elif ch < 24:
                    dst = X["RX"][s, (ch - 16) * 128:(ch - 15) * 128, :]
                    db = P.xb["RX"][s]
                else:
                    dst = X["RY"][s, (ch - 24) * 128:(ch - 23) * 128, :]
                    db = P.xb["RY"][s]
                dmab = Buf()
                mk.dma("sp", dst, stage[:], reads=sbufs_, writes=[dmab])
                for kk, vv in dmab.w.items():
                    db.w[kk] = max(db.w.get(kk, 0), vv)
            issue_casts(P, 3, gate=sbufs_[3])

        b_gtmp = bufs(2, "gtmp")
        b_gst = bufs(2, "gst")
        for tt in range(16):
            k = tt % 2
            pv = pcnt % 6
            pcnt += 1
            pg = 6 + (tt % 2)
            for c in range(16):
                lhs = yT[:, c, tt * 128:(tt + 1) * 128]
                mk.op("pe", lambda e, c=c, pv=pv, lhs=lhs: e.matmul(ps[pv][:], lhsT=lhs, rhs=Wtok[:, c, 0:512],
                                                                     start=(c == 0), stop=(c == 15)),
                      reads=[b_Wtok, b_yT[tt // 2]], writes=[psb[pv]])
                mk.op("pe", lambda e, c=c, pg=pg, lhs=lhs: e.matmul(ps[pg][:, 0:48], lhsT=lhs, rhs=Wtok[:, c, 512:560],
                                                                     start=(c == 0), stop=(c == 15)),
                      reads=[b_Wtok, b_yT[tt // 2]], writes=[psb[pg]])
            evac(mk, tt, Vst[k][:, :, :, 0:64], ps[pv][:].rearrange("p (a g d) -> p a g d", a=2, g=4),
                 [psb[pv]], [b_vst[k]])
            mk.op("dve", lambda e, k=k, pg=pg: e.tensor_tensor(out=gtmp[k][:], in0=ps[pg][:, 0:48], in1=bgate[:],
                                                                op=ALU.add),
                  reads=[psb[pg], b_bg], writes=[b_gtmp[k]])
            mk.op("act", lambda e, k=k: e.activation(out=gst[k][:], in_=gtmp[k][:], func=AF.Sigmoid),
                  reads=[b_gtmp[k]], writes=[b_gst[k]])
            for (dst, src, sbuf_, key) in ((X["VSW"][s, tt], Vst[k][:], b_vst[k], "VSW"),
                                           (X["GATE"][s, tt], gst[k][:], b_gst[k], "GATE")):
                dmab = Buf()
                mk.dma("sp", dst, src, reads=[sbuf_], writes=[dmab])
                db = P.xb[key][s]
                for kk, vv in dmab.w.items():
                    db.w[kk] = max(db.w.get(kk, 0), vv)
        if s == P.nseq - 1:
            issue_casts(P, 1000)
        mk.flush()


def pc(v, nchunk):
    return np.ascontiguousarray(np.asarray(v, np.float32).reshape(nchunk, 128).T)


def make_in_maps(inp, n_cores=N_CORES):
    f = lambda k: np.asarray(inp[k], np.float32)
    shared = {}
    shared["w_in"] = np.ascontiguousarray(f("w_in")[0])
    shared["w_out"] = np.ascontiguousarray(f("w_out")[0])
    shared["w_g"] = np.ascontiguousarray(f("w_ffn_gate")[0])
    shared["w_u"] = np.ascontiguousarray(f("w_ffn_up")[0])
    shared["w_d"] = np.ascontiguousarray(f("w_ffn_down")[0])
    shared["g_mix"] = pc(f("mix_norm_g")[0], 16)
    shared["g_ffn"] = pc(f("ffn_norm_g")[0], 16)
    shared["g_fin"] = pc(f("final_norm_g"), 16)
    shared["b_gate_bc"] = np.ascontiguousarray(np.broadcast_to(f("b_gate")[0][None, :], (128, 48)))
    shared["g_attn_bc"] = np.ascontiguousarray(np.broadcast_to(f("attn_out_g")[0][None, :], (128, 1024)))
    rv = np.zeros((128, 8, 10), np.float32)
    cw = f("rnn_conv_w")[0]
    for k in range(4):
        rv[:, :, k] = pc(cw[k], 8)
    rv[:, :, 4] = pc(f("rnn_conv_b")[0], 8)
    rv[:, :, 5] = pc(f("rg_a_b")[0], 8)
    rv[:, :, 6] = pc(f("rg_x_b")[0], 8)
    rv[:, :, 7] = pc(f("rg_lambda")[0], 8)
    rv[:, :, 8] = pc(f("rnn_out_g")[0], 8)
    shared["rnn_vec"] = rv
    shared["rg_a_w"] = np.ascontiguousarray(f("rg_a_w")[0])
    shared["rg_x_w"] = np.ascontiguousarray(f("rg_x_w")[0])
    fv = np.zeros((128, NFC, 4), np.float32)
    fw = f("ffn_conv_w")[0]
    for k in range(3):
        fv[:, :, k] = pc(fw[k], NFC)
    fv[:, :, 3] = pc(f("ffn_conv_b")[0], NFC)
    shared["ffn_vec"] = fv
    shared["rel_bias"] = np.ascontiguousarray(f("rel_bias"))
    w1 = np.stack([f("cmp_k_w1")[0], f("cmp_v_w1")[0]])
    shared["cmp_w1"] = np.ascontiguousarray(w1.reshape(2, 32, 64, 256).transpose(0, 2, 1, 3))
    w2 = np.stack([f("cmp_k_w2")[0], f("cmp_v_w2")[0]])
    shared["cmp_w2"] = np.ascontiguousarray(w2.reshape(2, 2, 128, 64).transpose(0, 2, 1, 3))
    pe = np.stack([f("cmp_pe_k")[0], f("cmp_pe_v")[0]])
    shared["cmp_peT"] = np.ascontiguousarray(pe.transpose(0, 2, 1))
    shared.update(host_constants())
    x = f("x")
    maps = []
    for c in range(n_cores):
        m = dict(shared)
        m["xT"] = np.ascontiguousarray(x[c * NSEQ:(c + 1) * NSEQ].transpose(0, 2, 1))
        maps.append(m)
    return maps


_NC_CACHE = {}


def kernel(**inputs):
    if "nc" not in _NC_CACHE:
        _NC_CACHE["nc"] = build()
    nc = _NC_CACHE["nc"]
    maps = make_in_maps(inputs)
    res = run_bass_kernel_spmd(nc, maps, core_ids=list(range(N_CORES)))
    outs = [np.asarray(r["outT"]).transpose(0, 2, 1) for r in res.results]
    return np.ascontiguousarray(np.concatenate(outs, axis=0).astype(np.float32))


import os
P2E = os.environ.get("P2E", "dve")


def phase2(P, s):
    nc, mk, I, W, X, C = P.nc, P.mk, P.I, P.W, P.X, P.C
    mk.barrier()
    with ExitStack() as ts:
        def sb(name, shape, dt):
            return ts.enter_context(nc.sbuf_tensor(f"p2_{s}_{name}", shape, dt))

        rv = sb("rv", [128, 8, 10], F32)
        cl = sb("cl", [128, 8, 2], F32)
        tmp8 = sb("tmp8", [128, 8], F32)
        cst1 = sb("cst1", [128, 2], F32)
        BDa = sb("BDa", [128, 8, 128], BF16)
        BDx = sb("BDx", [128, 8, 128], BF16)
        rxp = [sb(f"rxp{i}", [128, 3 + S], F32) for i in range(2)]
        ryt = [sb(f"ryt{i}", [128, S], F32) for i in range(3)]
        xr_ = [sb(f"xr{i}", [128, S], F32) for i in range(2)]
        xrb_ = [sb(f"xrb{i}", [128, S], BF16) for i in range(2)]
        rr_ = [sb(f"rr{i}", [128, S], F32) for i in range(2)]
        gi_ = [sb(f"gi{i}", [128, S], F32) for i in range(2)]
        aa_ = [sb(f"aa{i}", [128, S], F32) for i in range(2)]
        mm_ = [sb(f"mm{i}", [128, S], F32) for i in range(2)]
        hh_ = [sb(f"hh{i}", [128, S], F32) for i in range(2)]
        sqo_ = [sb(f"sqo{i}", [128, S], BF16) for i in range(2)]
        mst = [sb(f"mst{i}", [128, S], BF16) for i in range(2)]
        rstdr = sb("rstdr", [128, S], F32)
        ps = [ts.enter_context(nc.psum_tensor(f"p2_{s}_ps{i}", [128, 512], F32)) for i in range(8)]
        psb = bufs(8, "ps")

        b_rv = Buf("rv")
        mk.dma("sp", rv[:], I["rnn_vec"][:, :, :], writes=[b_rv])
        b_cst = Buf("cst")
        mk.op("dve", lambda e: e.memset(cst1[:, 0:1], 1.0), writes=[b_cst])
        mk.op("dve", lambda e: e.memset(cst1[:, 1:2], EPS), writes=[b_cst])
        b_cl = Buf("cl")
        b_t8 = Buf("t8")
        mk.op("act", lambda e: e.activation(out=tmp8[:], in_=rv[:, :, 7], func=AF.Exp, scale=-1.0),
              reads=[b_rv], writes=[b_t8])
        mk.op("act", lambda e: e.activation(out=tmp8[:], in_=tmp8[:], func=AF.Ln, bias=cst1[:, 0:1]),
              reads=[b_t8, b_cst], writes=[b_t8])
        mk.op("dve", lambda e: e.tensor_scalar(out=cl[:, :, 0], in0=tmp8[:], scalar1=-8.0, scalar2=None, op0=ALU.mult),
              reads=[b_t8], writes=[b_cl])
        mk.op("dve", lambda e: e.tensor_scalar(out=cl[:, :, 1], in0=tmp8[:], scalar1=-16.0, scalar2=None, op0=ALU.mult),
              reads=[b_t8], writes=[b_cl])
        b_bd = Buf("bd")
        mk.op("dve", lambda e: e.memset(BDa[:], 0.0), writes=[b_bd])
        mk.op("dve", lambda e: e.memset(BDx[:], 0.0), writes=[b_bd])
        for (bd, key) in ((BDa, "rg_a_w"), (BDx, "rg_x_w")):
            src = I[key].rearrange("(c two) i j -> two i c j", two=2)
            mk.dma("pool", bd[0:64, :, 0:64], src[0], writes=[b_bd])
            mk.dma("pool", bd[64:128, :, 64:128], src[1], writes=[b_bd])
        b_rxp = bufs(2, "rxp")
        b_ry = bufs(3, "ry")
        for i in range(2):
            mk.op("dve", lambda e, i=i: e.memset(rxp[i][:, 0:3], 0.0), writes=[b_rxp[i]])
        b_xr_, b_xrb_, b_rr_, b_gi_, b_aa_, b_mm_, b_hh_, b_sqo_ = (bufs(2, n) for n in
                                                                      ("xr", "xrb", "rr", "gi", "aa", "mm", "hh", "sqo"))
        b_mst = bufs(2, "mst")

        def load(c):
            k = c % 2
            mk.dma("sp", rxp[k][:, 3:3 + S], X["RX"][s, c * 128:(c + 1) * 128, :], reads=[P.xb["RX"][s]],
                   writes=[b_rxp[k]])
            mk.dma("sp", ryt[c % 3][:], X["RY"][s, c * 128:(c + 1) * 128, :], reads=[P.xb["RY"][s]],
                   writes=[b_ry[c % 3]])

        load(0)

        def front(c):
            k = c % 2
            if c + 1 < 8:
                load(c + 1)
            xr, xrb, rr, gi, aa, mm, hh, sqo = (t_[k] for t_ in (xr_, xrb_, rr_, gi_, aa_, mm_, hh_, sqo_))
            b_xr, b_xrb, b_rr, b_gi, b_aa, b_mm, b_hh, b_sqo = (t_[k] for t_ in (b_xr_, b_xrb_, b_rr_, b_gi_, b_aa_,
                                                                                  b_mm_, b_hh_, b_sqo_))
            mk.op("act", lambda e, k=k, c=c: e.activation(out=xr[:], in_=rxp[k][:, 3:3 + S], func=AF.Identity,
                                                          scale=rv[:, c, 3:4], bias=rv[:, c, 4:5]),
                  reads=[b_rxp[k], b_rv], writes=[b_xr])
            for kk in range(3):
                mk.op("dve", lambda e, k=k, c=c, kk=kk: e.scalar_tensor_tensor(
                    out=xr[:], in0=rxp[k][:, kk:kk + S], scalar=rv[:, c, kk:kk + 1], in1=xr[:],
                    op0=ALU.mult, op1=ALU.add), reads=[b_rxp[k], b_rv, b_xr], writes=[b_xr])
            mk.op("act", act_copy(xrb[:], xr[:]), reads=[b_xr], writes=[b_xrb])
            for tg in range(4):
                sl = slice(tg * 512, (tg + 1) * 512)
                pr = tg % 2
                pg = 2 + tg % 2
                mk.op("pe", lambda e, c=c, sl=sl, pr=pr: e.matmul(ps[pr][:], lhsT=BDa[:, c, :], rhs=xrb[:, sl],
                                                                 start=True, stop=True),
                      reads=[b_bd, b_xrb], writes=[psb[pr]])
                mk.op("pe", lambda e, c=c, sl=sl, pg=pg: e.matmul(ps[pg][:], lhsT=BDx[:, c, :], rhs=xrb[:, sl],
                                                                 start=True, stop=True),
                      reads=[b_bd, b_xrb], writes=[psb[pg]])
                mk.op("act", lambda e, c=c, sl=sl, pr=pr: e.activation(out=rr[:, sl], in_=ps[pr][:], func=AF.Sigmoid,
                                                                      bias=rv[:, c, 5:6]),
                      reads=[psb[pr], b_rv], writes=[b_rr])
                mk.op("act", lambda e, c=c, sl=sl, pg=pg: e.activation(out=gi[:, sl], in_=ps[pg][:], func=AF.Sigmoid,
                                                                      bias=rv[:, c, 6:7]),
                      reads=[psb[pg], b_rv], writes=[b_gi])
            mk.op("act", lambda e, c=c: e.activation(out=aa[:], in_=rr[:], func=AF.Exp, scale=cl[:, c, 0:1]),
                  reads=[b_rr, b_cl], writes=[b_aa])
            mk.op("act", lambda e, c=c: e.activation(out=mm[:], in_=rr[:], func=AF.Exp, scale=cl[:, c, 1:2]),
                  reads=[b_rr, b_cl], writes=[b_mm])
            mk.op("act", lambda e: e.activation(out=mm[:], in_=mm[:], func=AF.Sqrt, scale=-1.0, bias=cst1[:, 0:1]),
                  reads=[b_mm, b_cst], writes=[b_mm])
            mk.op("act", lambda e, c=c: e.activation(out=ryt[c % 3][:], in_=ryt[c % 3][:], func=AF.Gelu_apprx_tanh),
                  reads=[b_ry[c % 3]], writes=[b_ry[c % 3]])

        def back(c):
            k = c % 2
            xr, xrb, rr, gi, aa, mm, hh, sqo = (t_[k] for t_ in (xr_, xrb_, rr_, gi_, aa_, mm_, hh_, sqo_))
            b_xr, b_xrb, b_rr, b_gi, b_aa, b_mm, b_hh, b_sqo = (t_[k] for t_ in (b_xr_, b_xrb_, b_rr_, b_gi_, b_aa_,
                                                                                  b_mm_, b_hh_, b_sqo_))
            mk.op(P2E, lambda e: e.tensor_tensor(out=gi[:], in0=gi[:], in1=xr[:], op=ALU.mult),
                  reads=[b_gi, b_xr], writes=[b_gi])
            mk.op(P2E, lambda e: e.tensor_tensor(out=gi[:], in0=gi[:], in1=mm[:], op=ALU.mult),
                  reads=[b_gi, b_mm], writes=[b_gi])
            mk.op("dve", lambda e: e.tensor_tensor_scan(out=hh[:], data0=aa[:], data1=gi[:], initial=0.0,
                                                        op0=ALU.mult, op1=ALU.add),
                  reads=[b_aa, b_gi], writes=[b_hh])
            mk.op("dve", lambda e, c=c: e.tensor_tensor(out=hh[:], in0=hh[:], in1=ryt[c % 3][:], op=ALU.mult),
                  reads=[b_hh, b_ry[c % 3]], writes=[b_hh])
            mk.op("act", lambda e: e.activation(out=sqo[:], in_=hh[:], func=AF.Square), reads=[b_hh], writes=[b_sqo])
            for tg in range(4):
                sl = slice(tg * 512, (tg + 1) * 512)
                mk.op("pe", lambda e, c=c, sl=sl, tg=tg: e.matmul(ps[4 + tg][:], lhsT=C["ones"][:], rhs=sqo[:, sl],
                                                                 start=(c == 0), stop=(c == 7)),
                      reads=[b_sqo, P.cb], writes=[psb[4 + tg]])
            mk.op("dve", lambda e, k=k, c=c: e.tensor_scalar(out=mst[k][:], in0=hh[:], scalar1=rv[:, c, 8:9],
                                                             scalar2=None, op0=ALU.mult),
                  reads=[b_hh, b_rv], writes=[b_mst[k]])
            dmab = Buf()
            mk.dma("pool", X["MIXR"][s, c * 128:(c + 1) * 128, :], mst[k][:], reads=[b_mst[k]], writes=[dmab])
            db = P.xb["MIXR"][s]
            for kk_, vv in dmab.w.items():
                db.w[kk_] = max(db.w.get(kk_, 0), vv)
        front(0)
        for c in range(8):
            if c + 1 < 8:
                front(c + 1)
            back(c)
        b_rs = Buf("rstdr")
        for tg in range(4):
            sl = slice(tg * 512, (tg + 1) * 512)
            mk.op("act", lambda e, sl=sl, tg=tg: e.activation(out=rstdr[:, sl], in_=ps[4 + tg][:], func=AF.Sqrt,
                                                              scale=1.0 / 1024.0, bias=cst1[:, 1:2]),
                  reads=[psb[4 + tg], b_cst], writes=[b_rs])
        mk.op("dve", lambda e: e.reciprocal(out=rstdr[:], in_=rstdr[:]), reads=[b_rs], writes=[b_rs])
        mk.dma("pool", X["RSTDR"][s], rstdr[:], reads=[b_rs], writes=[P.xb["RSTDR"][s]])
        mk.flush()


def merge_ev(dst_buf, src_buf):
    for kk, vv in src_buf.w.items():
        dst_buf.w[kk] = max(dst_buf.w.get(kk, 0), vv)


def phase3_setup(P):
    pass


def phase3(P, s):
    if s == 0:
        phase3_all(P)


def phase3_all(P):
    nc, mk, I, W, X, C = P.nc, P.mk, P.I, P.W, P.X, P.C
    nseq = P.nseq
    mk.barrier()
    with ExitStack() as ts:
        def sb(name, shape, dt):
            return ts.enter_context(nc.sbuf_tensor(f"p3_{name}", shape, dt))

        DNb = sb("DNb", [128, 16, 2, 128], BF16)
        DN4b = sb("DN4b", [128, 4, 128], BF16)
        Mb = sb("Mb", [16, 16, 128], BF16)
        zc = sb("zc", [16, 272], BF16)
        W1 = sb("W1", [64, 2, 32, 256], BF16)
        W2 = sb("W2", [128, 2, 2, 64], BF16)
        peT = sb("peT", [64, 2, 34], BF16)
        pebias = sb("pebias", [128, 2, 2], F32)
        VAL = sb("VAL", [128, 16, 32], F32)
        ADD = sb("ADD", [128, 16, 32], F32)
        gbc = sb("gbc", [128, 1024], F32)
        KsA = sb("KsA", [96, S], BF16)
        VCA = sb("VCA", [128, 4, 97], BF16)
        NSP = [sb(f"NSP{i}", [128, 96], BF16) for i in range(2)]
        cst1 = sb("cst1", [128, 2], F32)
        ts2 = ExitStack()

        def sb2(name, shape, dt):
            return ts2.enter_context(nc.sbuf_tensor(f"p3t_{name}", shape, dt))

        tab = sb2("tab", [32, 16], F32)
        tabb = [sb2(f"tabb{i}", [32, 128], F32) for i in range(2)]
        oh = sb2("oh", [32, GW], F32)
        rst = [sb2(f"rst{i}", [128, GW], F32) for i in range(2)]
        dn01 = sb2("dn01", [128, 16, 2, 128], F32)
        m0 = sb2("m0", [128, 128], F32)
        negm0 = sb2("negm0", [128, 128], F32)
        m4 = sb2("m4", [128, 128], F32)
        mbf = sb2("mbf", [16, 16, 128], F32)
        cvm = sb2("cvm", [16, 128], F32)
        negcv = sb2("negcv", [16, 128], F32)
        ps = [ts.enter_context(nc.psum_tensor(f"p3_ps{i}", [128, 512], F32)) for i in range(8)]
        psb = bufs(8, "ps")

        b_c = Buf("p3c")
        for (t_, src) in ((tab, I["rel_bias"]), (oh, I["oh"]), (m0, I["mask_d0"]), (m4, I["mask_d4"]),
                          (cvm, I["cmp_valid"]), (zc, I["zc"]), (VAL, I["sel_val"]), (ADD, I["sel_add"]),
                          (gbc, I["g_attn_bc"])):
            b = Buf()
            mk.dma("sp", t_[:], src, writes=[b])
            merge_ev(b_c, b)
        b = Buf()
        mk.dma("sp", KsA[64:96, :], I["e_rows"][:, :], writes=[b])
        merge_ev(b_c, b)
        for i in range(2):
            b = Buf()
            mk.op("dve", lambda e, i=i: e.memset(NSP[i][:], 0.0), writes=[b])
            merge_ev(b_c, b)
        b = Buf()
        mk.op("dve", lambda e: e.memset(cst1[:, 0:1], 1.0), writes=[b])
        mk.op("dve", lambda e: e.memset(cst1[:, 1:2], EPS), writes=[b])
        merge_ev(b_c, b)
        b_vca = Buf("vca")
        mk.op("dve", lambda e: e.memset(VCA[:], 1.0), writes=[b_vca])
        for g in range(4):
            mk.dma("pool", VCA[0:NCMP, g, 65:97], I["overlap"][:, :], writes=[b_vca])
        b_w1 = Buf("w1")
        mk.dma("pool", W1[:, 0], I["cmp_w1"][0], writes=[b_w1])
        mk.dma("pool", W1[:, 1], I["cmp_w1"][1], writes=[b_w1])
        mk.dma("pool", W2[:], I["cmp_w2"].rearrange("kv p m e -> p kv m e"), writes=[b_w1])
        mk.op("dve", lambda e: e.memset(peT[:], 0.0), writes=[b_w1])
        mk.dma("pool", peT[:, :, 0:32], I["cmp_peT"].rearrange("kv d l -> d kv l"), writes=[b_w1])

        b_tabb = bufs(2, "tabb")
        b_rst = bufs(2, "rst")
        b_rtab = Buf("rtab")
        for h in range(16):
            k = h % 2
            mk.op("dve", lambda e, k=k, h=h: e.tensor_copy(out=tabb[k][:], in_=tab[:, h:h + 1].to_broadcast([32, 128])),
                  reads=[b_c], writes=[b_tabb[k]])
            mk.op("pe", lambda e, k=k: e.matmul(ps[k][:, 0:GW], lhsT=tabb[k][:], rhs=oh[:], start=True, stop=True),
                  reads=[b_tabb[k], b_c], writes=[psb[k]])
            mk.op("act", act_copy(rst[k][:], ps[k][:, 0:GW]), reads=[psb[k]], writes=[b_rst[k]])
            b = Buf()
            mk.dma("sp", X["RTAB"][h], rst[k][:], reads=[b_rst[k]], writes=[b])
            merge_ev(b_rtab, b)
        b_dn = Buf("dn")
        rt_t = X["RTAB"].tensor
        mk.dma("sp", dn01[:], bass.AP(rt_t, 127, [[GW - 1, 128], [128 * GW, 16], [128, 2], [1, 128]]),
               reads=[b_rtab], writes=[b_dn])
        b_mb = Buf("mbf")
        mk.dma("sp", mbf[:], bass.AP(rt_t, 224, [[GW - 16, 16], [128 * GW, 16], [1, 128]]), reads=[b_rtab],
               writes=[b_mb])
        b_m = Buf("masks")
        mk.op("dve", lambda e: e.tensor_scalar(out=negm0[:], in0=m0[:], scalar1=-1.0, scalar2=-NEG, op0=ALU.add,
                                               op1=ALU.mult), reads=[b_c], writes=[b_m])
        mk.op("dve", lambda e: e.tensor_scalar(out=m4[:], in0=m4[:], scalar1=-1.0, scalar2=-NEG, op0=ALU.add,
                                               op1=ALU.mult), reads=[b_c], writes=[b_m])
        mk.op("dve", lambda e: e.tensor_scalar(out=negcv[:], in0=cvm[:], scalar1=-1.0, scalar2=-NEG, op0=ALU.add,
                                               op1=ALU.mult), reads=[b_c], writes=[b_m])
        b_DN = Buf("DN")
        mk.op("dve", lambda e: e.tensor_tensor(out=dn01[:, :, 0, :], in0=dn01[:, :, 0, :],
                                               in1=m0[:].unsqueeze(1).to_broadcast([128, 16, 128]), op=ALU.mult),
              reads=[b_dn, b_c], writes=[b_dn])
        mk.op("dve", lambda e: e.tensor_tensor(out=DNb[:, :, 0, :], in0=dn01[:, :, 0, :],
                                               in1=negm0[:].unsqueeze(1).to_broadcast([128, 16, 128]), op=ALU.add),
              reads=[b_dn, b_m], writes=[b_DN])
        mk.op("dve", lambda e: e.tensor_copy(out=DNb[:, :, 1, :], in_=dn01[:, :, 1, :]), reads=[b_dn], writes=[b_DN])
        mk.op("dve", lambda e: e.tensor_copy(out=DN4b[:], in_=m4[:].unsqueeze(1).to_broadcast([128, 4, 128])),
              reads=[b_m], writes=[b_DN])
        mk.op("dve", lambda e: e.tensor_tensor(out=mbf[:], in0=mbf[:],
                                               in1=cvm[:].unsqueeze(1).to_broadcast([16, 16, 128]), op=ALU.mult),
              reads=[b_mb, b_c], writes=[b_mb])
        mk.op("dve", lambda e: e.tensor_tensor(out=Mb[:], in0=mbf[:],
                                               in1=negcv[:].unsqueeze(1).to_broadcast([16, 16, 128]), op=ALU.add),
              reads=[b_mb, b_m], writes=[b_DN])
        b_pb = Buf("pebias")
        for kv in range(2):
            for mc in range(2):
                pb = 2 + mc
                for l in range(32):
                    mk.op("pe", lambda e, kv=kv, mc=mc, l=l, pb=pb: e.matmul(
                        ps[pb][:, 0:2], lhsT=W1[:, kv, l, mc * 128:(mc + 1) * 128], rhs=peT[:, kv, l:l + 2],
                        start=(l == 0), stop=(l == 31)), reads=[b_w1], writes=[psb[pb]])
                mk.op("act", act_copy(pebias[:, kv, mc:mc + 1], ps[pb][:, 0:1]), reads=[psb[pb]], writes=[b_pb])

        if "DBGDN" in X:
            mk.dma("sp", X["DBGDN"], DNb[:], reads=[b_DN], writes=[Buf()])
            mk.dma("sp", X["DBGMB"], Mb[:], reads=[b_DN], writes=[Buf()])
        mk.flush()
        ts2.close()
        L = dict(locals())
        for s in range(nseq):
            attention_seq(P, s, L)


def attention_seq(P, s, L):
    nc, mk, I, W, X, C = P.nc, P.mk, P.I, P.W, P.X, P.C
    ps, psb = L["ps"], L["psb"]
    b_c, b_DN, b_w1, b_pb, b_vca = L["b_c"], L["b_DN"], L["b_w1"], L["b_pb"], L["b_vca"]
    W1, W2, pebias, VCA, KsA, zc, Mb, DNb, DN4b = (L[k] for k in ("W1", "W2", "pebias", "VCA", "KsA", "zc", "Mb",
                                                                 "DNb", "DN4b"))
    VAL, ADD, gbc, NSP, cst1 = L["VAL"], L["ADD"], L["gbc"], L["NSP"], L["cst1"]
    ident = C["ident"]
    with ExitStack() as ts:
        def sb(name, shape, dt):
            return ts.enter_context(nc.sbuf_tensor(f"p3s_{s}_{name}", shape, dt))

        big = sb("big", [128, 16384], BF16)
        KCt = big[0:64, :].rearrange("p (kv g t) -> p kv g t", kv=2, g=4)
        HT = sb("HT", [128, 2, 2, 508], BF16)
        KCMP = sb("KCMP", [64, 4, NCMP], BF16)
        Gt = sb("Gt", [128, 16, 48], F32)
        QA = sb("QA", [96, 4, S], BF16)
        KwT = sb("KwT", [64, S], BF16)
        Vsw = sb("Vsw", [128, 16, 2, 65], BF16)
        PTc = [sb(f"PTc{i}", [128, 512], BF16) for i in range(2)]
        PTs = [big[:, i * 8192:(i + 1) * 8192].rearrange("p (k n) -> p k n", k=16) for i in range(2)]
        PTw = [sb(f"PTw{i}", [128, 5, 512], BF16) for i in range(2)]
        ocmp = sb("ocmp", [128, 16, 4, 64], F32)
        oacc = [sb(f"oacc{i}", [128, 4, 64], F32) for i in range(2)]
        rs = sb("rs", [128, 4], F32)
        rinv = sb("rinv", [128, 4], F32)
        coef = sb("coef", [128, 4], F32)
        coefc = [sb(f"coefc{i}", [128, 4], F32) for i in range(2)]
        imp = sb("imp", [128, 32], F32)
        score = sb("score", [128, 32], F32)
        sc2 = sb("sc2", [128, 32], F32)
        m8a = sb("m8a", [128, 8], F32)
        m8b = sb("m8b", [128, 8], F32)
        thr = sb("thr", [128, 1], F32)
        selt = sb("selt", [128, 32], F32)
        oat = [sb(f"oat{i}", [128, 1024], F32) for i in range(2)]
        junk = sb("junk", [128, 1024], BF16)
        ssq = sb("ssq", [128, 1], F32)
        rstd = sb("rstd", [128, 1], F32)
        mtok = [sb(f"mtok{i}", [128, 1024], BF16) for i in range(2)]
        mixst = [sb(f"mixst{i}", [128, 8, 128], BF16) for i in range(2)]
        pst = [ts.enter_context(nc.psum_tensor(f"p3s_{s}_pst{i}", [128, 8, 128], BF16)) for i in range(0)]

        mk.barrier()
        b_kct = Buf("kct")
        mk.dma("sp", KCt, X["KC"][s].rearrange("(kv g d) t -> d kv g t", kv=2, g=4), reads=[P.xb["KC"][s]],
               writes=[b_kct])
        b_G = Buf("G")
        mk.dma("sp", Gt[:], X["GATE"][s].rearrange("tt p e -> p tt e"), reads=[P.xb["GATE"][s]], writes=[b_G])
        b_ht = bufs(4, "ht")
        for kv in range(2):
            for mc in range(2):
                pb = (2 * kv + mc) % 4
                for l in range(32):
                    mk.op("pe", lambda e, kv=kv, mc=mc, l=l, pb=pb: e.matmul(
                        ps[pb][:, 0:508].rearrange("p (g c) -> p g c", g=4),
                        lhsT=W1[:, kv, l, mc * 128:(mc + 1) * 128], rhs=KCt[:, kv, :, l:l + 2017:16],
                        start=(l == 0), stop=(l == 31)), reads=[b_w1, b_kct], writes=[psb[pb]])
                mk.op("act", lambda e, kv=kv, mc=mc, pb=pb: e.activation(
                    out=HT[:, kv, mc, :], in_=ps[pb][:, 0:508], func=AF.Gelu_apprx_tanh, bias=pebias[:, kv, mc:mc + 1]),
                    reads=[psb[pb], b_pb], writes=[b_ht[2 * kv + mc]])
        b_kcmp = Buf("kcmp")
        for mc in range(2):
            mk.op("pe", lambda e, mc=mc: e.matmul(ps[4][0:64, 0:508], lhsT=W2[:, 0, mc, :], rhs=HT[:, 0, mc, :],
                                                  start=(mc == 0), stop=(mc == 1)),
                  reads=[b_w1, b_ht[0], b_ht[1]], writes=[psb[4]])
        mk.op("act", act_copy(KCMP[:], ps[4][0:64, 0:508].rearrange("p (g c) -> p g c", g=4)), reads=[psb[4]],
              writes=[b_kcmp])
        b_vc = Buf("vcmp")
        merge_ev(b_vc, b_vca)
        for g in range(4):
            pb = 5 + g % 2
            for mc in range(2):
                mk.op("pe", lambda e, mc=mc, g=g, pb=pb: e.matmul(
                    ps[pb][0:NCMP, 0:64], lhsT=HT[:, 1, mc, g * NCMP:(g + 1) * NCMP], rhs=W2[:, 1, mc, :],
                    start=(mc == 0), stop=(mc == 1)), reads=[b_w1, b_ht[2], b_ht[3]], writes=[psb[pb]])
            bb = Buf()
            mk.op("dve", lambda e, g=g, pb=pb: e.tensor_copy(out=VCA[0:NCMP, g, 0:64], in_=ps[pb][0:NCMP, 0:64]),
                  reads=[psb[pb], b_vca], writes=[bb])
            merge_ev(b_vc, bb)
        b_vca.r = {}
        L["b_vca_last"] = b_vc

        mk.barrier()
        b_qa = Buf("qa")
        b_qsel = bufs(16, "qsel")
        b_ks = Buf("ks")
        b_kw = Buf("kw")
        b_v = Buf("v")
        b_ptc = bufs(2, "ptc")
        b_pts = [bufs(16, f"pts{i}_") for i in range(2)]
        b_ptw = [bufs(5, f"ptw{i}_") for i in range(2)]
        b_ocmp = bufs(16, "ocmp")
        b_oacc = bufs(2, "oacc")
        b_t = Buf("dvetmp")
        b_coefc = bufs(2, "coefc")
        b_nsp = bufs(2, "nsp")
        for i in range(2):
            merge_ev(b_nsp[i], b_c)
        scnt = [0]

        def sbank():
            b = scnt[0] % 3
            scnt[0] += 1
            return b

        for g in range(4):
            mk.dma("sp", QA[0:64, :, :], X["QT"][s, g * 256:(g + 1) * 256, :].rearrange("(r d) t -> d r t", r=4),
                   reads=[P.xb["QT"][s]], writes=[b_qa] + b_qsel)
            mk.dma("sp", KsA[0:64, :], X["KS"][s, g * 64:(g + 1) * 64, :], reads=[P.xb["KS"][s]], writes=[b_ks])
            mk.dma("sp", KwT[:], X["KW"][s, g * 64:(g + 1) * 64, :], reads=[P.xb["KW"][s]], writes=[b_kw])
            for a_ in range(2):
                mk.dma("sp", Vsw[:, :, a_, :], X["VSW"][s][:, :, a_, g, :].rearrange("tt p e -> p tt e"),
                       reads=[P.xb["VSW"][s]], writes=[b_v])

            def la1(qt):
                nk = min(NCMP, 8 * qt + 7)
                qs = slice(qt * 128, (qt + 1) * 128)
                sbk = sbank()
                k2 = qt % 2
                off = 136 - 8 * qt
                mk.op("pe", lambda e, g=g, nk=nk, qs=qs, sbk=sbk: e.matmul(
                    ps[sbk][0:nk, :].rearrange("p (r i) -> p r i", r=4), lhsT=KCMP[:, g, 0:nk], rhs=QA[0:64, :, qs],
                    start=True, stop=False), reads=[b_kcmp, b_qa], writes=[psb[sbk]])
                mk.op("pe", lambda e, g=g, nk=nk, off=off, sbk=sbk: e.matmul(
                    ps[sbk][0:nk, :].rearrange("p (r i) -> p r i", r=4), lhsT=zc[0:16, off:off + nk],
                    rhs=Mb[0:16, 4 * g:4 * g + 4, :], start=False, stop=True), reads=[b_c, b_DN], writes=[psb[sbk]])
                mk.op("act", lambda e, nk=nk, sbk=sbk, k2=k2: e.activation(out=PTc[k2][0:nk, :], in_=ps[sbk][0:nk, :],
                                                                          func=AF.Exp),
                      reads=[psb[sbk]], writes=[b_ptc[k2]])
                ob = 3
                for r in range(4):
                    mk.op("pe", lambda e, r=r, nk=nk, g=g, k2=k2, ob=ob: e.matmul(
                        ps[ob][:, r * 128:r * 128 + 97], lhsT=PTc[k2][0:nk, r * 128:(r + 1) * 128], rhs=VCA[0:nk, g, :],
                        start=True, stop=True), reads=[b_ptc[k2], b_vc], writes=[psb[ob]])

            def la2a(qt):
                k2 = qt % 2
                qs = slice(qt * 128, (qt + 1) * 128)
                ob = 3
                O = ps[ob][:, :].rearrange("p (r e) -> p r e", r=4)
                mk.op("dve", lambda e, O=O: e.tensor_scalar(out=rs[:], in0=O[:, :, 64], scalar1=1e-30, scalar2=None,
                                                            op0=ALU.max), reads=[psb[ob]], writes=[b_t])
                mk.op("dve", lambda e: e.reciprocal(out=rinv[:], in_=rs[:]), reads=[b_t], writes=[b_t])
                mk.op("dve", lambda e, O=O: e.tensor_scalar(out=imp[:], in0=O[:, 0, 65:97], scalar1=rinv[:, 0:1],
                                                            scalar2=None, op0=ALU.mult),
                      reads=[psb[ob], b_t], writes=[b_t])
                for r in range(1, 4):
                    mk.op("dve", lambda e, O=O, r=r: e.scalar_tensor_tensor(
                        out=imp[:], in0=O[:, r, 65:97], scalar=rinv[:, r:r + 1], in1=imp[:], op0=ALU.mult,
                        op1=ALU.add), reads=[psb[ob], b_t], writes=[b_t])
                mk.op("dve", lambda e, qt=qt, g=g: e.tensor_tensor(out=coef[:], in0=rinv[:],
                                                                  in1=Gt[:, qt, g * 12:g * 12 + 12:3], op=ALU.mult),
                      reads=[b_t, b_G], writes=[b_t])
                for r in range(4):
                    mk.op("dve", lambda e, O=O, r=r, qt=qt: e.tensor_scalar(
                        out=ocmp[:, qt, r, :], in0=O[:, r, 0:64], scalar1=coef[:, r:r + 1], scalar2=None,
                        op0=ALU.mult), reads=[psb[ob], b_t], writes=[b_ocmp[qt]])
                mk.op("dve", lambda e, qt=qt: e.tensor_tensor(out=score[:], in0=imp[:], in1=VAL[:, qt, :], op=ALU.mult),
                      reads=[b_t, b_c], writes=[b_t])
                mk.op("dve", lambda e, qt=qt: e.tensor_tensor(out=score[:], in0=score[:], in1=ADD[:, qt, :], op=ALU.add),
                      reads=[b_t, b_c], writes=[b_t])
                mk.op("dve", lambda e: e.max(out=m8a[:], in_=score[:]), reads=[b_t], writes=[b_t])
                mk.op("dve", lambda e: e.match_replace(out=sc2[:], in_to_replace=m8a[:], in_values=score[:],
                                                       imm_value=-1e30), reads=[b_t], writes=[b_t])
                mk.op("dve", lambda e: e.max(out=m8b[:], in_=sc2[:]), reads=[b_t], writes=[b_t])
                mk.op("dve", lambda e: e.tensor_scalar(out=thr[:], in0=m8b[:, 7:8], scalar1=0.0, scalar2=None,
                                                       op0=ALU.max), reads=[b_t], writes=[b_t])
                mk.op("dve", lambda e: e.tensor_scalar(out=selt[:], in0=score[:], scalar1=thr[:, 0:1], scalar2=None,
                                                       op0=ALU.is_ge), reads=[b_t], writes=[b_t])
                mk.op("dve", lambda e, k2=k2: e.tensor_scalar(out=NSP[k2][:, 64:96], in0=selt[:], scalar1=-1.0,
                                                              scalar2=-NEG, op0=ALU.add, op1=ALU.mult),
                      reads=[b_t], writes=[b_nsp[k2]])

            def la2b(qt):
                k2 = qt % 2
                qs = slice(qt * 128, (qt + 1) * 128)
                tb = sbank()
                mk.op("pe", lambda e, k2=k2, tb=tb: e.matmul(ps[tb][0:96, 0:128], lhsT=NSP[k2][:, 0:96], rhs=ident[:],
                                                             start=True, stop=True),
                      reads=[b_nsp[k2], P.cb], writes=[psb[tb]])
                mk.op("act", lambda e, tb=tb, qs=qs: e.activation(
                    out=QA[64:96, :, qs], in_=ps[tb][64:96, 0:128].unsqueeze(1).to_broadcast([32, 4, 128]),
                    func=AF.Copy), reads=[psb[tb]], writes=[b_qsel[qt]])


            def qk(qt):
                k2 = qt % 2
                qs = slice(qt * 128, (qt + 1) * 128)
                for kt in range(0, qt + 1):
                    sbk = sbank()
                    ks_ = slice(kt * 128, (kt + 1) * 128)
                    near = kt >= qt - 1
                    mk.op("pe", lambda e, sbk=sbk, ks_=ks_, qs=qs, near=near: e.matmul(
                        ps[sbk][:, :].rearrange("p (r i) -> p r i", r=4), lhsT=KsA[0:96, ks_], rhs=QA[0:96, :, qs],
                        start=True, stop=(not near)), reads=[b_ks, b_c, b_qa, b_qsel[qt]], writes=[psb[sbk]])
                    if near:
                        mk.op("pe", lambda e, sbk=sbk, g=g, dl=qt - kt: e.matmul(
                            ps[sbk][:, :].rearrange("p (r i) -> p r i", r=4), lhsT=ident[:],
                            rhs=DNb[:, 4 * g:4 * g + 4, dl, :], start=False, stop=True),
                            reads=[P.cb, b_DN], writes=[psb[sbk]])
                    mk.op("act", lambda e, sbk=sbk, k2=k2, kt=kt: e.activation(out=PTs[k2][:, kt, :], in_=ps[sbk][:, :],
                                                                              func=AF.Exp),
                          reads=[psb[sbk]], writes=[b_pts[k2][kt]])
                for wi, kt in enumerate(range(max(0, qt - 4), qt + 1)):
                    sbk = sbank()
                    ks_ = slice(kt * 128, (kt + 1) * 128)
                    dl = qt - kt
                    sp_ = dl in (0, 1, 4)
                    mk.op("pe", lambda e, sbk=sbk, ks_=ks_, qs=qs, sp_=sp_: e.matmul(
                        ps[sbk][:, :].rearrange("p (r i) -> p r i", r=4), lhsT=KwT[0:64, ks_], rhs=QA[0:64, :, qs],
                        start=True, stop=(not sp_)), reads=[b_kw, b_qa], writes=[psb[sbk]])
                    if sp_:
                        rhs = DN4b[:] if dl == 4 else DNb[:, 4 * g:4 * g + 4, dl, :]
                        mk.op("pe", lambda e, sbk=sbk, rhs=rhs: e.matmul(
                            ps[sbk][:, :].rearrange("p (r i) -> p r i", r=4), lhsT=ident[:], rhs=rhs, start=False,
                            stop=True), reads=[P.cb, b_DN], writes=[psb[sbk]])
                    mk.op("act", lambda e, sbk=sbk, k2=k2, wi=wi: e.activation(out=PTw[k2][:, wi, :], in_=ps[sbk][:, :],
                                                                              func=AF.Exp),
                          reads=[psb[sbk]], writes=[b_ptw[k2][wi]])

            def pv(qt):
                k2 = qt % 2
                obs = 4 + 2 * k2
                obw = 5 + 2 * k2
                for r in range(4):
                    for kt in range(0, qt + 1):
                        mk.op("pe", lambda e, r=r, kt=kt, k2=k2, obs=obs, qt=qt: e.matmul(
                            ps[obs][:, r * 128:r * 128 + 65], lhsT=PTs[k2][:, kt, r * 128:(r + 1) * 128],
                            rhs=Vsw[:, kt, 0, :], start=(kt == 0), stop=(kt == qt)),
                            reads=[b_pts[k2][kt], b_v], writes=[psb[obs]])
                kts = list(range(max(0, qt - 4), qt + 1))
                for r in range(4):
                    for wi, kt in enumerate(kts):
                        mk.op("pe", lambda e, r=r, kt=kt, wi=wi, k2=k2, obw=obw: e.matmul(
                            ps[obw][:, r * 128:r * 128 + 65], lhsT=PTw[k2][:, wi, r * 128:(r + 1) * 128],
                            rhs=Vsw[:, kt, 1, :], start=(wi == 0), stop=(wi == len(kts) - 1)),
                            reads=[b_ptw[k2][wi], b_v], writes=[psb[obw]])
                for bi, ob in ((1, obs), (2, obw)):
                    O = ps[ob][:, :].rearrange("p (r e) -> p r e", r=4)
                    mk.op("dve", lambda e, O=O: e.reciprocal(out=rinv[:], in_=O[:, :, 64]), reads=[psb[ob]], writes=[b_t])
                    mk.op("dve", lambda e, qt=qt, bi=bi, g=g: e.tensor_tensor(
                        out=coef[:], in0=rinv[:], in1=Gt[:, qt, g * 12 + bi:g * 12 + 12:3], op=ALU.mult),
                        reads=[b_t, b_G], writes=[b_t])
                    for r in range(4):
                        src1 = ocmp[:, qt, r, :] if bi == 1 else oacc[k2][:, r, :]
                        mk.op("dve", lambda e, O=O, r=r, src1=src1, k2=k2: e.scalar_tensor_tensor(
                            out=oacc[k2][:, r, :], in0=O[:, r, 0:64], scalar=coef[:, r:r + 1], in1=src1, op0=ALU.mult,
                            op1=ALU.add), reads=[psb[ob], b_t, b_ocmp[qt], b_oacc[k2]], writes=[b_oacc[k2]])
                bb = Buf()
                mk.dma("pool", X["OATT"][s, qt, :, g * 256:(g + 1) * 256], oacc[k2][:].rearrange("p r e -> p (r e)"),
                       reads=[b_oacc[k2]], writes=[bb])
                merge_ev(P.xb["OATT"][s], bb)

            for v in range(-3, 16):
                if 0 <= v + 3 < 16:
                    la1(v + 3)
                    la2a(v + 3)
                if 0 <= v + 2 < 16:
                    la2b(v + 2)
                if 0 <= v + 1 < 16:
                    qk(v + 1)
                if 0 <= v < 16:
                    pv(v)

        b_oat = bufs(2, "oat")
        b_n = Buf("normtmp")
        b_mtok = bufs(2, "mtok")
        b_mixst = bufs(2, "mixst")
        for qt in range(16):
            k2 = qt % 2
            mk.dma("sp", oat[k2][:], X["OATT"][s, qt], reads=[P.xb["OATT"][s]], writes=[b_oat[k2]])
            mk.op("act", lambda e, k2=k2: e.activation(out=junk[:], in_=oat[k2][:], func=AF.Square, accum_out=ssq[:]),
                  reads=[b_oat[k2]], writes=[b_n])
            mk.op("act", lambda e: e.activation(out=rstd[:], in_=ssq[:], func=AF.Sqrt, scale=1.0 / 1024.0,
                                                bias=cst1[:, 1:2]), reads=[b_n, b_c], writes=[b_n])
            mk.op("dve", lambda e: e.reciprocal(out=rstd[:], in_=rstd[:]), reads=[b_n], writes=[b_n])
            mk.op("dve", lambda e, k2=k2: e.scalar_tensor_tensor(out=mtok[k2][:], in0=oat[k2][:], scalar=rstd[:, 0:1],
                                                                 in1=gbc[:], op0=ALU.mult, op1=ALU.mult),
                  reads=[b_oat[k2], b_n, b_c], writes=[b_mtok[k2]])
            tb = 2 * k2
            for c in range(8):
                mk.op("pe", lambda e, c=c, k2=k2, tb=tb: e.matmul(
                    ps[tb + c // 4][:, (c % 4) * 128:(c % 4 + 1) * 128], lhsT=mtok[k2][:, c * 128:(c + 1) * 128],
                    rhs=ident[:], start=True, stop=True), reads=[b_mtok[k2], P.cb], writes=[psb[tb + c // 4]])
            for hh_ in range(2):
                evac(mk, hh_, mixst[k2][:, 4 * hh_:4 * hh_ + 4, :],
                     ps[tb + hh_][:, :].rearrange("p (c t) -> p c t", c=4), [psb[tb + hh_]], [b_mixst[k2]])
            bb = Buf()
            mk.dma("pool", X["MIXA"][s, :, qt * 128:(qt + 1) * 128].rearrange("(c p) t -> p c t", p=128), mixst[k2][:],
                   reads=[b_mixst[k2]], writes=[bb])
            merge_ev(P.xb["MIXA"][s], bb)
        mk.flush()


def phase4(P, nseq):
    nc, mk, I, W, X, C = P.nc, P.mk, P.I, P.W, P.X, P.C
    mk.barrier()
    TG = 512
    with ExitStack() as ts:
        def sb(name, shape, dt):
            return ts.enter_context(nc.sbuf_tensor(f"p4_{name}", shape, dt))

        hx = sb("hx", [128, 16, TG], F32)
        mixT = sb("mixT", [128, 16, TG], BF16)
        rstr = sb("rstr", [128, TG], F32)
        y2T = sb("y2T", [128, 16, TG], BF16)
        actT = sb("actT", [128, NFC, TG], BF16)
        W16 = [sb(f"W16_{i}", [128, 16, 256], BF16) for i in range(4)]
        WD = [sb(f"WD_{i}", [128, NFC, 128], BF16) for i in range(2)]
        gpre = [sb(f"gpre{i}", [128, TG + 2], F32) for i in range(2)]
        cv = [sb(f"cv{i}", [128, TG], F32) for i in range(2)]
        ge = [sb(f"ge{i}", [128, TG], F32) for i in range(2)]
        GC = sb("GC", [128, NFC, 2], F32)
        rt = sb("rt", [128, TG], F32)
        rstd = sb("rstd", [128, TG], F32)
        fv = sb("fv", [128, NFC, 4], F32)
        gffn = sb("gffn", [128, 16], F32)
        gfin = sb("gfin", [128, 16], F32)
        epst = sb("eps", [128, 1], F32)
        ps = [ts.enter_context(nc.psum_tensor(f"p4_ps{i}", [128, 512], F32)) for i in range(8)]
        psb = bufs(8, "ps")
        ones = C["ones"]

        b_c = Buf("p4c")
        for (t_, src) in ((fv, I["ffn_vec"]), (gffn, I["g_ffn"]), (gfin, I["g_fin"])):
            b = Buf()
            mk.dma("pool", t_[:], src, writes=[b])
            merge_ev(b_c, b)
        b = Buf()
        mk.op("dve", lambda e: e.memset(epst[:], EPS), writes=[b])
        merge_ev(b_c, b)
        P_EPS[0] = epst[:]
        P_EPS[1] = b_c

        b_hx = bufs(16, "hx")
        b_mix = bufs(16, "mix")
        b_rstr = Buf("rstr")
        b_y2 = bufs(16, "y2")
        b_act = bufs(NFC, "act")
        b_w16 = bufs(4, "w16")
        b_wd = bufs(2, "wd")
        b_gpre = bufs(2, "gpre")
        b_cv = bufs(2, "cv")
        b_ge = bufs(2, "ge")
        b_gc = bufs(NFC, "gc")
        b_rt = Buf("rt")
        b_rstd = Buf("rstd")

        groups = [(s, j) for s in range(nseq) for j in range(S // TG)]
        wo_src = W["w_out"].rearrange("(c p) n -> p c n", p=128)
        wg_src = W["w_g"].rearrange("(c p) n -> p c n", p=128)
        wu_src = W["w_u"].rearrange("(c p) n -> p c n", p=128)
        wd_src = W["w_d"].rearrange("(f p) n -> p f n", p=128)
        items16 = []
        for gi_ in range(len(groups)):
            for dcp in range(8):
                items16.append((wo_src[:, :, dcp * 256:(dcp + 1) * 256], P.wb["w_out"]))
            for fp in range(NFC // 2):
                items16.append((wg_src[:, :, fp * 256:(fp + 1) * 256], P.wb["w_g"]))
                items16.append((wu_src[:, :, fp * 256:(fp + 1) * 256], P.wb["w_u"]))
        st16 = [0]

        def need16(n):
            while st16[0] <= min(n + 3, len(items16) - 1):
                i = st16[0]
                src, wb_ = items16[i]
                mk.dma("sp", W16[i % 4][:], src, reads=[wb_], writes=[b_w16[i % 4]])
                st16[0] += 1

        itemsd = []
        for gi_ in range(len(groups)):
            for dc in range(16):
                itemsd.append(wd_src[:, :, dc * 128:(dc + 1) * 128])
        std = [0]

        def needd(n):
            while std[0] <= min(n + 1, len(itemsd) - 1):
                i = std[0]
                mk.dma("pool", WD[i % 2][:], itemsd[i], reads=[P.wb["w_d"]], writes=[b_wd[i % 2]])
                std[0] += 1

        pcnt = [0]

        def bank():
            b = pcnt[0] % 8
            pcnt[0] += 1
            return b

        def norm_stats(n_feat):
            pb = bank()
            for c in range(16):
                mk.op("act", lambda e, c=c: e.activation(out=mixT[:, c, :], in_=hx[:, c, :], func=AF.Square),
                      reads=[b_hx[c]], writes=[b_mix[c]])
                mk.op("pe", lambda e, c=c, pb=pb: e.matmul(ps[pb][:, :], lhsT=ones[:], rhs=mixT[:, c, :], start=(c == 0),
                                                           stop=(c == 15)), reads=[b_mix[c], P.cb], writes=[psb[pb]])
            rms_rstd(mk, ps[pb][:, :], psb[pb], rt[:], b_rt, rstd[:], b_rstd, float(n_feat))

        i16 = 0
        idn = 0
        import os
        P4S = int(os.environ.get("P4_STOP", "9"))
        CPE = os.environ.get("P4_CPE", "dve")
        if P4S < 9:
            groups = groups[:1]
        for gi_, (s, j) in enumerate(groups):
            t0 = j * TG
            tsl = slice(t0, t0 + TG)
            need16(i16)
            xsrc = I["xT"][s].rearrange("(c p) t -> p c t", p=128)
            for hf in range(2):
                cs = slice(hf * 8, hf * 8 + 8)
                mk.dma("pool", hx[:, cs, :], xsrc[:, cs, tsl], writes=b_hx[hf * 8:hf * 8 + 8])
            mk.dma("pool", mixT[:, 0:8, :], X["MIXA"][s].rearrange("(c p) t -> p c t", p=128)[:, :, tsl],
                   reads=[P.xb["MIXA"][s]], writes=b_mix[0:8])
            mk.dma("pool", mixT[:, 8:16, :], X["MIXR"][s].rearrange("(c p) t -> p c t", p=128)[:, :, tsl],
                   reads=[P.xb["MIXR"][s]], writes=b_mix[8:16])
            mk.dma("pool", rstr[:], X["RSTDR"][s][:, tsl], reads=[P.xb["RSTDR"][s]], writes=[b_rstr])
            for c in range(8, 16):
                mk.op("dve", lambda e, c=c: e.tensor_tensor(out=mixT[:, c, :], in0=mixT[:, c, :], in1=rstr[:],
                                                            op=ALU.mult), reads=[b_mix[c], b_rstr], writes=[b_mix[c]])
            if j == 0:
                for fc in range(NFC):
                    mk.op("dve", lambda e, fc=fc: e.memset(GC[:, fc, :], 0.0), writes=[b_gc[fc]])
            if P4S <= 1:
                break
            for dcp in range(8):
                need16(i16)
                slot = i16 % 4
                for dd in range(2):
                    dc = 2 * dcp + dd
                    pb = bank()
                    for c in range(16):
                        mk.op("pe", lambda e, c=c, pb=pb, slot=slot, dd=dd: e.matmul(
                            ps[pb][:, :], lhsT=W16[slot][:, c, dd * 128:(dd + 1) * 128], rhs=mixT[:, c, :],
                            start=(c == 0), stop=(c == 15)), reads=[b_w16[slot], b_mix[c]], writes=[psb[pb]])
                    mk.op("dve", lambda e, dc=dc, pb=pb: e.tensor_tensor(out=hx[:, dc, :], in0=ps[pb][:, :],
                                                                        in1=hx[:, dc, :], op=ALU.add),
                          reads=[psb[pb], b_hx[dc]], writes=[b_hx[dc]])
                i16 += 1
            if P4S <= 2:
                break
            norm_stats(D)
            for c in range(16):
                mk.op("dve", lambda e, c=c: e.scalar_tensor_tensor(out=y2T[:, c, :], in0=hx[:, c, :],
                                                                   scalar=gffn[:, c:c + 1], in1=rstd[:],
                                                                   op0=ALU.mult, op1=ALU.mult),
                      reads=[b_hx[c], b_rstd, b_c], writes=[b_y2[c]])
            if P4S <= 3:
                break
            for fp in range(NFC // 2):
                need16(i16)
                sg = i16 % 4
                su = (i16 + 1) % 4
                for ff in range(2):
                    fc = 2 * fp + ff
                    k = fc % 2
                    pg = bank()
                    pu = bank()
                    for c in range(16):
                        mk.op("pe", lambda e, c=c, pg=pg, sg=sg, ff=ff: e.matmul(
                            ps[pg][:, :], lhsT=W16[sg][:, c, ff * 128:(ff + 1) * 128], rhs=y2T[:, c, :],
                            start=(c == 0), stop=(c == 15)), reads=[b_w16[sg], b_y2[c]], writes=[psb[pg]])
                    for c in range(16):
                        mk.op("pe", lambda e, c=c, pu=pu, su=su, ff=ff: e.matmul(
                            ps[pu][:, :], lhsT=W16[su][:, c, ff * 128:(ff + 1) * 128], rhs=y2T[:, c, :],
                            start=(c == 0), stop=(c == 15)), reads=[b_w16[su], b_y2[c]], writes=[psb[pu]])
                    mk.op(CPE, lambda e, k=k, fc=fc: e.tensor_copy(out=gpre[k][:, 0:2], in_=GC[:, fc, :]),
                          reads=[b_gc[fc]], writes=[b_gpre[k]])
                    mk.op("act", act_copy(gpre[k][:, 2:TG + 2], ps[pg][:, :]), reads=[psb[pg]], writes=[b_gpre[k]])
                    mk.op("act", lambda e, k=k, fc=fc, pg=pg: e.activation(
                        out=cv[k][:], in_=ps[pg][:, :], func=AF.Identity, scale=fv[:, fc, 2:3], bias=fv[:, fc, 3:4]),
                        reads=[psb[pg], b_c], writes=[b_cv[k]])
                    mk.op(CPE, lambda e, k=k, fc=fc: e.tensor_copy(out=GC[:, fc, :], in_=gpre[k][:, TG:TG + 2]),
                          reads=[b_gpre[k]], writes=[b_gc[fc]])
                    for kk in (1, 0):
                        mk.op("dve", lambda e, k=k, fc=fc, kk=kk: e.scalar_tensor_tensor(
                            out=cv[k][:], in0=gpre[k][:, kk:kk + TG], scalar=fv[:, fc, kk:kk + 1], in1=cv[k][:],
                            op0=ALU.mult, op1=ALU.add), reads=[b_gpre[k], b_cv[k], b_c], writes=[b_cv[k]])
                    mk.op("act", lambda e, k=k: e.activation(out=ge[k][:], in_=cv[k][:], func=AF.Gelu_apprx_tanh),
                          reads=[b_cv[k]], writes=[b_ge[k]])
                    mk.op("dve", lambda e, k=k, fc=fc, pu=pu: e.tensor_tensor(out=actT[:, fc, :], in0=ps[pu][:, :],
                                                                              in1=ge[k][:], op=ALU.mult),
                          reads=[psb[pu], b_ge[k]], writes=[b_act[fc]])
                i16 += 2
            if P4S <= 4:
                break
            for dc in range(16):
                needd(idn)
                slot = idn % 2
                pb = bank()
                for fc in range(NFC):
                    mk.op("pe", lambda e, fc=fc, pb=pb, slot=slot: e.matmul(
                        ps[pb][:, :], lhsT=WD[slot][:, fc, :], rhs=actT[:, fc, :], start=(fc == 0),
                        stop=(fc == NFC - 1)), reads=[b_wd[slot], b_act[fc]], writes=[psb[pb]])
                mk.op("dve", lambda e, dc=dc, pb=pb: e.tensor_tensor(out=hx[:, dc, :], in0=ps[pb][:, :],
                                                                    in1=hx[:, dc, :], op=ALU.add),
                      reads=[psb[pb], b_hx[dc]], writes=[b_hx[dc]])
                idn += 1
            if P4S <= 5:
                break
            norm_stats(D)
            for c in range(16):
                mk.op("dve", lambda e, c=c: e.scalar_tensor_tensor(out=hx[:, c, :], in0=hx[:, c, :],
                                                                   scalar=gfin[:, c:c + 1], in1=rstd[:],
                                                                   op0=ALU.mult, op1=ALU.mult),
                      reads=[b_hx[c], b_rstd, b_c], writes=[b_hx[c]])
            osrc = P.outT[s].rearrange("(c p) t -> p c t", p=128)
            for hf in range(2):
                cs = slice(hf * 8, hf * 8 + 8)
                mk.dma("pool", osrc[:, cs, tsl], hx[:, cs, :], reads=b_hx[hf * 8:hf * 8 + 8], writes=[Buf()])
        mk.flush()
```

```python
import math
from contextlib import ExitStack

import numpy as np
import ml_dtypes

import concourse.bass as bass
import concourse.mybir as mybir
from concourse.bass_utils import run_bass_kernel_spmd

F32 = mybir.dt.float32
BF16 = mybir.dt.bfloat16
AF = mybir.ActivationFunctionType
ALU = mybir.AluOpType

N_CORES = 8
D = 2048
S = 2048
NSEQ = 2
NH = 16
NG = 4
HD = 64
INW = 4656
DFF = 5632
NFC = DFF // 128
NCMP = 127
EPS = 1e-6
NEG = -30000.0


class Buf:
    __slots__ = ("name", "w", "r")

    def __init__(self, name=""):
        self.name = name
        self.w = {}
        self.r = {}


def bufs(n, name=""):
    return [Buf(f"{name}{i}") for i in range(n)]


class MK:
    ENG = ("pe", "act", "dve", "pool", "sp")

    def __init__(self, nc, es):
        self.nc = nc
        self.q = {e: [] for e in self.ENG}
        self.semh = {}
        self.prog = {}
        for e in ("pe", "act", "dve", "pool"):
            h = es.enter_context(nc.semaphore("prog_" + e))
            self.prog[e] = [h, 0]
            self.semh[("p", e)] = h
        self.seen = {e: {} for e in self.ENG}
        self.dsem = {}
        for qn, n in (("sp", 10), ("pool", 6), ("act", 2), ("cast", 8)):
            lst = []
            for i in range(n):
                h = es.enter_context(nc.semaphore(f"d_{qn}{i}"))
                lst.append([h, 0])
                self.semh[("d", qn, i)] = h
            self.dsem[qn] = lst
        self.drr = {qn: 0 for qn in self.dsem}
        self.ninstr = {e: 0 for e in self.ENG}

    def _wait(self, eng, key, v):
        if eng == "pe" and key == ("p", "pe"):
            return
        seen = self.seen[eng]
        if seen.get(key, 0) < v:
            seen[key] = v
            h = self.semh[key]
            self.q[eng].append(lambda e, h=h, v=v: e.wait_ge(h, v))
            self.ninstr[eng] += 1

    def _waits(self, eng, reads, writes):
        need = {}
        for b in reads:
            for k, v in b.w.items():
                if need.get(k, 0) < v:
                    need[k] = v
        for b in writes:
            for k, v in b.w.items():
                if need.get(k, 0) < v:
                    need[k] = v
            for k, v in b.r.items():
                if need.get(k, 0) < v:
                    need[k] = v
        for k, v in need.items():
            self._wait(eng, k, v)

    def op(self, eng, fn, reads=(), writes=()):
        self._waits(eng, reads, writes)
        p = self.prog[eng]
        p[1] += 1
        v = p[1]
        key = ("p", eng)
        h = p[0]
        self.q[eng].append(lambda e, fn=fn, h=h: fn(e).then_inc(h, 1))
        self.ninstr[eng] += 1
        for b in reads:
            if b.r.get(key, 0) < v:
                b.r[key] = v
        for b in writes:
            b.w = {key: v}
            b.r = {}

    def dma(self, qn, out, in_, reads=(), writes=(), sems=None):
        self._waits(qn, reads, writes)
        sn = sems or qn
        lst = self.dsem[sn]
        i = self.drr[sn]
        self.drr[sn] = (i + 1) % len(lst)
        s = lst[i]
        key = ("d", sn, i)
        if s[1] > 0:
            self._wait(qn, key, s[1])
        s[1] += 16
        v = s[1]
        h = s[0]
        self.q[qn].append(lambda e, h=h, out=out, in_=in_: e.dma_start(out=out, in_=in_).then_inc(h, 16))
        self.ninstr[qn] += 1
        for b in reads:
            if b.r.get(key, 0) < v:
                b.r[key] = v
        for b in writes:
            b.w = {key: v}
            b.r = {}

    def barrier(self):
        for eng in self.ENG:
            for e2, p in self.prog.items():
                if p[1] > 0:
                    if eng == e2 and eng == "pe":
                        continue
                    seen = self.seen[eng]
                    key = ("p", e2)
                    if seen.get(key, 0) < p[1]:
                        seen[key] = p[1]
                        self.q[eng].append(lambda e, h=p[0], v=p[1]: e.wait_ge(h, v))
            for qn, lst in self.dsem.items():
                if qn == "cast":
                    continue
                for i, s in enumerate(lst):
                    if s[1] > 0:
                        key = ("d", qn, i)
                        seen = self.seen[eng]
                        if seen.get(key, 0) < s[1]:
                            seen[key] = s[1]
                            self.q[eng].append(lambda e, h=s[0], v=s[1]: e.wait_ge(h, v))

    def flush(self, final=False):
        nc = self.nc
        if final:
            for qn, lst in self.dsem.items():
                for i, s in enumerate(lst):
                    if s[1] > 0:
                        self._wait("pool" if qn == "cast" else qn, ("d", qn, i), s[1])
        q = self.q
        with nc.Block() as block:
            @block.tensor
            def _(e):
                for f in q["pe"]:
                    f(e)

            @block.scalar
            def _(e):
                for f in q["act"]:
                    f(e)

            @block.vector
            def _(e):
                for f in q["dve"]:
                    f(e)

            @block.gpsimd
            def _(e):
                for f in q["pool"]:
                    f(e)

            @block.sync
            def _(e):
                for f in q["sp"]:
                    f(e)
        self.q = {e: [] for e in self.ENG}


def t5_bucket_np(dist):
    n = np.maximum(dist, 0)
    max_exact = 16
    nf = np.maximum(n, 1).astype(np.float32)
    large = max_exact + (np.log(nf / np.float32(max_exact)) / np.float32(math.log(128 / max_exact))
                         * np.float32(32 - max_exact)).astype(np.int32)
    large = np.minimum(large, 31)
    return np.where(n < max_exact, n, large)


GW = 384


def host_constants():
    c = {}
    c["ident_bf"] = np.eye(128, dtype=np.float32).astype(ml_dtypes.bfloat16)
    c["ones_bf"] = np.ones((128, 128), dtype=np.float32).astype(ml_dtypes.bfloat16)
    n = np.arange(GW)
    bk = t5_bucket_np(n - 127)
    oh = np.zeros((32, GW), np.float32)
    oh[bk, n] = 1.0
    oh[31, :] -= 1.0
    c["oh"] = oh
    j = np.arange(128)[:, None]
    i = np.arange(128)[None, :]
    c["mask_d0"] = (i >= j).astype(np.float32)
    c["mask_d4"] = (j > i).astype(np.float32)
    e = np.zeros((32, S), np.float32)
    e[np.arange(S) // 64, np.arange(S)] = 1.0
    c["e_rows"] = e.astype(ml_dtypes.bfloat16)
    cc = np.arange(NCMP)[:, None]
    jj = np.arange(32)[None, :]
    lo = np.maximum(cc * 16, jj * 64)
    hi = np.minimum(cc * 16 + 32, (jj + 1) * 64)
    c["overlap"] = (np.maximum(hi - lo, 0) / 32.0).astype(np.float32)
    t = np.arange(S)[:, None]
    jb = np.arange(32)[None, :]
    cur = t // 64
    valid = jb <= cur
    forced = ((jb == 0) | (jb == cur) | (jb == cur - 1))
    val = valid.astype(np.float32)
    add = np.where(valid, 1000.0 * forced, -1.0).astype(np.float32)
    c["sel_val"] = np.ascontiguousarray(val.reshape(16, 128, 32).transpose(1, 0, 2))
    c["sel_add"] = np.ascontiguousarray(add.reshape(16, 128, 32).transpose(1, 0, 2))
    zc = np.zeros((16, 272), np.float32)
    zc[np.arange(16), np.arange(16) + 128] = 1.0
    c["zc"] = zc.astype(ml_dtypes.bfloat16)
    k = np.arange(16)[:, None]
    dist = i - 16 * (k - 8) - 31
    c["cmp_valid"] = (dist >= 0).astype(np.float32)
    oh2 = np.zeros((32, 16 * 128), np.float32)
    bk2 = t5_bucket_np(dist.reshape(-1))
    oh2[bk2, np.arange(16 * 128)] = 1.0
    oh2[31, :] -= 1.0
    c["oh_cmp"] = oh2
    return c


def dram_in(nc, name, shape, dt=F32):
    return nc.dram_tensor(name, list(shape), dt, kind="ExternalInput").ap()


class Prog:
    pass


def build(phases=("p0", "p1", "p2", "p3", "p4"), debug=False, nseq=NSEQ):
    nc = bass.Bass("TRN2", target_bir_lowering=False)
    P = Prog()
    P.nc = nc
    P.nseq = nseq
    kind_scr = "ExternalOutput" if debug else "Internal"

    def scr(name, shape, dt):
        return nc.dram_tensor(name, list(shape), dt, kind=kind_scr).ap()

    I = {}
    I["xT"] = dram_in(nc, "xT", [NSEQ, D, S])
    I["w_in"] = dram_in(nc, "w_in", [D, INW])
    I["w_out"] = dram_in(nc, "w_out", [D, D])
    I["w_g"] = dram_in(nc, "w_g", [D, DFF])
    I["w_u"] = dram_in(nc, "w_u", [D, DFF])
    I["w_d"] = dram_in(nc, "w_d", [DFF, D])
    I["g_mix"] = dram_in(nc, "g_mix", [128, 16])
    I["g_ffn"] = dram_in(nc, "g_ffn", [128, 16])
    I["g_fin"] = dram_in(nc, "g_fin", [128, 16])
    I["b_gate_bc"] = dram_in(nc, "b_gate_bc", [128, 48])
    I["g_attn_bc"] = dram_in(nc, "g_attn_bc", [128, 1024])
    I["rnn_vec"] = dram_in(nc, "rnn_vec", [128, 8, 10])
    I["rg_a_w"] = dram_in(nc, "rg_a_w", [16, 64, 64])
    I["rg_x_w"] = dram_in(nc, "rg_x_w", [16, 64, 64])
    I["ffn_vec"] = dram_in(nc, "ffn_vec", [128, NFC, 4])
    I["rel_bias"] = dram_in(nc, "rel_bias", [32, 16])
    I["cmp_w1"] = dram_in(nc, "cmp_w1", [2, 64, 32, 256])
    I["cmp_w2"] = dram_in(nc, "cmp_w2", [2, 128, 2, 64])
    I["cmp_peT"] = dram_in(nc, "cmp_peT", [2, 64, 32])
    I["ident_bf"] = dram_in(nc, "ident_bf", [128, 128], BF16)
    I["ones_bf"] = dram_in(nc, "ones_bf", [128, 128], BF16)
    I["oh"] = dram_in(nc, "oh", [32, GW])
    I["mask_d0"] = dram_in(nc, "mask_d0", [128, 128])
    I["mask_d4"] = dram_in(nc, "mask_d4", [128, 128])
    I["e_rows"] = dram_in(nc, "e_rows", [32, S], BF16)
    I["overlap"] = dram_in(nc, "overlap", [NCMP, 32])
    I["sel_val"] = dram_in(nc, "sel_val", [128, 16, 32])
    I["sel_add"] = dram_in(nc, "sel_add", [128, 16, 32])
    I["zc"] = dram_in(nc, "zc", [16, 272], BF16)
    I["cmp_valid"] = dram_in(nc, "cmp_valid", [16, 128])
    P.I = I

    outT = nc.dram_tensor("outT", [NSEQ, D, S], F32, kind="ExternalOutput").ap()
    P.outT = outT

    W = {}
    W["w_in"] = scr("w_in_b", [D, INW], BF16)
    W["w_out"] = scr("w_out_b", [8, 128, 16 * 256], BF16)
    W["w_g"] = scr("w_g_b", [NFC // 2, 128, 16 * 256], BF16)
    W["w_u"] = scr("w_u_b", [NFC // 2, 128, 16 * 256], BF16)
    W["w_d"] = scr("w_d_b", [16, 128, NFC * 128], BF16)
    P.W = W
    X = {}
    X["QT"] = scr("QT", [NSEQ, 1024, S], BF16)
    X["KC"] = scr("KC", [NSEQ, 512, S], BF16)
    X["KS"] = scr("KS", [NSEQ, 256, S], BF16)
    X["KW"] = scr("KW", [NSEQ, 256, S], BF16)
    X["RX"] = scr("RX", [NSEQ, 1024, S], F32)
    X["RY"] = scr("RY", [NSEQ, 1024, S], F32)
    X["VSW"] = scr("VSW", [NSEQ, 16, 128, 2, 4, 65], BF16)
    X["GATE"] = scr("GATE", [NSEQ, 16, 128, 48], F32)
    X["MIXR"] = scr("MIXR", [NSEQ, 1024, S], BF16)
    X["RSTDR"] = scr("RSTDR", [NSEQ, 128, S], F32)
    X["OATT"] = scr("OATT", [NSEQ, 16, 128, 1024], F32)
    X["MIXA"] = scr("MIXA", [NSEQ, 1024, S], BF16)
    X["RTAB"] = scr("RTAB", [16, 128, GW], F32)
    if debug:
        X["DBGDN"] = scr("DBGDN", [128, 16, 2, 128], BF16)
        X["DBGMB"] = scr("DBGMB", [16, 16, 128], BF16)
    P.X = X

    es = ExitStack()
    with es:
        mk = MK(nc, es)
        P.mk = mk
        P.wb = {k: Buf("wb_" + k) for k in W}
        P.xb = {k: [Buf(f"{k}{s}") for s in range(NSEQ)] for k in X}

        cst = ExitStack()
        with cst:
            C = {}
            C["ident"] = cst.enter_context(nc.sbuf_tensor("c_ident", [128, 128], BF16))
            C["ones"] = cst.enter_context(nc.sbuf_tensor("c_ones", [128, 128], BF16))
            P.C = C
            P.cb = Buf("consts")
            mk.dma("sp", C["ident"][:], I["ident_bf"][:, :], writes=[P.cb])
            mk.dma("sp", C["ones"][:], I["ones_bf"][:, :], writes=[P.cb])
            P.cb.w = dict(P.cb.w)

            if "p0" in phases:
                phase0(P)
            if "p1" in phases:
                for s in range(nseq):
                    phase1(P, s)
            if "p2" in phases:
                for s in range(nseq):
                    phase2(P, s)
            if "p3" in phases:
                phase3_setup(P)
                for s in range(nseq):
                    phase3(P, s)
            if "p4" in phases:
                phase4(P, nseq)
            mk.flush(final=True)
    return nc


def phase0(P):
    mk, I, W = P.mk, P.I, P.W
    P.cast_jobs = []
    ncp, cw, rb = 3, INW // 3, 512
    for r0 in range(0, D, rb):
        for cp in range(ncp):
            issue_cast(P, ("w_in", W["w_in"][r0:r0 + rb, cp * cw:(cp + 1) * cw],
                           I["w_in"][r0:r0 + rb, cp * cw:(cp + 1) * cw]))
    for name, nblk in (("w_out", 8), ("w_g", NFC // 2), ("w_u", NFC // 2)):
        for f0 in range(0, nblk, 8):
            f1 = min(nblk, f0 + 8)
            for c in range(16):
                src = I[name][c * 128:(c + 1) * 128, f0 * 256:f1 * 256].rearrange("p (f j) -> p f j", j=256)
                dst = W[name][f0:f1, :, c * 256:(c + 1) * 256].rearrange("f p j -> p f j")
                P.cast_jobs.append((name, dst, src))
    for fc in range(NFC):
        src = I["w_d"][fc * 128:(fc + 1) * 128, :].rearrange("p (d j) -> p d j", j=128)
        dst = W["w_d"][:, :, fc * 128:(fc + 1) * 128].rearrange("d p j -> p d j")
        P.cast_jobs.append(("w_d", dst, src))
    mk.flush()


def issue_cast(P, job, gate=None):
    mk = P.mk
    name, dst, src = job
    if gate is not None:
        mk._waits("pool", [gate], [])
    b = Buf()
    mk.dma("pool", dst, src, writes=[b], sems="cast")
    evs = P.wb[name].w
    for k, v in b.w.items():
        evs[k] = max(evs.get(k, 0), v)


def issue_casts(P, n, gate=None):
    for _ in range(n):
        if P.cast_jobs:
            issue_cast(P, P.cast_jobs.pop(0), gate)


def act_copy(out, in_, scale=1.0):
    return lambda e: e.activation(out=out, in_=in_, func=AF.Copy, scale=float(scale))


def dve_scale(out, in_, scale=1.0):
    return lambda e: e.tensor_scalar(out=out, in0=in_, scalar1=float(scale), scalar2=None, op0=ALU.mult)


def evac(mk, idx, out, in_, reads, writes, scale=1.0):
    if idx % 2 == 0:
        mk.op("act", act_copy(out, in_, scale), reads=reads, writes=writes)
    else:
        mk.op("dve", dve_scale(out, in_, scale), reads=reads, writes=writes)


def rms_rstd(mk, ps_ap, ps_buf, rt_ap, rt_buf, rstd_ap, rstd_buf, n):
    mk.op("act", lambda e: e.activation(out=rt_ap, in_=ps_ap, func=AF.Sqrt, scale=1.0 / n, bias=P_EPS[0]),
          reads=[ps_buf, P_EPS[1]], writes=[rt_buf])
    mk.op("dve", lambda e: e.reciprocal(out=rstd_ap, in_=rt_ap), reads=[rt_buf], writes=[rstd_buf])


P_EPS = [None, None]


def phase1(P, s):
    nc, mk, I, W, X, C = P.nc, P.mk, P.I, P.W, P.X, P.C
    mk.barrier()
    with ExitStack() as ts:
        def sb(name, shape, dt):
            return ts.enter_context(nc.sbuf_tensor(f"p1_{s}_{name}", shape, dt))

        yT = sb("yT", [128, 16, S], BF16)
        xin = [sb(f"xin{i}", [128, 16, 256], F32) for i in range(2)]
        sq = [sb(f"sq{i}", [128, 16, 256], BF16) for i in range(2)]
        rt = sb("rt", [128, 256], F32)
        rstd = sb("rstd", [128, 256], F32)
        gmix = sb("gmix", [128, 16], F32)
        epst = sb("eps", [128, 1], F32)
        WB = [sb(f"WB{i}", [128, 16, 512], BF16) for i in range(2)]
        Wtok = sb("Wtok", [128, 16, 560], BF16)
        stb = [sb(f"stb{i}", [128, S], BF16) for i in range(2)]
        stf = [sb(f"stf{i}", [128, S], F32) for i in range(2)]
        Vst = [sb(f"Vst{i}", [128, 2, 4, 65], BF16) for i in range(2)]
        gtmp = [sb(f"gtmp{i}", [128, 48], F32) for i in range(2)]
        gst = [sb(f"gst{i}", [128, 48], F32) for i in range(2)]
        bgate = sb("bgate", [128, 48], F32)
        ps = [ts.enter_context(nc.psum_tensor(f"p1_{s}_ps{i}", [128, 512], F32)) for i in range(8)]
        psb = bufs(8, "ps")

        b_small = Buf("small")
        mk.dma("sp", gmix[:], I["g_mix"][:, :], writes=[b_small])
        b_bg = Buf("bgate")
        mk.dma("sp", bgate[:], I["b_gate_bc"][:, :], writes=[b_bg])
        b_eps = Buf("eps")
        mk.op("dve", lambda e: e.memset(epst[:], EPS), writes=[b_eps])
        P_EPS[0] = epst[:]
        P_EPS[1] = b_eps
        b_vst = bufs(2, "vst")
        for i in range(2):
            mk.op("dve", lambda e, i=i: e.memset(Vst[i][:], 1.0), writes=[b_vst[i]])

        b_xin = bufs(2, "xin")
        b_sq = bufs(2, "sq")
        b_rt = Buf("rt")
        b_rstd = Buf("rstd")
        b_yT = bufs(8, "yT")
        xsrc = I["xT"][s].rearrange("(c p) t -> p c t", p=128)
        pcnt = 0
        for j in range(8):
            k = j % 2
            t0 = j * 256
            mk.dma("sp", xin[k][:], xsrc[:, :, t0:t0 + 256], writes=[b_xin[k]])
            mk.op("act", lambda e, k=k: e.activation(out=sq[k][:], in_=xin[k][:], func=AF.Square),
                  reads=[b_xin[k]], writes=[b_sq[k]])
            pb = 6 + (j % 2)
            for c in range(16):
                mk.op("pe", lambda e, k=k, c=c, pb=pb: e.matmul(ps[pb][:, 0:256], lhsT=C["ones"][:], rhs=sq[k][:, c, :],
                                                                 start=(c == 0), stop=(c == 15)),
                      reads=[b_sq[k], P.cb], writes=[psb[pb]])
            rms_rstd(mk, ps[pb][:, 0:256], psb[pb], rt[:], b_rt, rstd[:], b_rstd, float(D))
            for c in range(16):
                mk.op("dve", lambda e, k=k, c=c, t0=t0: e.scalar_tensor_tensor(
                    out=yT[:, c, t0:t0 + 256], in0=xin[k][:, c, :], scalar=gmix[:, c:c + 1], in1=rstd[:],
                    op0=ALU.mult, op1=ALU.mult), reads=[b_xin[k], b_rstd, b_small], writes=[b_yT[j]])

        wcols = [[(0, 512)], [(512, 1024)], [(1024, 1536)], [(1536, 1792), (2048, 2304)],
                 [(2608, 3120)], [(3120, 3632)], [(3632, 4144)], [(4144, 4656)]]
        b_WB = bufs(2, "WB")
        wsrc = W["w_in"].rearrange("(c p) n -> p c n", p=128)

        def load_w(w):
            k = w % 2
            o = 0
            for (c0, c1) in wcols[w]:
                mk.dma("sp", WB[k][:, :, o:o + (c1 - c0)], wsrc[:, :, c0:c1], reads=[P.wb["w_in"]], writes=[b_WB[k]])
                o += c1 - c0

        b_Wtok = Buf("Wtok")
        b_stb = [bufs(4, f"stb{i}_") for i in range(2)]
        b_stf = [bufs(4, f"stf{i}_") for i in range(2)]
        load_w(0)
        nb = 0
        nbf = 0
        ecnt = 0
        for w in range(8):
            if w + 1 < 8:
                load_w(w + 1)
            elif True:
                o = 0
                for (c0, c1) in ((1792, 2048), (2304, 2560), (2560, 2608)):
                    mk.dma("sp", Wtok[:, :, o:o + (c1 - c0)], wsrc[:, :, c0:c1], reads=[P.wb["w_in"]], writes=[b_Wtok])
                    o += c1 - c0
            k = w % 2
            for m in range(4):
                ch = 4 * w + m
                isf = ch >= 16
                if isf:
                    sidx = nbf % 2
                    nbf += 1
                    stage, sbufs_ = stf[sidx], b_stf[sidx]
                else:
                    sidx = nb % 2
                    nb += 1
                    stage, sbufs_ = stb[sidx], b_stb[sidx]
                for tg in range(4):
                    pb = pcnt % 6
                    pcnt += 1
                    for c in range(16):
                        mk.op("pe", lambda e, k=k, c=c, m=m, tg=tg, pb=pb: e.matmul(
                            ps[pb][:], lhsT=WB[k][:, c, m * 128:(m + 1) * 128], rhs=yT[:, c, tg * 512:(tg + 1) * 512],
                            start=(c == 0), stop=(c == 15)),
                            reads=[b_WB[k], b_yT[2 * tg], b_yT[2 * tg + 1]], writes=[psb[pb]])
                    evac(mk, ecnt, stage[:, tg * 512:(tg + 1) * 512], ps[pb][:], [psb[pb]], [sbufs_[tg]],
                         scale=(0.125 if ch < 8 else 1.0))
                    ecnt += 1
                if ch < 8:
                    dst = X["QT"][s, ch * 128:(ch + 1) * 128, :]
                    db = P.xb["QT"][s]
                elif ch < 12:
                    dst = X["KC"][s, (ch - 8) * 128:(ch - 7) * 128, :]
                    db = P.xb["KC"][s]
                elif ch < 14:
                    dst = X["KS"][s, (ch - 12) * 128:(ch - 11) * 128, :]
                    db = P.xb["KS"][s]
                elif ch < 16:
                    dst = X["KW"][s, (ch - 14) * 128:(ch - 13) * 128, :]
                    db = P.xb["KW"][s]
                elif ch < 24:
                    dst = X["RX"][s, (ch - 16) * 128:(ch - 15) * 128, :]
                    db = P.xb["RX"][s]
                else:
                    dst = X["RY"][s, (ch - 24) * 128:(ch - 23) * 128, :]
                    db = P.xb["RY"][s]
                dmab = Buf()
                mk.dma("sp", dst, stage[:], reads=sbufs_, writes=[dmab])
                for kk, vv in dmab.w.items():
                    db.w[kk] = max(db.w.get(kk, 0), vv)
            issue_casts(P, 12, gate=sbufs_[3])

        b_gtmp = bufs(2, "gtmp")
        b_gst = bufs(2, "gst")
        for tt in range(16):
            k = tt % 2
            pv = pcnt % 6
            pcnt += 1
            pg = 6 + (tt % 2)
            for c in range(16):
                lhs = yT[:, c, tt * 128:(tt + 1) * 128]
                mk.op("pe", lambda e, c=c, pv=pv, lhs=lhs: e.matmul(ps[pv][:], lhsT=lhs, rhs=Wtok[:, c, 0:512],
                                                                     start=(c == 0), stop=(c == 15)),
                      reads=[b_Wtok, b_yT[tt // 2]], writes=[psb[pv]])
                mk.op("pe", lambda e, c=c, pg=pg, lhs=lhs: e.matmul(ps[pg][:, 0:48], lhsT=lhs, rhs=Wtok[:, c, 512:560],
                                                                     start=(c == 0), stop=(c == 15)),
                      reads=[b_Wtok, b_yT[tt // 2]], writes=[psb[pg]])
            evac(mk, tt, Vst[k][:, :, :, 0:64], ps[pv][:].rearrange("p (a g d) -> p a g d", a=2, g=4),
                 [psb[pv]], [b_vst[k]])
            mk.op("dve", lambda e, k=k, pg=pg: e.tensor_tensor(out=gtmp[k][:], in0=ps[pg][:, 0:48], in1=bgate[:],
                                                                op=ALU.add),
                  reads=[psb[pg], b_bg], writes=[b_gtmp[k]])
            mk.op("act", lambda e, k=k: e.activation(out=gst[k][:], in_=gtmp[k][:], func=AF.Sigmoid),
                  reads=[b_gtmp[k]], writes=[b_gst[k]])
            for (dst, src, sbuf_, key) in ((X["VSW"][s, tt], Vst[k][:], b_vst[k], "VSW"),
                                           (X["GATE"][s, tt], gst[k][:], b_gst[k], "GATE")):
                dmab = Buf()
                mk.dma("sp", dst, src, reads=[sbuf_], writes=[dmab])
                db = P.xb[key][s]
                for kk, vv in dmab.w.items():
                    db.w[kk] = max(db.w.get(kk, 0), vv)
        if s == P.nseq - 1:
            issue_casts(P, 1000)
        mk.flush()


def pc(v, nchunk):
    return np.ascontiguousarray(np.asarray(v, np.float32).reshape(nchunk, 128).T)


def make_in_maps(inp, n_cores=N_CORES):
    f = lambda k: np.asarray(inp[k], np.float32)
    shared = {}
    shared["w_in"] = np.ascontiguousarray(f("w_in")[0])
    shared["w_out"] = np.ascontiguousarray(f("w_out")[0])
    shared["w_g"] = np.ascontiguousarray(f("w_ffn_gate")[0])
    shared["w_u"] = np.ascontiguousarray(f("w_ffn_up")[0])
    shared["w_d"] = np.ascontiguousarray(f("w_ffn_down")[0])
    shared["g_mix"] = pc(f("mix_norm_g")[0], 16)
    shared["g_ffn"] = pc(f("ffn_norm_g")[0], 16)
    shared["g_fin"] = pc(f("final_norm_g"), 16)
    shared["b_gate_bc"] = np.ascontiguousarray(np.broadcast_to(f("b_gate")[0][None, :], (128, 48)))
    shared["g_attn_bc"] = np.ascontiguousarray(np.broadcast_to(f("attn_out_g")[0][None, :], (128, 1024)))
    rv = np.zeros((128, 8, 10), np.float32)
    cw = f("rnn_conv_w")[0]
    for k in range(4):
        rv[:, :, k] = pc(cw[k], 8)
    rv[:, :, 4] = pc(f("rnn_conv_b")[0], 8)
    rv[:, :, 5] = pc(f("rg_a_b")[0], 8)
    rv[:, :, 6] = pc(f("rg_x_b")[0], 8)
    rv[:, :, 7] = pc(f("rg_lambda")[0], 8)
    rv[:, :, 8] = pc(f("rnn_out_g")[0], 8)
    shared["rnn_vec"] = rv
    shared["rg_a_w"] = np.ascontiguousarray(f("rg_a_w")[0])
    shared["rg_x_w"] = np.ascontiguousarray(f("rg_x_w")[0])
    fv = np.zeros((128, NFC, 4), np.float32)
    fw = f("ffn_conv_w")[0]
    for k in range(3):
        fv[:, :, k] = pc(fw[k], NFC)
    fv[:, :, 3] = pc(f("ffn_conv_b")[0], NFC)
    shared["ffn_vec"] = fv
    shared["rel_bias"] = np.ascontiguousarray(f("rel_bias"))
    w1 = np.stack([f("cmp_k_w1")[0], f("cmp_v_w1")[0]])
    shared["cmp_w1"] = np.ascontiguousarray(w1.reshape(2, 32, 64, 256).transpose(0, 2, 1, 3))
    w2 = np.stack([f("cmp_k_w2")[0], f("cmp_v_w2")[0]])
    shared["cmp_w2"] = np.ascontiguousarray(w2.reshape(2, 2, 128, 64).transpose(0, 2, 1, 3))
    pe = np.stack([f("cmp_pe_k")[0], f("cmp_pe_v")[0]])
    shared["cmp_peT"] = np.ascontiguousarray(pe.transpose(0, 2, 1))
    shared.update(host_constants())
    x = f("x")
    maps = []
    for c in range(n_cores):
        m = dict(shared)
        m["xT"] = np.ascontiguousarray(x[c * NSEQ:(c + 1) * NSEQ].transpose(0, 2, 1))
        maps.append(m)
    return maps


_NC_CACHE = {}


def kernel(**inputs):
    if "nc" not in _NC_CACHE:
        _NC_CACHE["nc"] = build()
    nc = _NC_CACHE["nc"]
    maps = make_in_maps(inputs)
    res = run_bass_kernel_spmd(nc, maps, core_ids=list(range(N_CORES)))
    outs = [np.asarray(r["outT"]).transpose(0, 2, 1) for r in res.results]
    return np.ascontiguousarray(np.concatenate(outs, axis=0).astype(np.float32))


import os
P2E = os.environ.get("P2E", "dve")


def phase2(P, s):
    nc, mk, I, W, X, C = P.nc, P.mk, P.I, P.W, P.X, P.C
    mk.barrier()
    with ExitStack() as ts:
        def sb(name, shape, dt):
            return ts.enter_context(nc.sbuf_tensor(f"p2_{s}_{name}", shape, dt))

        rv = sb("rv", [128, 8, 10], F32)
        cl = sb("cl", [128, 8, 2], F32)
        tmp8 = sb("tmp8", [128, 8], F32)
        cst1 = sb("cst1", [128, 2], F32)
        BDa = sb("BDa", [128, 8, 128], BF16)
        BDx = sb("BDx", [128, 8, 128], BF16)
        rxp = [sb(f"rxp{i}", [128, 3 + S], F32) for i in range(2)]
        ryt = [sb(f"ryt{i}", [128, S], F32) for i in range(3)]
        xr_ = [sb(f"xr{i}", [128, S], F32) for i in range(2)]
        xrb_ = [sb(f"xrb{i}", [128, S], BF16) for i in range(2)]
        rr_ = [sb(f"rr{i}", [128, S], F32) for i in range(2)]
        gi_ = [sb(f"gi{i}", [128, S], F32) for i in range(2)]
        aa_ = [sb(f"aa{i}", [128, S], F32) for i in range(2)]
        mm_ = [sb(f"mm{i}", [128, S], F32) for i in range(2)]
        hh_ = [sb(f"hh{i}", [128, S], F32) for i in range(2)]
        sqo_ = [sb(f"sqo{i}", [128, S], BF16) for i in range(2)]
        mst = [sb(f"mst{i}", [128, S], BF16) for i in range(2)]
        rstdr = sb("rstdr", [128, S], F32)
        ps = [ts.enter_context(nc.psum_tensor(f"p2_{s}_ps{i}", [128, 512], F32)) for i in range(8)]
        psb = bufs(8, "ps")

        b_rv = Buf("rv")
        mk.dma("sp", rv[:], I["rnn_vec"][:, :, :], writes=[b_rv])
        b_cst = Buf("cst")
        mk.op("dve", lambda e: e.memset(cst1[:, 0:1], 1.0), writes=[b_cst])
        mk.op("dve", lambda e: e.memset(cst1[:, 1:2], EPS), writes=[b_cst])
        b_cl = Buf("cl")
        b_t8 = Buf("t8")
        mk.op("act", lambda e: e.activation(out=tmp8[:], in_=rv[:, :, 7], func=AF.Exp, scale=-1.0),
              reads=[b_rv], writes=[b_t8])
        mk.op("act", lambda e: e.activation(out=tmp8[:], in_=tmp8[:], func=AF.Ln, bias=cst1[:, 0:1]),
              reads=[b_t8, b_cst], writes=[b_t8])
        mk.op("dve", lambda e: e.tensor_scalar(out=cl[:, :, 0], in0=tmp8[:], scalar1=-8.0, scalar2=None, op0=ALU.mult),
              reads=[b_t8], writes=[b_cl])
        mk.op("dve", lambda e: e.tensor_scalar(out=cl[:, :, 1], in0=tmp8[:], scalar1=-16.0, scalar2=None, op0=ALU.mult),
              reads=[b_t8], writes=[b_cl])
        b_bd = Buf("bd")
        mk.op("dve", lambda e: e.memset(BDa[:], 0.0), writes=[b_bd])
        mk.op("dve", lambda e: e.memset(BDx[:], 0.0), writes=[b_bd])
        for (bd, key) in ((BDa, "rg_a_w"), (BDx, "rg_x_w")):
            src = I[key].rearrange("(c two) i j -> two i c j", two=2)
            mk.dma("pool", bd[0:64, :, 0:64], src[0], writes=[b_bd])
            mk.dma("pool", bd[64:128, :, 64:128], src[1], writes=[b_bd])
        b_rxp = bufs(2, "rxp")
        b_ry = bufs(3, "ry")
        for i in range(2):
            mk.op("dve", lambda e, i=i: e.memset(rxp[i][:, 0:3], 0.0), writes=[b_rxp[i]])
        b_xr_, b_xrb_, b_rr_, b_gi_, b_aa_, b_mm_, b_hh_, b_sqo_ = (bufs(2, n) for n in
                                                                      ("xr", "xrb", "rr", "gi", "aa", "mm", "hh", "sqo"))
        b_mst = bufs(2, "mst")

        def load(c):
            k = c % 2
            mk.dma("sp", rxp[k][:, 3:3 + S], X["RX"][s, c * 128:(c + 1) * 128, :], reads=[P.xb["RX"][s]],
                   writes=[b_rxp[k]])
            mk.dma("sp", ryt[c % 3][:], X["RY"][s, c * 128:(c + 1) * 128, :], reads=[P.xb["RY"][s]],
                   writes=[b_ry[c % 3]])

        load(0)

        def front(c):
            k = c % 2
            if c + 1 < 8:
                load(c + 1)
            xr, xrb, rr, gi, aa, mm, hh, sqo = (t_[k] for t_ in (xr_, xrb_, rr_, gi_, aa_, mm_, hh_, sqo_))
            b_xr, b_xrb, b_rr, b_gi, b_aa, b_mm, b_hh, b_sqo = (t_[k] for t_ in (b_xr_, b_xrb_, b_rr_, b_gi_, b_aa_,
                                                                                  b_mm_, b_hh_, b_sqo_))
            mk.op("act", lambda e, k=k, c=c: e.activation(out=xr[:], in_=rxp[k][:, 3:3 + S], func=AF.Identity,
                                                          scale=rv[:, c, 3:4], bias=rv[:, c, 4:5]),
                  reads=[b_rxp[k], b_rv], writes=[b_xr])
            for kk in range(3):
                mk.op("dve", lambda e, k=k, c=c, kk=kk: e.scalar_tensor_tensor(
                    out=xr[:], in0=rxp[k][:, kk:kk + S], scalar=rv[:, c, kk:kk + 1], in1=xr[:],
                    op0=ALU.mult, op1=ALU.add), reads=[b_rxp[k], b_rv, b_xr], writes=[b_xr])
            mk.op("act", act_copy(xrb[:], xr[:]), reads=[b_xr], writes=[b_xrb])
            for tg in range(4):
                sl = slice(tg * 512, (tg + 1) * 512)
                pr = tg % 2
                pg = 2 + tg % 2
                mk.op("pe", lambda e, c=c, sl=sl, pr=pr: e.matmul(ps[pr][:], lhsT=BDa[:, c, :], rhs=xrb[:, sl],
                                                                 start=True, stop=True),
                      reads=[b_bd, b_xrb], writes=[psb[pr]])
                mk.op("pe", lambda e, c=c, sl=sl, pg=pg: e.matmul(ps[pg][:], lhsT=BDx[:, c, :], rhs=xrb[:, sl],
                                                                 start=True, stop=True),
                      reads=[b_bd, b_xrb], writes=[psb[pg]])
                mk.op("act", lambda e, c=c, sl=sl, pr=pr: e.activation(out=rr[:, sl], in_=ps[pr][:], func=AF.Sigmoid,
                                                                      bias=rv[:, c, 5:6]),
                      reads=[psb[pr], b_rv], writes=[b_rr])
                mk.op("act", lambda e, c=c, sl=sl, pg=pg: e.activation(out=gi[:, sl], in_=ps[pg][:], func=AF.Sigmoid,
                                                                      bias=rv[:, c, 6:7]),
                      reads=[psb[pg], b_rv], writes=[b_gi])
            mk.op("act", lambda e, c=c: e.activation(out=aa[:], in_=rr[:], func=AF.Exp, scale=cl[:, c, 0:1]),
                  reads=[b_rr, b_cl], writes=[b_aa])
            mk.op("act", lambda e, c=c: e.activation(out=mm[:], in_=rr[:], func=AF.Exp, scale=cl[:, c, 1:2]),
                  reads=[b_rr, b_cl], writes=[b_mm])
            mk.op("act", lambda e: e.activation(out=mm[:], in_=mm[:], func=AF.Sqrt, scale=-1.0, bias=cst1[:, 0:1]),
                  reads=[b_mm, b_cst], writes=[b_mm])
            mk.op("act", lambda e, c=c: e.activation(out=ryt[c % 3][:], in_=ryt[c % 3][:], func=AF.Gelu_apprx_tanh),
                  reads=[b_ry[c % 3]], writes=[b_ry[c % 3]])

        def back(c):
            k = c % 2
            xr, xrb, rr, gi, aa, mm, hh, sqo = (t_[k] for t_ in (xr_, xrb_, rr_, gi_, aa_, mm_, hh_, sqo_))
            b_xr, b_xrb, b_rr, b_gi, b_aa, b_mm, b_hh, b_sqo = (t_[k] for t_ in (b_xr_, b_xrb_, b_rr_, b_gi_, b_aa_,
                                                                                  b_mm_, b_hh_, b_sqo_))
            mk.op(P2E, lambda e: e.tensor_tensor(out=gi[:], in0=gi[:], in1=xr[:], op=ALU.mult),
                  reads=[b_gi, b_xr], writes=[b_gi])
            mk.op(P2E, lambda e: e.tensor_tensor(out=gi[:], in0=gi[:], in1=mm[:], op=ALU.mult),
                  reads=[b_gi, b_mm], writes=[b_gi])
            mk.op("dve", lambda e: e.tensor_tensor_scan(out=hh[:], data0=aa[:], data1=gi[:], initial=0.0,
                                                        op0=ALU.mult, op1=ALU.add),
                  reads=[b_aa, b_gi], writes=[b_hh])
            mk.op("dve", lambda e, c=c: e.tensor_tensor(out=hh[:], in0=hh[:], in1=ryt[c % 3][:], op=ALU.mult),
                  reads=[b_hh, b_ry[c % 3]], writes=[b_hh])
            mk.op("act", lambda e: e.activation(out=sqo[:], in_=hh[:], func=AF.Square), reads=[b_hh], writes=[b_sqo])
            for tg in range(4):
                sl = slice(tg * 512, (tg + 1) * 512)
                mk.op("pe", lambda e, c=c, sl=sl, tg=tg: e.matmul(ps[4 + tg][:], lhsT=C["ones"][:], rhs=sqo[:, sl],
                                                                 start=(c == 0), stop=(c == 7)),
                      reads=[b_sqo, P.cb], writes=[psb[4 + tg]])
            mk.op("dve", lambda e, k=k, c=c: e.tensor_scalar(out=mst[k][:], in0=hh[:], scalar1=rv[:, c, 8:9],
                                                             scalar2=None, op0=ALU.mult),
                  reads=[b_hh, b_rv], writes=[b_mst[k]])
            dmab = Buf()
            mk.dma("pool", X["MIXR"][s, c * 128:(c + 1) * 128, :], mst[k][:], reads=[b_mst[k]], writes=[dmab])
            db = P.xb["MIXR"][s]
            for kk_, vv in dmab.w.items():
                db.w[kk_] = max(db.w.get(kk_, 0), vv)
        front(0)
        for c in range(8):
            if c + 1 < 8:
                front(c + 1)
            back(c)
        b_rs = Buf("rstdr")
        for tg in range(4):
            sl = slice(tg * 512, (tg + 1) * 512)
            mk.op("act", lambda e, sl=sl, tg=tg: e.activation(out=rstdr[:, sl], in_=ps[4 + tg][:], func=AF.Sqrt,
                                                              scale=1.0 / 1024.0, bias=cst1[:, 1:2]),
                  reads=[psb[4 + tg], b_cst], writes=[b_rs])
        mk.op("dve", lambda e: e.reciprocal(out=rstdr[:], in_=rstdr[:]), reads=[b_rs], writes=[b_rs])
        mk.dma("pool", X["RSTDR"][s], rstdr[:], reads=[b_rs], writes=[P.xb["RSTDR"][s]])
        mk.flush()


def merge_ev(dst_buf, src_buf):
    for kk, vv in src_buf.w.items():
        dst_buf.w[kk] = max(dst_buf.w.get(kk, 0), vv)


def phase3_setup(P):
    pass


def phase3(P, s):
    if s == 0:
        phase3_all(P)


def phase3_all(P):
    nc, mk, I, W, X, C = P.nc, P.mk, P.I, P.W, P.X, P.C
    nseq = P.nseq
    mk.barrier()
    with ExitStack() as ts:
        def sb(name, shape, dt):
            return ts.enter_context(nc.sbuf_tensor(f"p3_{name}", shape, dt))

        DNb = sb("DNb", [128, 16, 2, 128], BF16)
        DN4b = sb("DN4b", [128, 4, 128], BF16)
        Mb = sb("Mb", [16, 16, 128], BF16)
        zc = sb("zc", [16, 272], BF16)
        W1 = sb("W1", [64, 2, 32, 256], BF16)
        W2 = sb("W2", [128, 2, 2, 64], BF16)
        peT = sb("peT", [64, 2, 34], BF16)
        pebias = sb("pebias", [128, 2, 2], F32)
        VAL = sb("VAL", [128, 16, 32], F32)
        ADD = sb("ADD", [128, 16, 32], F32)
        gbc = sb("gbc", [128, 1024], F32)
        KsA = sb("KsA", [96, S], BF16)
        VCA = sb("VCA", [128, 4, 97], BF16)
        NSP = [sb(f"NSP{i}", [128, 96], BF16) for i in range(2)]
        cst1 = sb("cst1", [128, 2], F32)
        ts2 = ExitStack()

        def sb2(name, shape, dt):
            return ts2.enter_context(nc.sbuf_tensor(f"p3t_{name}", shape, dt))

        tab = sb2("tab", [32, 16], F32)
        tabb = [sb2(f"tabb{i}", [32, 128], F32) for i in range(2)]
        oh = sb2("oh", [32, GW], F32)
        rst = [sb2(f"rst{i}", [128, GW], F32) for i in range(2)]
        dn01 = sb2("dn01", [128, 16, 2, 128], F32)
        m0 = sb2("m0", [128, 128], F32)
        negm0 = sb2("negm0", [128, 128], F32)
        m4 = sb2("m4", [128, 128], F32)
        mbf = sb2("mbf", [16, 16, 128], F32)
        cvm = sb2("cvm", [16, 128], F32)
        negcv = sb2("negcv", [16, 128], F32)
        ps = [ts.enter_context(nc.psum_tensor(f"p3_ps{i}", [128, 512], F32)) for i in range(8)]
        psb = bufs(8, "ps")

        b_c = Buf("p3c")
        for (t_, src) in ((tab, I["rel_bias"]), (oh, I["oh"]), (m0, I["mask_d0"]), (m4, I["mask_d4"]),
                          (cvm, I["cmp_valid"]), (zc, I["zc"]), (VAL, I["sel_val"]), (ADD, I["sel_add"]),
                          (gbc, I["g_attn_bc"])):
            b = Buf()
            mk.dma("sp", t_[:], src, writes=[b])
            merge_ev(b_c, b)
        b = Buf()
        mk.dma("sp", KsA[64:96, :], I["e_rows"][:, :], writes=[b])
        merge_ev(b_c, b)
        for i in range(2):
            b = Buf()
            mk.op("dve", lambda e, i=i: e.memset(NSP[i][:], 0.0), writes=[b])
            merge_ev(b_c, b)
        b = Buf()
        mk.op("dve", lambda e: e.memset(cst1[:, 0:1], 1.0), writes=[b])
        mk.op("dve", lambda e: e.memset(cst1[:, 1:2], EPS), writes=[b])
        merge_ev(b_c, b)
        b_vca = Buf("vca")
        mk.op("dve", lambda e: e.memset(VCA[:], 1.0), writes=[b_vca])
        for g in range(4):
            mk.dma("pool", VCA[0:NCMP, g, 65:97], I["overlap"][:, :], writes=[b_vca])
        b_w1 = Buf("w1")
        mk.dma("pool", W1[:, 0], I["cmp_w1"][0], writes=[b_w1])
        mk.dma("pool", W1[:, 1], I["cmp_w1"][1], writes=[b_w1])
        mk.dma("pool", W2[:], I["cmp_w2"].rearrange("kv p m e -> p kv m e"), writes=[b_w1])
        mk.op("dve", lambda e: e.memset(peT[:], 0.0), writes=[b_w1])
        mk.dma("pool", peT[:, :, 0:32], I["cmp_peT"].rearrange("kv d l -> d kv l"), writes=[b_w1])

        b_tabb = bufs(2, "tabb")
        b_rst = bufs(2, "rst")
        b_rtab = Buf("rtab")
        for h in range(16):
            k = h % 2
            mk.op("dve", lambda e, k=k, h=h: e.tensor_copy(out=tabb[k][:], in_=tab[:, h:h + 1].to_broadcast([32, 128])),
                  reads=[b_c], writes=[b_tabb[k]])
            mk.op("pe", lambda e, k=k: e.matmul(ps[k][:, 0:GW], lhsT=tabb[k][:], rhs=oh[:], start=True, stop=True),
                  reads=[b_tabb[k], b_c], writes=[psb[k]])
            mk.op("act", act_copy(rst[k][:], ps[k][:, 0:GW]), reads=[psb[k]], writes=[b_rst[k]])
            b = Buf()
            mk.dma("sp", X["RTAB"][h], rst[k][:], reads=[b_rst[k]], writes=[b])
            merge_ev(b_rtab, b)
        b_dn = Buf("dn")
        rt_t = X["RTAB"].tensor
        mk.dma("sp", dn01[:], bass.AP(rt_t, 127, [[GW - 1, 128], [128 * GW, 16], [128, 2], [1, 128]]),
               reads=[b_rtab], writes=[b_dn])
        b_mb = Buf("mbf")
        mk.dma("sp", mbf[:], bass.AP(rt_t, 224, [[GW - 16, 16], [128 * GW, 16], [1, 128]]), reads=[b_rtab],
               writes=[b_mb])
        b_m = Buf("masks")
        mk.op("dve", lambda e: e.tensor_scalar(out=negm0[:], in0=m0[:], scalar1=-1.0, scalar2=-NEG, op0=ALU.add,
                                               op1=ALU.mult), reads=[b_c], writes=[b_m])
        mk.op("dve", lambda e: e.tensor_scalar(out=m4[:], in0=m4[:], scalar1=-1.0, scalar2=-NEG, op0=ALU.add,
                                               op1=ALU.mult), reads=[b_c], writes=[b_m])
        mk.op("dve", lambda e: e.tensor_scalar(out=negcv[:], in0=cvm[:], scalar1=-1.0, scalar2=-NEG, op0=ALU.add,
                                               op1=ALU.mult), reads=[b_c], writes=[b_m])
        b_DN = Buf("DN")
        mk.op("dve", lambda e: e.tensor_tensor(out=dn01[:, :, 0, :], in0=dn01[:, :, 0, :],
                                               in1=m0[:].unsqueeze(1).to_broadcast([128, 16, 128]), op=ALU.mult),
              reads=[b_dn, b_c], writes=[b_dn])
        mk.op("dve", lambda e: e.tensor_tensor(out=DNb[:, :, 0, :], in0=dn01[:, :, 0, :],
                                               in1=negm0[:].unsqueeze(1).to_broadcast([128, 16, 128]), op=ALU.add),
              reads=[b_dn, b_m], writes=[b_DN])
        mk.op("dve", lambda e: e.tensor_copy(out=DNb[:, :, 1, :], in_=dn01[:, :, 1, :]), reads=[b_dn], writes=[b_DN])
        mk.op("dve", lambda e: e.tensor_copy(out=DN4b[:], in_=m4[:].unsqueeze(1).to_broadcast([128, 4, 128])),
              reads=[b_m], writes=[b_DN])
        mk.op("dve", lambda e: e.tensor_tensor(out=mbf[:], in0=mbf[:],
                                               in1=cvm[:].unsqueeze(1).to_broadcast([16, 16, 128]), op=ALU.mult),
              reads=[b_mb, b_c], writes=[b_mb])
        mk.op("dve", lambda e: e.tensor_tensor(out=Mb[:], in0=mbf[:],
                                               in1=negcv[:].unsqueeze(1).to_broadcast([16, 16, 128]), op=ALU.add),
              reads=[b_mb, b_m], writes=[b_DN])
        b_pb = Buf("pebias")
        for kv in range(2):
            for mc in range(2):
                pb = 2 + mc
                for l in range(32):
                    mk.op("pe", lambda e, kv=kv, mc=mc, l=l, pb=pb: e.matmul(
                        ps[pb][:, 0:2], lhsT=W1[:, kv, l, mc * 128:(mc + 1) * 128], rhs=peT[:, kv, l:l + 2],
                        start=(l == 0), stop=(l == 31)), reads=[b_w1], writes=[psb[pb]])
                mk.op("act", act_copy(pebias[:, kv, mc:mc + 1], ps[pb][:, 0:1]), reads=[psb[pb]], writes=[b_pb])

        if "DBGDN" in X:
            mk.dma("sp", X["DBGDN"], DNb[:], reads=[b_DN], writes=[Buf()])
            mk.dma("sp", X["DBGMB"], Mb[:], reads=[b_DN], writes=[Buf()])
        mk.flush()
        ts2.close()
        L = dict(locals())
        for s in range(nseq):
            attention_seq(P, s, L)


def attention_seq(P, s, L):
    nc, mk, I, W, X, C = P.nc, P.mk, P.I, P.W, P.X, P.C
    ps, psb = L["ps"], L["psb"]
    b_c, b_DN, b_w1, b_pb, b_vca = L["b_c"], L["b_DN"], L["b_w1"], L["b_pb"], L["b_vca"]
    W1, W2, pebias, VCA, KsA, zc, Mb, DNb, DN4b = (L[k] for k in ("W1", "W2", "pebias", "VCA", "KsA", "zc", "Mb",
                                                                 "DNb", "DN4b"))
    VAL, ADD, gbc, NSP, cst1 = L["VAL"], L["ADD"], L["gbc"], L["NSP"], L["cst1"]
    ident = C["ident"]
    with ExitStack() as ts:
        def sb(name, shape, dt):
            return ts.enter_context(nc.sbuf_tensor(f"p3s_{s}_{name}", shape, dt))

        big = sb("big", [128, 16384], BF16)
        KCt = big[0:64, :].rearrange("p (kv g t) -> p kv g t", kv=2, g=4)
        HT = sb("HT", [128, 2, 2, 508], BF16)
        KCMP = sb("KCMP", [64, 4, NCMP], BF16)
        Gt = sb("Gt", [128, 16, 48], F32)
        QA = sb("QA", [96, 4, S], BF16)
        KwT = sb("KwT", [64, S], BF16)
        Vsw = sb("Vsw", [128, 16, 2, 65], BF16)
        PTc = [sb(f"PTc{i}", [128, 512], BF16) for i in range(2)]
        PTs = [big[:, i * 8192:(i + 1) * 8192].rearrange("p (k n) -> p k n", k=16) for i in range(2)]
        PTw = [sb(f"PTw{i}", [128, 5, 512], BF16) for i in range(2)]
        ocmp = sb("ocmp", [128, 16, 4, 64], F32)
        oacc = [sb(f"oacc{i}", [128, 4, 64], F32) for i in range(2)]
        rs = sb("rs", [128, 4], F32)
        rinv = sb("rinv", [128, 4], F32)
        coef = sb("coef", [128, 4], F32)
        coefc = [sb(f"coefc{i}", [128, 4], F32) for i in range(2)]
        imp = sb("imp", [128, 32], F32)
        score = sb("score", [128, 32], F32)
        sc2 = sb("sc2", [128, 32], F32)
        m8a = sb("m8a", [128, 8], F32)
        m8b = sb("m8b", [128, 8], F32)
        thr = sb("thr", [128, 1], F32)
        selt = sb("selt", [128, 32], F32)
        oat = [sb(f"oat{i}", [128, 1024], F32) for i in range(2)]
        junk = sb("junk", [128, 1024], BF16)
        ssq = sb("ssq", [128, 1], F32)
        rstd = sb("rstd", [128, 1], F32)
        mtok = [sb(f"mtok{i}", [128, 1024], BF16) for i in range(2)]
        mixst = [sb(f"mixst{i}", [128, 8, 128], BF16) for i in range(2)]
        pst = [ts.enter_context(nc.psum_tensor(f"p3s_{s}_pst{i}", [128, 8, 128], BF16)) for i in range(0)]

        mk.barrier()
        b_kct = Buf("kct")
        mk.dma("sp", KCt, X["KC"][s].rearrange("(kv g d) t -> d kv g t", kv=2, g=4), reads=[P.xb["KC"][s]],
               writes=[b_kct])
        b_G = Buf("G")
        mk.dma("sp", Gt[:], X["GATE"][s].rearrange("tt p e -> p tt e"), reads=[P.xb["GATE"][s]], writes=[b_G])
        b_ht = bufs(4, "ht")
        for kv in range(2):
            for mc in range(2):
                pb = (2 * kv + mc) % 4
                for l in range(32):
                    mk.op("pe", lambda e, kv=kv, mc=mc, l=l, pb=pb: e.matmul(
                        ps[pb][:, 0:508].rearrange("p (g c) -> p g c", g=4),
                        lhsT=W1[:, kv, l, mc * 128:(mc + 1) * 128], rhs=KCt[:, kv, :, l:l + 2017:16],
                        start=(l == 0), stop=(l == 31)), reads=[b_w1, b_kct], writes=[psb[pb]])
                mk.op("act", lambda e, kv=kv, mc=mc, pb=pb: e.activation(
                    out=HT[:, kv, mc, :], in_=ps[pb][:, 0:508], func=AF.Gelu_apprx_tanh, bias=pebias[:, kv, mc:mc + 1]),
                    reads=[psb[pb], b_pb], writes=[b_ht[2 * kv + mc]])
        b_kcmp = Buf("kcmp")
        for mc in range(2):
            mk.op("pe", lambda e, mc=mc: e.matmul(ps[4][0:64, 0:508], lhsT=W2[:, 0, mc, :], rhs=HT[:, 0, mc, :],
                                                  start=(mc == 0), stop=(mc == 1)),
                  reads=[b_w1, b_ht[0], b_ht[1]], writes=[psb[4]])
        mk.op("act", act_copy(KCMP[:], ps[4][0:64, 0:508].rearrange("p (g c) -> p g c", g=4)), reads=[psb[4]],
              writes=[b_kcmp])
        b_vc = Buf("vcmp")
        merge_ev(b_vc, b_vca)
        for g in range(4):
            pb = 5 + g % 2
            for mc in range(2):
                mk.op("pe", lambda e, mc=mc, g=g, pb=pb: e.matmul(
                    ps[pb][0:NCMP, 0:64], lhsT=HT[:, 1, mc, g * NCMP:(g + 1) * NCMP], rhs=W2[:, 1, mc, :],
                    start=(mc == 0), stop=(mc == 1)), reads=[b_w1, b_ht[2], b_ht[3]], writes=[psb[pb]])
            bb = Buf()
            mk.op("dve", lambda e, g=g, pb=pb: e.tensor_copy(out=VCA[0:NCMP, g, 0:64], in_=ps[pb][0:NCMP, 0:64]),
                  reads=[psb[pb], b_vca], writes=[bb])
            merge_ev(b_vc, bb)
        b_vca.r = {}
        L["b_vca_last"] = b_vc

        mk.barrier()
        b_qa = Buf("qa")
        b_qsel = bufs(16, "qsel")
        b_ks = Buf("ks")
        b_kw = Buf("kw")
        b_v = Buf("v")
        b_ptc = bufs(2, "ptc")
        b_pts = [bufs(16, f"pts{i}_") for i in range(2)]
        b_ptw = [bufs(5, f"ptw{i}_") for i in range(2)]
        b_ocmp = bufs(16, "ocmp")
        b_oacc = bufs(2, "oacc")
        b_t = Buf("dvetmp")
        b_coefc = bufs(2, "coefc")
        b_nsp = bufs(2, "nsp")
        for i in range(2):
            merge_ev(b_nsp[i], b_c)
        scnt = [0]

        def sbank():
            b = scnt[0] % 3
            scnt[0] += 1
            return b

        for g in range(4):
            mk.dma("sp", QA[0:64, :, :], X["QT"][s, g * 256:(g + 1) * 256, :].rearrange("(r d) t -> d r t", r=4),
                   reads=[P.xb["QT"][s]], writes=[b_qa] + b_qsel)
            mk.dma("sp", KsA[0:64, :], X["KS"][s, g * 64:(g + 1) * 64, :], reads=[P.xb["KS"][s]], writes=[b_ks])
            mk.dma("sp", KwT[:], X["KW"][s, g * 64:(g + 1) * 64, :], reads=[P.xb["KW"][s]], writes=[b_kw])
            for a_ in range(2):
                mk.dma("sp", Vsw[:, :, a_, :], X["VSW"][s][:, :, a_, g, :].rearrange("tt p e -> p tt e"),
                       reads=[P.xb["VSW"][s]], writes=[b_v])

            def la1(qt):
                nk = min(NCMP, 8 * qt + 7)
                qs = slice(qt * 128, (qt + 1) * 128)
                sbk = sbank()
                k2 = qt % 2
                off = 136 - 8 * qt
                mk.op("pe", lambda e, g=g, nk=nk, qs=qs, sbk=sbk: e.matmul(
                    ps[sbk][0:nk, :].rearrange("p (r i) -> p r i", r=4), lhsT=KCMP[:, g, 0:nk], rhs=QA[0:64, :, qs],
                    start=True, stop=False), reads=[b_kcmp, b_qa], writes=[psb[sbk]])
                mk.op("pe", lambda e, g=g, nk=nk, off=off, sbk=sbk: e.matmul(
                    ps[sbk][0:nk, :].rearrange("p (r i) -> p r i", r=4), lhsT=zc[0:16, off:off + nk],
                    rhs=Mb[0:16, 4 * g:4 * g + 4, :], start=False, stop=True), reads=[b_c, b_DN], writes=[psb[sbk]])
                mk.op("act", lambda e, nk=nk, sbk=sbk, k2=k2: e.activation(out=PTc[k2][0:nk, :], in_=ps[sbk][0:nk, :],
                                                                          func=AF.Exp),
                      reads=[psb[sbk]], writes=[b_ptc[k2]])
                ob = 3
                for r in range(4):
                    mk.op("pe", lambda e, r=r, nk=nk, g=g, k2=k2, ob=ob: e.matmul(
                        ps[ob][:, r * 128:r * 128 + 97], lhsT=PTc[k2][0:nk, r * 128:(r + 1) * 128], rhs=VCA[0:nk, g, :],
                        start=True, stop=True), reads=[b_ptc[k2], b_vc], writes=[psb[ob]])

            def la2a(qt):
                k2 = qt % 2
                qs = slice(qt * 128, (qt + 1) * 128)
                ob = 3
                O = ps[ob][:, :].rearrange("p (r e) -> p r e", r=4)
                mk.op("dve", lambda e, O=O: e.tensor_scalar(out=rs[:], in0=O[:, :, 64], scalar1=1e-30, scalar2=None,
                                                            op0=ALU.max), reads=[psb[ob]], writes=[b_t])
                mk.op("dve", lambda e: e.reciprocal(out=rinv[:], in_=rs[:]), reads=[b_t], writes=[b_t])
                mk.op("dve", lambda e, O=O: e.tensor_scalar(out=imp[:], in0=O[:, 0, 65:97], scalar1=rinv[:, 0:1],
                                                            scalar2=None, op0=ALU.mult),
                      reads=[psb[ob], b_t], writes=[b_t])
                for r in range(1, 4):
                    mk.op("dve", lambda e, O=O, r=r: e.scalar_tensor_tensor(
                        out=imp[:], in0=O[:, r, 65:97], scalar=rinv[:, r:r + 1], in1=imp[:], op0=ALU.mult,
                        op1=ALU.add), reads=[psb[ob], b_t], writes=[b_t])
                mk.op("dve", lambda e, qt=qt, g=g: e.tensor_tensor(out=coef[:], in0=rinv[:],
                                                                  in1=Gt[:, qt, g * 12:g * 12 + 12:3], op=ALU.mult),
                      reads=[b_t, b_G], writes=[b_t])
                for r in range(4):
                    mk.op("dve", lambda e, O=O, r=r, qt=qt: e.tensor_scalar(
                        out=ocmp[:, qt, r, :], in0=O[:, r, 0:64], scalar1=coef[:, r:r + 1], scalar2=None,
                        op0=ALU.mult), reads=[psb[ob], b_t], writes=[b_ocmp[qt]])
                mk.op("dve", lambda e, qt=qt: e.tensor_tensor(out=score[:], in0=imp[:], in1=VAL[:, qt, :], op=ALU.mult),
                      reads=[b_t, b_c], writes=[b_t])
                mk.op("dve", lambda e, qt=qt: e.tensor_tensor(out=score[:], in0=score[:], in1=ADD[:, qt, :], op=ALU.add),
                      reads=[b_t, b_c], writes=[b_t])
                mk.op("dve", lambda e: e.max(out=m8a[:], in_=score[:]), reads=[b_t], writes=[b_t])
                mk.op("dve", lambda e: e.match_replace(out=sc2[:], in_to_replace=m8a[:], in_values=score[:],
                                                       imm_value=-1e30), reads=[b_t], writes=[b_t])
                mk.op("dve", lambda e: e.max(out=m8b[:], in_=sc2[:]), reads=[b_t], writes=[b_t])
                mk.op("dve", lambda e: e.tensor_scalar(out=thr[:], in0=m8b[:, 7:8], scalar1=0.0, scalar2=None,
                                                       op0=ALU.max), reads=[b_t], writes=[b_t])
                mk.op("dve", lambda e: e.tensor_scalar(out=selt[:], in0=score[:], scalar1=thr[:, 0:1], scalar2=None,
                                                       op0=ALU.is_ge), reads=[b_t], writes=[b_t])
                mk.op("dve", lambda e, k2=k2: e.tensor_scalar(out=NSP[k2][:, 64:96], in0=selt[:], scalar1=-1.0,
                                                              scalar2=-NEG, op0=ALU.add, op1=ALU.mult),
                      reads=[b_t], writes=[b_nsp[k2]])

            def la2b(qt):
                k2 = qt % 2
                qs = slice(qt * 128, (qt + 1) * 128)
                tb = sbank()
                mk.op("pe", lambda e, k2=k2, tb=tb: e.matmul(ps[tb][0:96, 0:128], lhsT=NSP[k2][:, 0:96], rhs=ident[:],
                                                             start=True, stop=True),
                      reads=[b_nsp[k2], P.cb], writes=[psb[tb]])
                mk.op("act", lambda e, tb=tb, qs=qs: e.activation(
                    out=QA[64:96, :, qs], in_=ps[tb][64:96, 0:128].unsqueeze(1).to_broadcast([32, 4, 128]),
                    func=AF.Copy), reads=[psb[tb]], writes=[b_qsel[qt]])


            def qk(qt):
                k2 = qt % 2
                qs = slice(qt * 128, (qt + 1) * 128)
                for kt in range(0, qt + 1):
                    yield
                    sbk = sbank()
                    ks_ = slice(kt * 128, (kt + 1) * 128)
                    near = kt >= qt - 1
                    mk.op("pe", lambda e, sbk=sbk, ks_=ks_, qs=qs, near=near: e.matmul(
                        ps[sbk][:, :].rearrange("p (r i) -> p r i", r=4), lhsT=KsA[0:96, ks_], rhs=QA[0:96, :, qs],
                        start=True, stop=(not near)), reads=[b_ks, b_c, b_qa, b_qsel[qt]], writes=[psb[sbk]])
                    if near:
                        mk.op("pe", lambda e, sbk=sbk, g=g, dl=qt - kt: e.matmul(
                            ps[sbk][:, :].rearrange("p (r i) -> p r i", r=4), lhsT=ident[:],
                            rhs=DNb[:, 4 * g:4 * g + 4, dl, :], start=False, stop=True),
                            reads=[P.cb, b_DN], writes=[psb[sbk]])
                    mk.op("act", lambda e, sbk=sbk, k2=k2, kt=kt: e.activation(out=PTs[k2][:, kt, :], in_=ps[sbk][:, :],
                                                                              func=AF.Exp),
                          reads=[psb[sbk]], writes=[b_pts[k2][kt]])
                for wi, kt in enumerate(range(max(0, qt - 4), qt + 1)):
                    yield
                    sbk = sbank()
                    ks_ = slice(kt * 128, (kt + 1) * 128)
                    dl = qt - kt
                    sp_ = dl in (0, 1, 4)
                    mk.op("pe", lambda e, sbk=sbk, ks_=ks_, qs=qs, sp_=sp_: e.matmul(
                        ps[sbk][:, :].rearrange("p (r i) -> p r i", r=4), lhsT=KwT[0:64, ks_], rhs=QA[0:64, :, qs],
                        start=True, stop=(not sp_)), reads=[b_kw, b_qa], writes=[psb[sbk]])
                    if sp_:
                        rhs = DN4b[:] if dl == 4 else DNb[:, 4 * g:4 * g + 4, dl, :]
                        mk.op("pe", lambda e, sbk=sbk, rhs=rhs: e.matmul(
                            ps[sbk][:, :].rearrange("p (r i) -> p r i", r=4), lhsT=ident[:], rhs=rhs, start=False,
                            stop=True), reads=[P.cb, b_DN], writes=[psb[sbk]])
                    mk.op("act", lambda e, sbk=sbk, k2=k2, wi=wi: e.activation(out=PTw[k2][:, wi, :], in_=ps[sbk][:, :],
                                                                              func=AF.Exp),
                          reads=[psb[sbk]], writes=[b_ptw[k2][wi]])

            def pv(qt):
                k2 = qt % 2
                obs = 4 + 2 * k2
                obw = 5 + 2 * k2
                for r in range(4):
                    for kt in range(0, qt + 1):
                        if kt % 4 == 0:
                            yield
                        mk.op("pe", lambda e, r=r, kt=kt, k2=k2, obs=obs, qt=qt: e.matmul(
                            ps[obs][:, r * 128:r * 128 + 65], lhsT=PTs[k2][:, kt, r * 128:(r + 1) * 128],
                            rhs=Vsw[:, kt, 0, :], start=(kt == 0), stop=(kt == qt)),
                            reads=[b_pts[k2][kt], b_v], writes=[psb[obs]])
                kts = list(range(max(0, qt - 4), qt + 1))
                for r in range(4):
                    yield
                    for wi, kt in enumerate(kts):
                        mk.op("pe", lambda e, r=r, kt=kt, wi=wi, k2=k2, obw=obw: e.matmul(
                            ps[obw][:, r * 128:r * 128 + 65], lhsT=PTw[k2][:, wi, r * 128:(r + 1) * 128],
                            rhs=Vsw[:, kt, 1, :], start=(wi == 0), stop=(wi == len(kts) - 1)),
                            reads=[b_ptw[k2][wi], b_v], writes=[psb[obw]])
                for bi, ob in ((1, obs), (2, obw)):
                    O = ps[ob][:, :].rearrange("p (r e) -> p r e", r=4)
                    mk.op("dve", lambda e, O=O: e.reciprocal(out=rinv[:], in_=O[:, :, 64]), reads=[psb[ob]], writes=[b_t])
                    mk.op("dve", lambda e, qt=qt, bi=bi, g=g: e.tensor_tensor(
                        out=coef[:], in0=rinv[:], in1=Gt[:, qt, g * 12 + bi:g * 12 + 12:3], op=ALU.mult),
                        reads=[b_t, b_G], writes=[b_t])
                    for r in range(4):
                        src1 = ocmp[:, qt, r, :] if bi == 1 else oacc[k2][:, r, :]
                        mk.op("dve", lambda e, O=O, r=r, src1=src1, k2=k2: e.scalar_tensor_tensor(
                            out=oacc[k2][:, r, :], in0=O[:, r, 0:64], scalar=coef[:, r:r + 1], in1=src1, op0=ALU.mult,
                            op1=ALU.add), reads=[psb[ob], b_t, b_ocmp[qt], b_oacc[k2]], writes=[b_oacc[k2]])
                bb = Buf()
                mk.dma("pool", X["OATT"][s, qt, :, g * 256:(g + 1) * 256], oacc[k2][:].rearrange("p r e -> p (r e)"),
                       reads=[b_oacc[k2]], writes=[bb])
                merge_ev(P.xb["OATT"][s], bb)

            for v in range(-3, 16):
                if 0 <= v + 3 < 16:
                    la1(v + 3)
                    la2a(v + 3)
                if 0 <= v + 2 < 16:
                    la2b(v + 2)
                gq = qk(v + 1) if 0 <= v + 1 < 16 else iter(())
                gp = pv(v) if 0 <= v < 16 else iter(())
                live = [gq, gp]
                while live:
                    for g_ in list(live):
                        try:
                            next(g_)
                        except StopIteration:
                            live.remove(g_)

        b_oat = bufs(2, "oat")
        b_n = Buf("normtmp")
        b_mtok = bufs(2, "mtok")
        b_mixst = bufs(2, "mixst")
        for qt in range(16):
            k2 = qt % 2
            mk.dma("sp", oat[k2][:], X["OATT"][s, qt], reads=[P.xb["OATT"][s]], writes=[b_oat[k2]])
            mk.op("act", lambda e, k2=k2: e.activation(out=junk[:], in_=oat[k2][:], func=AF.Square, accum_out=ssq[:]),
                  reads=[b_oat[k2]], writes=[b_n])
            mk.op("act", lambda e: e.activation(out=rstd[:], in_=ssq[:], func=AF.Sqrt, scale=1.0 / 1024.0,
                                                bias=cst1[:, 1:2]), reads=[b_n, b_c], writes=[b_n])
            mk.op("dve", lambda e: e.reciprocal(out=rstd[:], in_=rstd[:]), reads=[b_n], writes=[b_n])
            mk.op("dve", lambda e, k2=k2: e.scalar_tensor_tensor(out=mtok[k2][:], in0=oat[k2][:], scalar=rstd[:, 0:1],
                                                                 in1=gbc[:], op0=ALU.mult, op1=ALU.mult),
                  reads=[b_oat[k2], b_n, b_c], writes=[b_mtok[k2]])
            tb = 2 * k2
            for c in range(8):
                mk.op("pe", lambda e, c=c, k2=k2, tb=tb: e.matmul(
                    ps[tb + c // 4][:, (c % 4) * 128:(c % 4 + 1) * 128], lhsT=mtok[k2][:, c * 128:(c + 1) * 128],
                    rhs=ident[:], start=True, stop=True), reads=[b_mtok[k2], P.cb], writes=[psb[tb + c // 4]])
            for hh_ in range(2):
                evac(mk, hh_, mixst[k2][:, 4 * hh_:4 * hh_ + 4, :],
                     ps[tb + hh_][:, :].rearrange("p (c t) -> p c t", c=4), [psb[tb + hh_]], [b_mixst[k2]])
            bb = Buf()
            mk.dma("pool", X["MIXA"][s, :, qt * 128:(qt + 1) * 128].rearrange("(c p) t -> p c t", p=128), mixst[k2][:],
                   reads=[b_mixst[k2]], writes=[bb])
            merge_ev(P.xb["MIXA"][s], bb)
        mk.flush()


def phase4(P, nseq):
    nc, mk, I, W, X, C = P.nc, P.mk, P.I, P.W, P.X, P.C
    mk.barrier()
    TG = 512
    with ExitStack() as ts:
        def sb(name, shape, dt):
            return ts.enter_context(nc.sbuf_tensor(f"p4_{name}", shape, dt))

        hx = sb("hx", [128, 16, TG], F32)
        mixT = sb("mixT", [128, 16, TG], BF16)
        rstr = sb("rstr", [128, TG], F32)
        y2T = sb("y2T", [128, 16, TG], BF16)
        actT = sb("actT", [128, NFC, TG], BF16)
        NW16, NWD = 5, 3
        W16 = [sb(f"W16_{i}", [128, 16, 256], BF16) for i in range(NW16)]
        WD = [sb(f"WD_{i}", [128, NFC, 128], BF16) for i in range(NWD)]
        gpre = [sb(f"gpre{i}", [128, TG + 2], F32) for i in range(2)]
        cv = [sb(f"cv{i}", [128, TG], F32) for i in range(2)]
        ge = [sb(f"ge{i}", [128, TG], F32) for i in range(2)]
        GC = sb("GC", [128, NFC, 2], F32)
        rt = sb("rt", [128, TG], F32)
        rstd = sb("rstd", [128, TG], F32)
        fv = sb("fv", [128, NFC, 4], F32)
        gffn = sb("gffn", [128, 16], F32)
        gfin = sb("gfin", [128, 16], F32)
        epst = sb("eps", [128, 1], F32)
        ps = [ts.enter_context(nc.psum_tensor(f"p4_ps{i}", [128, 512], F32)) for i in range(8)]
        psb = bufs(8, "ps")
        ones = C["ones"]

        b_c = Buf("p4c")
        for (t_, src) in ((fv, I["ffn_vec"]), (gffn, I["g_ffn"]), (gfin, I["g_fin"])):
            b = Buf()
            mk.dma("pool", t_[:], src, writes=[b])
            merge_ev(b_c, b)
        b = Buf()
        mk.op("dve", lambda e: e.memset(epst[:], EPS), writes=[b])
        merge_ev(b_c, b)
        P_EPS[0] = epst[:]
        P_EPS[1] = b_c

        b_hx = bufs(16, "hx")
        b_mix = bufs(16, "mix")
        b_rstr = Buf("rstr")
        b_y2 = bufs(16, "y2")
        b_act = bufs(NFC, "act")
        b_w16 = bufs(NW16, "w16")
        b_wd = bufs(NWD, "wd")
        b_gpre = bufs(2, "gpre")
        b_cv = bufs(2, "cv")
        b_ge = bufs(2, "ge")
        b_gc = bufs(NFC, "gc")
        b_rt = Buf("rt")
        b_rstd = Buf("rstd")

        groups = [(s, j) for s in range(nseq) for j in range(S // TG)]
        items16 = []
        for gi_ in range(len(groups)):
            for dcp in range(8):
                items16.append((W["w_out"][dcp], P.wb["w_out"]))
            for fp in range(NFC // 2):
                items16.append((W["w_g"][fp], P.wb["w_g"]))
                items16.append((W["w_u"][fp], P.wb["w_u"]))
        st16 = [0]

        def need16(n):
            while st16[0] <= min(n + NW16 - 2, len(items16) - 1):
                i = st16[0]
                src, wb_ = items16[i]
                mk.dma("sp", W16[i % NW16][:].rearrange("p c n -> p (c n)"), src, reads=[wb_], writes=[b_w16[i % NW16]])
                st16[0] += 1

        itemsd = []
        for gi_ in range(len(groups)):
            for dc in range(16):
                itemsd.append(W["w_d"][dc])
        std = [0]

        def needd(n):
            while std[0] <= min(n + NWD - 1, len(itemsd) - 1):
                i = std[0]
                mk.dma("pool", WD[i % NWD][:].rearrange("p f n -> p (f n)"), itemsd[i], reads=[P.wb["w_d"]],
                       writes=[b_wd[i % NWD]])
                std[0] += 1

        pcnt = [0]

        def bank():
            b = pcnt[0] % 8
            pcnt[0] += 1
            return b

        def norm_stats(n_feat):
            pb = bank()
            for c in range(16):
                mk.op("act", lambda e, c=c: e.activation(out=y2T[:, c, :], in_=hx[:, c, :], func=AF.Square),
                      reads=[b_hx[c]], writes=[b_y2[c]])
                mk.op("pe", lambda e, c=c, pb=pb: e.matmul(ps[pb][:, :], lhsT=ones[:], rhs=y2T[:, c, :], start=(c == 0),
                                                           stop=(c == 15)), reads=[b_y2[c], P.cb], writes=[psb[pb]])
            rms_rstd(mk, ps[pb][:, :], psb[pb], rt[:], b_rt, rstd[:], b_rstd, float(n_feat))

        def load_mix(gi2):
            s2, j2 = groups[gi2]
            tsl2 = slice(j2 * TG, (j2 + 1) * TG)
            mk.dma("pool", mixT[:, 0:8, :], X["MIXA"][s2].rearrange("(c p) t -> p c t", p=128)[:, :, tsl2],
                   reads=[P.xb["MIXA"][s2]], writes=b_mix[0:8])
            mk.dma("pool", mixT[:, 8:16, :], X["MIXR"][s2].rearrange("(c p) t -> p c t", p=128)[:, :, tsl2],
                   reads=[P.xb["MIXR"][s2]], writes=b_mix[8:16])
            mk.dma("pool", rstr[:], X["RSTDR"][s2][:, tsl2], reads=[P.xb["RSTDR"][s2]], writes=[b_rstr])
            for c in range(8, 16):
                mk.op("dve", lambda e, c=c: e.tensor_tensor(out=mixT[:, c, :], in0=mixT[:, c, :], in1=rstr[:],
                                                            op=ALU.mult), reads=[b_mix[c], b_rstr], writes=[b_mix[c]])

        i16 = 0
        idn = 0
        import os
        P4S = int(os.environ.get("P4_STOP", "9"))
        CPE = os.environ.get("P4_CPE", "dve")
        if P4S < 9:
            groups = groups[:1]
        for gi_, (s, j) in enumerate(groups):
            t0 = j * TG
            tsl = slice(t0, t0 + TG)
            need16(i16)
            xsrc = I["xT"][s].rearrange("(c p) t -> p c t", p=128)
            for hf in range(2):
                cs = slice(hf * 8, hf * 8 + 8)
                mk.dma("pool", hx[:, cs, :], xsrc[:, cs, tsl], writes=b_hx[hf * 8:hf * 8 + 8])
            if gi_ == 0:
                load_mix(0)
            if j == 0:
                for fc in range(NFC):
                    mk.op("dve", lambda e, fc=fc: e.memset(GC[:, fc, :], 0.0), writes=[b_gc[fc]])
            if P4S <= 1:
                break
            for dcp in range(8):
                need16(i16)
                slot = i16 % NW16
                for dd in range(2):
                    dc = 2 * dcp + dd
                    pb = bank()
                    for c in range(16):
                        mk.op("pe", lambda e, c=c, pb=pb, slot=slot, dd=dd: e.matmul(
                            ps[pb][:, :], lhsT=W16[slot][:, c, dd * 128:(dd + 1) * 128], rhs=mixT[:, c, :],
                            start=(c == 0), stop=(c == 15)), reads=[b_w16[slot], b_mix[c]], writes=[psb[pb]])
                    mk.op("dve", lambda e, dc=dc, pb=pb: e.tensor_tensor(out=hx[:, dc, :], in0=ps[pb][:, :],
                                                                        in1=hx[:, dc, :], op=ALU.add),
                          reads=[psb[pb], b_hx[dc]], writes=[b_hx[dc]])
                i16 += 1
            if P4S <= 2:
                break
            norm_stats(D)
            for c in range(16):
                mk.op("dve", lambda e, c=c: e.scalar_tensor_tensor(out=y2T[:, c, :], in0=hx[:, c, :],
                                                                   scalar=gffn[:, c:c + 1], in1=rstd[:],
                                                                   op0=ALU.mult, op1=ALU.mult),
                      reads=[b_hx[c], b_rstd, b_c], writes=[b_y2[c]])
            if P4S <= 3:
                break
            for fp in range(NFC // 2):
                need16(i16)
                sg = i16 % NW16
                su = (i16 + 1) % NW16
                for ff in range(2):
                    fc = 2 * fp + ff
                    k = fc % 2
                    pg = bank()
                    pu = bank()
                    for c in range(16):
                        mk.op("pe", lambda e, c=c, pg=pg, sg=sg, ff=ff: e.matmul(
                            ps[pg][:, :], lhsT=W16[sg][:, c, ff * 128:(ff + 1) * 128], rhs=y2T[:, c, :],
                            start=(c == 0), stop=(c == 15)), reads=[b_w16[sg], b_y2[c]], writes=[psb[pg]])
                    for c in range(16):
                        mk.op("pe", lambda e, c=c, pu=pu, su=su, ff=ff: e.matmul(
                            ps[pu][:, :], lhsT=W16[su][:, c, ff * 128:(ff + 1) * 128], rhs=y2T[:, c, :],
                            start=(c == 0), stop=(c == 15)), reads=[b_w16[su], b_y2[c]], writes=[psb[pu]])
                    mk.op(CPE, lambda e, k=k, fc=fc: e.tensor_copy(out=gpre[k][:, 0:2], in_=GC[:, fc, :]),
                          reads=[b_gc[fc]], writes=[b_gpre[k]])
                    mk.op("act", act_copy(gpre[k][:, 2:TG + 2], ps[pg][:, :]), reads=[psb[pg]], writes=[b_gpre[k]])
                    mk.op("act", lambda e, k=k, fc=fc, pg=pg: e.activation(
                        out=cv[k][:], in_=ps[pg][:, :], func=AF.Identity, scale=fv[:, fc, 2:3], bias=fv[:, fc, 3:4]),
                        reads=[psb[pg], b_c], writes=[b_cv[k]])
                    mk.op(CPE, lambda e, k=k, fc=fc: e.tensor_copy(out=GC[:, fc, :], in_=gpre[k][:, TG:TG + 2]),
                          reads=[b_gpre[k]], writes=[b_gc[fc]])
                    for kk in (1, 0):
                        mk.op("dve", lambda e, k=k, fc=fc, kk=kk: e.scalar_tensor_tensor(
                            out=cv[k][:], in0=gpre[k][:, kk:kk + TG], scalar=fv[:, fc, kk:kk + 1], in1=cv[k][:],
                            op0=ALU.mult, op1=ALU.add), reads=[b_gpre[k], b_cv[k], b_c], writes=[b_cv[k]])
                    mk.op("act", lambda e, k=k: e.activation(out=ge[k][:], in_=cv[k][:], func=AF.Gelu_apprx_tanh),
                          reads=[b_cv[k]], writes=[b_ge[k]])
                    mk.op("dve", lambda e, k=k, fc=fc, pu=pu: e.tensor_tensor(out=actT[:, fc, :], in0=ps[pu][:, :],
                                                                              in1=ge[k][:], op=ALU.mult),
                          reads=[psb[pu], b_ge[k]], writes=[b_act[fc]])
                i16 += 2
            if P4S <= 4:
                break
            if gi_ + 1 < len(groups):
                load_mix(gi_ + 1)
            for dc in range(16):
                needd(idn)
                slot = idn % NWD
                pb = bank()
                for fc in range(NFC):
                    mk.op("pe", lambda e, fc=fc, pb=pb, slot=slot: e.matmul(
                        ps[pb][:, :], lhsT=WD[slot][:, fc, :], rhs=actT[:, fc, :], start=(fc == 0),
                        stop=(fc == NFC - 1)), reads=[b_wd[slot], b_act[fc]], writes=[psb[pb]])
                mk.op("dve", lambda e, dc=dc, pb=pb: e.tensor_tensor(out=hx[:, dc, :], in0=ps[pb][:, :],
                                                                    in1=hx[:, dc, :], op=ALU.add),
                      reads=[psb[pb], b_hx[dc]], writes=[b_hx[dc]])
                idn += 1
            if P4S <= 5:
                break
            norm_stats(D)
            for c in range(16):
                mk.op("dve", lambda e, c=c: e.scalar_tensor_tensor(out=hx[:, c, :], in0=hx[:, c, :],
                                                                   scalar=gfin[:, c:c + 1], in1=rstd[:],
                                                                   op0=ALU.mult, op1=ALU.mult),
                      reads=[b_hx[c], b_rstd, b_c], writes=[b_hx[c]])
            osrc = P.outT[s].rearrange("(c p) t -> p c t", p=128)
            for hf in range(2):
                cs = slice(hf * 8, hf * 8 + 8)
                mk.dma("pool", osrc[:, cs, tsl], hx[:, cs, :], reads=b_hx[hf * 8:hf * 8 + 8], writes=[Buf()])
        mk.flush()
```

```python
import math
from contextlib import ExitStack

import numpy as np
import ml_dtypes

import concourse.bass as bass
import concourse.mybir as mybir
from concourse.bass_utils import run_bass_kernel_spmd

F32 = mybir.dt.float32
BF16 = mybir.dt.bfloat16
AF = mybir.ActivationFunctionType
ALU = mybir.AluOpType

N_CORES = 8
D = 2048
S = 2048
NSEQ = 2
NH = 16
NG = 4
HD = 64
INW = 4656
DFF = 5632
NFC = DFF // 128
NCMP = 127
EPS = 1e-6
NEG = -30000.0


class Buf:
    __slots__ = ("name", "w", "r")

    def __init__(self, name=""):
        self.name = name
        self.w = {}
        self.r = {}


def bufs(n, name=""):
    return [Buf(f"{name}{i}") for i in range(n)]


class MK:
    ENG = ("pe", "act", "dve", "pool", "sp")

    def __init__(self, nc, es):
        self.nc = nc
        self.q = {e: [] for e in self.ENG}
        self.semh = {}
        self.prog = {}
        for e in ("pe", "act", "dve", "pool"):
            h = es.enter_context(nc.semaphore("prog_" + e))
            self.prog[e] = [h, 0]
            self.semh[("p", e)] = h
        self.seen = {e: {} for e in self.ENG}
        self.dsem = {}
        for qn, n in (("sp", 10), ("pool", 6), ("act", 2), ("cast", 8)):
            lst = []
            for i in range(n):
                h = es.enter_context(nc.semaphore(f"d_{qn}{i}"))
                lst.append([h, 0])
                self.semh[("d", qn, i)] = h
            self.dsem[qn] = lst
        self.drr = {qn: 0 for qn in self.dsem}
        self.ninstr = {e: 0 for e in self.ENG}

    def _wait(self, eng, key, v):
        if eng == "pe" and key == ("p", "pe"):
            return
        seen = self.seen[eng]
        if seen.get(key, 0) < v:
            seen[key] = v
            h = self.semh[key]
            self.q[eng].append(lambda e, h=h, v=v: e.wait_ge(h, v))
            self.ninstr[eng] += 1

    def _waits(self, eng, reads, writes):
        need = {}
        for b in reads:
            for k, v in b.w.items():
                if need.get(k, 0) < v:
                    need[k] = v
        for b in writes:
            for k, v in b.w.items():
                if need.get(k, 0) < v:
                    need[k] = v
            for k, v in b.r.items():
                if need.get(k, 0) < v:
                    need[k] = v
        for k, v in need.items():
            self._wait(eng, k, v)

    def op(self, eng, fn, reads=(), writes=()):
        self._waits(eng, reads, writes)
        p = self.prog[eng]
        p[1] += 1
        v = p[1]
        key = ("p", eng)
        h = p[0]
        self.q[eng].append(lambda e, fn=fn, h=h: fn(e).then_inc(h, 1))
        self.ninstr[eng] += 1
        for b in reads:
            if b.r.get(key, 0) < v:
                b.r[key] = v
        for b in writes:
            b.w = {key: v}
            b.r = {}

    def dma(self, qn, out, in_, reads=(), writes=(), sems=None):
        self._waits(qn, reads, writes)
        sn = sems or qn
        lst = self.dsem[sn]
        i = self.drr[sn]
        self.drr[sn] = (i + 1) % len(lst)
        s = lst[i]
        key = ("d", sn, i)
        if s[1] > 0:
            self._wait(qn, key, s[1])
        s[1] += 16
        v = s[1]
        h = s[0]
        self.q[qn].append(lambda e, h=h, out=out, in_=in_: e.dma_start(out=out, in_=in_).then_inc(h, 16))
        self.ninstr[qn] += 1
        for b in reads:
            if b.r.get(key, 0) < v:
                b.r[key] = v
        for b in writes:
            b.w = {key: v}
            b.r = {}

    def barrier(self):
        for eng in self.ENG:
            for e2, p in self.prog.items():
                if p[1] > 0:
                    if eng == e2 and eng == "pe":
                        continue
                    seen = self.seen[eng]
                    key = ("p", e2)
                    if seen.get(key, 0) < p[1]:
                        seen[key] = p[1]
                        self.q[eng].append(lambda e, h=p[0], v=p[1]: e.wait_ge(h, v))
            for qn, lst in self.dsem.items():
                if qn == "cast":
                    continue
                for i, s in enumerate(lst):
                    if s[1] > 0:
                        key = ("d", qn, i)
                        seen = self.seen[eng]
                        if seen.get(key, 0) < s[1]:
                            seen[key] = s[1]
                            self.q[eng].append(lambda e, h=s[0], v=s[1]: e.wait_ge(h, v))

    def flush(self, final=False):
        nc = self.nc
        if final:
            for qn, lst in self.dsem.items():
                for i, s in enumerate(lst):
                    if s[1] > 0:
                        self._wait("pool" if qn == "cast" else qn, ("d", qn, i), s[1])
        q = self.q
        with nc.Block() as block:
            @block.tensor
            def _(e):
                for f in q["pe"]:
                    f(e)

            @block.scalar
            def _(e):
                for f in q["act"]:
                    f(e)

            @block.vector
            def _(e):
                for f in q["dve"]:
                    f(e)

            @block.gpsimd
            def _(e):
                for f in q["pool"]:
                    f(e)

            @block.sync
            def _(e):
                for f in q["sp"]:
                    f(e)
        self.q = {e: [] for e in self.ENG}


def t5_bucket_np(dist):
    n = np.maximum(dist, 0)
    max_exact = 16
    nf = np.maximum(n, 1).astype(np.float32)
    large = max_exact + (np.log(nf / np.float32(max_exact)) / np.float32(math.log(128 / max_exact))
                         * np.float32(32 - max_exact)).astype(np.int32)
    large = np.minimum(large, 31)
    return np.where(n < max_exact, n, large)


GW = 384


def host_constants():
    c = {}
    c["ident_bf"] = np.eye(128, dtype=np.float32).astype(ml_dtypes.bfloat16)
    c["ones_bf"] = np.ones((128, 128), dtype=np.float32).astype(ml_dtypes.bfloat16)
    n = np.arange(GW)
    bk = t5_bucket_np(n - 127)
    oh = np.zeros((32, GW), np.float32)
    oh[bk, n] = 1.0
    oh[31, :] -= 1.0
    c["oh"] = oh
    j = np.arange(128)[:, None]
    i = np.arange(128)[None, :]
    c["mask_d0"] = (i >= j).astype(np.float32)
    c["mask_d4"] = (j > i).astype(np.float32)
    e = np.zeros((32, S), np.float32)
    e[np.arange(S) // 64, np.arange(S)] = 1.0
    c["e_rows"] = e.astype(ml_dtypes.bfloat16)
    cc = np.arange(NCMP)[:, None]
    jj = np.arange(32)[None, :]
    lo = np.maximum(cc * 16, jj * 64)
    hi = np.minimum(cc * 16 + 32, (jj + 1) * 64)
    c["overlap"] = (np.maximum(hi - lo, 0) / 32.0).astype(np.float32)
    t = np.arange(S)[:, None]
    jb = np.arange(32)[None, :]
    cur = t // 64
    valid = jb <= cur
    forced = ((jb == 0) | (jb == cur) | (jb == cur - 1))
    val = valid.astype(np.float32)
    add = np.where(valid, 1000.0 * forced, -1.0).astype(np.float32)
    c["sel_val"] = np.ascontiguousarray(val.reshape(16, 128, 32).transpose(1, 0, 2))
    c["sel_add"] = np.ascontiguousarray(add.reshape(16, 128, 32).transpose(1, 0, 2))
    zc = np.zeros((16, 272), np.float32)
    zc[np.arange(16), np.arange(16) + 128] = 1.0
    c["zc"] = zc.astype(ml_dtypes.bfloat16)
    k = np.arange(16)[:, None]
    dist = i - 16 * (k - 8) - 31
    c["cmp_valid"] = (dist >= 0).astype(np.float32)
    oh2 = np.zeros((32, 16 * 128), np.float32)
    bk2 = t5_bucket_np(dist.reshape(-1))
    oh2[bk2, np.arange(16 * 128)] = 1.0
    oh2[31, :] -= 1.0
    c["oh_cmp"] = oh2
    return c


def dram_in(nc, name, shape, dt=F32):
    return nc.dram_tensor(name, list(shape), dt, kind="ExternalInput").ap()


class Prog:
    pass


def build(phases=("p0", "p1", "p2", "p3", "p4"), debug=False, nseq=NSEQ):
    nc = bass.Bass("TRN2", target_bir_lowering=False)
    P = Prog()
    P.nc = nc
    P.nseq = nseq
    kind_scr = "ExternalOutput" if debug else "Internal"

    def scr(name, shape, dt):
        return nc.dram_tensor(name, list(shape), dt, kind=kind_scr).ap()

    I = {}
    I["xT"] = dram_in(nc, "xT", [NSEQ, D, S])
    I["w_in"] = dram_in(nc, "w_in", [D, INW])
    I["w_out"] = dram_in(nc, "w_out", [D, D])
    I["w_g"] = dram_in(nc, "w_g", [D, DFF])
    I["w_u"] = dram_in(nc, "w_u", [D, DFF])
    I["w_d"] = dram_in(nc, "w_d", [DFF, D])
    I["g_mix"] = dram_in(nc, "g_mix", [128, 16])
    I["g_ffn"] = dram_in(nc, "g_ffn", [128, 16])
    I["g_fin"] = dram_in(nc, "g_fin", [128, 16])
    I["b_gate_bc"] = dram_in(nc, "b_gate_bc", [128, 48])
    I["g_attn_bc"] = dram_in(nc, "g_attn_bc", [128, 1024])
    I["rnn_vec"] = dram_in(nc, "rnn_vec", [128, 8, 10])
    I["rg_a_w"] = dram_in(nc, "rg_a_w", [16, 64, 64])
    I["rg_x_w"] = dram_in(nc, "rg_x_w", [16, 64, 64])
    I["ffn_vec"] = dram_in(nc, "ffn_vec", [128, NFC, 4])
    I["rel_bias"] = dram_in(nc, "rel_bias", [32, 16])
    I["cmp_w1"] = dram_in(nc, "cmp_w1", [2, 64, 32, 256])
    I["cmp_w2"] = dram_in(nc, "cmp_w2", [2, 128, 2, 64])
    I["cmp_peT"] = dram_in(nc, "cmp_peT", [2, 64, 32])
    I["ident_bf"] = dram_in(nc, "ident_bf", [128, 128], BF16)
    I["ones_bf"] = dram_in(nc, "ones_bf", [128, 128], BF16)
    I["oh"] = dram_in(nc, "oh", [32, GW])
    I["mask_d0"] = dram_in(nc, "mask_d0", [128, 128])
    I["mask_d4"] = dram_in(nc, "mask_d4", [128, 128])
    I["e_rows"] = dram_in(nc, "e_rows", [32, S], BF16)
    I["overlap"] = dram_in(nc, "overlap", [NCMP, 32])
    I["sel_val"] = dram_in(nc, "sel_val", [128, 16, 32])
    I["sel_add"] = dram_in(nc, "sel_add", [128, 16, 32])
    I["zc"] = dram_in(nc, "zc", [16, 272], BF16)
    I["cmp_valid"] = dram_in(nc, "cmp_valid", [16, 128])
    P.I = I

    outT = nc.dram_tensor("outT", [NSEQ, D, S], F32, kind="ExternalOutput").ap()
    P.outT = outT

    W = {}
    W["w_in"] = scr("w_in_b", [D, INW], BF16)
    W["w_out"] = scr("w_out_b", [8, 128, 16 * 256], BF16)
    W["w_g"] = scr("w_g_b", [NFC // 2, 128, 16 * 256], BF16)
    W["w_u"] = scr("w_u_b", [NFC // 2, 128, 16 * 256], BF16)
    W["w_d"] = scr("w_d_b", [16, 128, NFC * 128], BF16)
    P.W = W
    X = {}
    X["QT"] = scr("QT", [NSEQ, 1024, S], BF16)
    X["KC"] = scr("KC", [NSEQ, 512, S], BF16)
    X["KS"] = scr("KS", [NSEQ, 256, S], BF16)
    X["KW"] = scr("KW", [NSEQ, 256, S], BF16)
    X["RX"] = scr("RX", [NSEQ, 1024, S], F32)
    X["RY"] = scr("RY", [NSEQ, 1024, S], F32)
    X["VSW"] = scr("VSW", [NSEQ, 16, 128, 2, 4, 65], BF16)
    X["GATE"] = scr("GATE", [NSEQ, 16, 128, 48], F32)
    X["MIXR"] = scr("MIXR", [NSEQ, 1024, S], BF16)
    X["RSTDR"] = scr("RSTDR", [NSEQ, 128, S], F32)
    X["OATT"] = scr("OATT", [NSEQ, 16, 128, 1024], F32)
    X["MIXA"] = scr("MIXA", [NSEQ, 1024, S], BF16)
    X["RTAB"] = scr("RTAB", [16, 128, GW], F32)
    if debug:
        X["DBGDN"] = scr("DBGDN", [128, 16, 2, 128], BF16)
        X["DBGMB"] = scr("DBGMB", [16, 16, 128], BF16)
    P.X = X

    es = ExitStack()
    with es:
        mk = MK(nc, es)
        P.mk = mk
        P.wb = {k: Buf("wb_" + k) for k in W}
        P.xb = {k: [Buf(f"{k}{s}") for s in range(NSEQ)] for k in X}

        cst = ExitStack()
        with cst:
            C = {}
            C["ident"] = cst.enter_context(nc.sbuf_tensor("c_ident", [128, 128], BF16))
            C["ones"] = cst.enter_context(nc.sbuf_tensor("c_ones", [128, 128], BF16))
            P.C = C
            P.cb = Buf("consts")
            mk.dma("sp", C["ident"][:], I["ident_bf"][:, :], writes=[P.cb])
            mk.dma("sp", C["ones"][:], I["ones_bf"][:, :], writes=[P.cb])
            P.cb.w = dict(P.cb.w)

            if "p0" in phases:
                phase0(P)
            if "p1" in phases:
                for s in range(nseq):
                    phase1(P, s)
            if "p2" in phases:
                for s in range(nseq):
                    phase2(P, s)
            if "p3" in phases:
                phase3_setup(P)
                for s in range(nseq):
                    phase3(P, s)
            if "p4" in phases:
                phase4(P, nseq)
            mk.flush(final=True)
    return nc


def phase0(P):
    mk, I, W = P.mk, P.I, P.W
    P.cast_jobs = []
    ncp, cw, rb = 3, INW // 3, 512
    P.win_jobs = []
    for cp in range(ncp):
        for r0 in range(0, D, rb):
            P.win_jobs.append(("w_in", W["w_in"][r0:r0 + rb, cp * cw:(cp + 1) * cw],
                               I["w_in"][r0:r0 + rb, cp * cw:(cp + 1) * cw]))
    for name, nblk in (("w_out", 8), ("w_g", NFC // 2), ("w_u", NFC // 2)):
        for f0 in range(0, nblk, 8):
            f1 = min(nblk, f0 + 8)
            for c in range(16):
                src = I[name][c * 128:(c + 1) * 128, f0 * 256:f1 * 256].rearrange("p (f j) -> p f j", j=256)
                dst = W[name][f0:f1, :, c * 256:(c + 1) * 256].rearrange("f p j -> p f j")
                P.cast_jobs.append((name, dst, src))
    for fc in range(NFC):
        src = I["w_d"][fc * 128:(fc + 1) * 128, :].rearrange("p (d j) -> p d j", j=128)
        dst = W["w_d"][:, :, fc * 128:(fc + 1) * 128].rearrange("d p j -> p d j")
        P.cast_jobs.append(("w_d", dst, src))
    mk.flush()


def issue_cast(P, job, gate=None):
    mk = P.mk
    name, dst, src = job
    if gate is not None:
        mk._waits("pool", [gate], [])
    b = Buf()
    mk.dma("pool", dst, src, writes=[b], sems="cast")
    evs = P.wb[name].w
    for k, v in b.w.items():
        evs[k] = max(evs.get(k, 0), v)


def issue_casts(P, n, gate=None):
    for _ in range(n):
        if P.cast_jobs:
            issue_cast(P, P.cast_jobs.pop(0), gate)


def act_copy(out, in_, scale=1.0):
    return lambda e: e.activation(out=out, in_=in_, func=AF.Copy, scale=float(scale))


def dve_scale(out, in_, scale=1.0):
    return lambda e: e.tensor_scalar(out=out, in0=in_, scalar1=float(scale), scalar2=None, op0=ALU.mult)


def evac(mk, idx, out, in_, reads, writes, scale=1.0):
    if idx % 2 == 0:
        mk.op("act", act_copy(out, in_, scale), reads=reads, writes=writes)
    else:
        mk.op("dve", dve_scale(out, in_, scale), reads=reads, writes=writes)


def rms_rstd(mk, ps_ap, ps_buf, rt_ap, rt_buf, rstd_ap, rstd_buf, n):
    mk.op("act", lambda e: e.activation(out=rt_ap, in_=ps_ap, func=AF.Sqrt, scale=1.0 / n, bias=P_EPS[0]),
          reads=[ps_buf, P_EPS[1]], writes=[rt_buf])
    mk.op("dve", lambda e: e.reciprocal(out=rstd_ap, in_=rt_ap), reads=[rt_buf], writes=[rstd_buf])


P_EPS = [None, None]


def phase1(P, s):
    nc, mk, I, W, X, C = P.nc, P.mk, P.I, P.W, P.X, P.C
    mk.barrier()
    with ExitStack() as ts:
        def sb(name, shape, dt):
            return ts.enter_context(nc.sbuf_tensor(f"p1_{s}_{name}", shape, dt))

        yT = sb("yT", [128, 16, S], BF16)
        xin = [sb(f"xin{i}", [128, 16, 256], F32) for i in range(2)]
        sq = [sb(f"sq{i}", [128, 16, 256], BF16) for i in range(2)]
        rt = sb("rt", [128, 256], F32)
        rstd = sb("rstd", [128, 256], F32)
        gmix = sb("gmix", [128, 16], F32)
        epst = sb("eps", [128, 1], F32)
        WB = [sb(f"WB{i}", [128, 16, 512], BF16) for i in range(2)]
        Wtok = sb("Wtok", [128, 16, 560], BF16)
        stb = [sb(f"stb{i}", [128, S], BF16) for i in range(2)]
        stf = [sb(f"stf{i}", [128, S], F32) for i in range(2)]
        Vst = [sb(f"Vst{i}", [128, 2, 4, 65], BF16) for i in range(2)]
        gtmp = [sb(f"gtmp{i}", [128, 48], F32) for i in range(2)]
        gst = [sb(f"gst{i}", [128, 48], F32) for i in range(2)]
        bgate = sb("bgate", [128, 48], F32)
        ps = [ts.enter_context(nc.psum_tensor(f"p1_{s}_ps{i}", [128, 512], F32)) for i in range(8)]
        psb = bufs(8, "ps")

        b_small = Buf("small")
        mk.dma("sp", gmix[:], I["g_mix"][:, :], writes=[b_small])
        b_bg = Buf("bgate")
        mk.dma("sp", bgate[:], I["b_gate_bc"][:, :], writes=[b_bg])
        b_eps = Buf("eps")
        mk.op("dve", lambda e: e.memset(epst[:], EPS), writes=[b_eps])
        P_EPS[0] = epst[:]
        P_EPS[1] = b_eps
        b_vst = bufs(2, "vst")
        for i in range(2):
            mk.op("dve", lambda e, i=i: e.memset(Vst[i][:], 1.0), writes=[b_vst[i]])

        b_xin = bufs(2, "xin")
        b_sq = bufs(2, "sq")
        b_rt = Buf("rt")
        b_rstd = Buf("rstd")
        b_yT = bufs(8, "yT")
        xsrc = I["xT"][s].rearrange("(c p) t -> p c t", p=128)
        pcnt = 0
        for j in range(8):
            k = j % 2
            t0 = j * 256
            mk.dma("sp", xin[k][:], xsrc[:, :, t0:t0 + 256], writes=[b_xin[k]])
            for _ in range(2):
                if P.win_jobs:
                    issue_cast(P, P.win_jobs.pop(0), gate=b_xin[k])
            mk.op("act", lambda e, k=k: e.activation(out=sq[k][:], in_=xin[k][:], func=AF.Square),
                  reads=[b_xin[k]], writes=[b_sq[k]])
            pb = 6 + (j % 2)
            for c in range(16):
                mk.op("pe", lambda e, k=k, c=c, pb=pb: e.matmul(ps[pb][:, 0:256], lhsT=C["ones"][:], rhs=sq[k][:, c, :],
                                                                 start=(c == 0), stop=(c == 15)),
                      reads=[b_sq[k], P.cb], writes=[psb[pb]])
            rms_rstd(mk, ps[pb][:, 0:256], psb[pb], rt[:], b_rt, rstd[:], b_rstd, float(D))
            for c in range(16):
                mk.op("dve", lambda e, k=k, c=c, t0=t0: e.scalar_tensor_tensor(
                    out=yT[:, c, t0:t0 + 256], in0=xin[k][:, c, :], scalar=gmix[:, c:c + 1], in1=rstd[:],
                    op0=ALU.mult, op1=ALU.mult), reads=[b_xin[k], b_rstd, b_small], writes=[b_yT[j]])

        while P.win_jobs:
            issue_cast(P, P.win_jobs.pop(0))
        wcols = [[(0, 512)], [(512, 1024)], [(1024, 1536)], [(1536, 1792), (2048, 2304)],
                 [(2608, 3120)], [(3120, 3632)], [(3632, 4144)], [(4144, 4656)]]
        b_WB = bufs(2, "WB")
        wsrc = W["w_in"].rearrange("(c p) n -> p c n", p=128)

        def load_w(w):
            k = w % 2
            o = 0
            for (c0, c1) in wcols[w]:
                mk.dma("sp", WB[k][:, :, o:o + (c1 - c0)], wsrc[:, :, c0:c1], reads=[P.wb["w_in"]], writes=[b_WB[k]])
                o += c1 - c0

        b_Wtok = Buf("Wtok")
        b_stb = [bufs(4, f"stb{i}_") for i in range(2)]
        b_stf = [bufs(4, f"stf{i}_") for i in range(2)]
        load_w(0)
        nb = 0
        nbf = 0
        ecnt = 0
        for w in range(8):
            if w + 1 < 8:
                load_w(w + 1)
            elif True:
                o = 0
                for (c0, c1) in ((1792, 2048), (2304, 2560), (2560, 2608)):
                    mk.dma("sp", Wtok[:, :, o:o + (c1 - c0)], wsrc[:, :, c0:c1], reads=[P.wb["w_in"]], writes=[b_Wtok])
                    o += c1 - c0
            k = w % 2
            for m in range(4):
                ch = 4 * w + m
                isf = ch >= 16
                if isf:
                    sidx = nbf % 2
                    nbf += 1
                    stage, sbufs_ = stf[sidx], b_stf[sidx]
                else:
                    sidx = nb % 2
                    nb += 1
                    stage, sbufs_ = stb[sidx], b_stb[sidx]
                for tg in range(4):
                    pb = pcnt % 6
                    pcnt += 1
                    for c in range(16):
                        mk.op("pe", lambda e, k=k, c=c, m=m, tg=tg, pb=pb: e.matmul(
                            ps[pb][:], lhsT=WB[k][:, c, m * 128:(m + 1) * 128], rhs=yT[:, c, tg * 512:(tg + 1) * 512],
                            start=(c == 0), stop=(c == 15)),
                            reads=[b_WB[k], b_yT[2 * tg], b_yT[2 * tg + 1]], writes=[psb[pb]])
                    if ch >= 24:
                        mk.op("act", lambda e, stage=stage, tg=tg, pb=pb: e.activation(
                            out=stage[:, tg * 512:(tg + 1) * 512], in_=ps[pb][:], func=AF.Gelu_apprx_tanh),
                            reads=[psb[pb]], writes=[sbufs_[tg]])
                    else:
                        evac(mk, ecnt, stage[:, tg * 512:(tg + 1) * 512], ps[pb][:], [psb[pb]], [sbufs_[tg]],
                             scale=(0.125 if ch < 8 else 1.0))
                        ecnt += 1
                if ch < 8:
                    dst = X["QT"][s, ch * 128:(ch + 1) * 128, :]
                    db = P.xb["QT"][s]
                elif ch < 12:
                    dst = X["KC"][s, (ch - 8) * 128:(ch - 7) * 128, :]
                    db = P.xb["KC"][s]
                elif ch < 14:
                    dst = X["KS"][s, (ch - 12) * 128:(ch - 11) * 128, :]
                    db = P.xb["KS"][s]
                elif ch < 16:
                    dst = X["KW"][s, (ch - 14) * 128:(ch - 13) * 128, :]
                    db = P.xb["KW"][s]
                elif ch < 24:
                    dst = X["RX"][s, (ch - 16) * 128:(ch - 15) * 128, :]
                    db = P.xb["RX"][s]
                else:
                    dst = X["RY"][s, (ch - 24) * 128:(ch - 23) * 128, :]
                    db = P.xb["RY"][s]
                dmab = Buf()
                mk.dma("sp", dst, stage[:], reads=sbufs_, writes=[dmab])
                for kk, vv in dmab.w.items():
                    db.w[kk] = max(db.w.get(kk, 0), vv)
            issue_casts(P, 12, gate=sbufs_[3])

        b_gtmp = bufs(2, "gtmp")
        b_gst = bufs(2, "gst")
        for tt in range(16):
            k = tt % 2
            pv = pcnt % 6
            pcnt += 1
            pg = 6 + (tt % 2)
            for c in range(16):
                lhs = yT[:, c, tt * 128:(tt + 1) * 128]
                mk.op("pe", lambda e, c=c, pv=pv, lhs=lhs: e.matmul(ps[pv][:], lhsT=lhs, rhs=Wtok[:, c, 0:512],
                                                                     start=(c == 0), stop=(c == 15)),
                      reads=[b_Wtok, b_yT[tt // 2]], writes=[psb[pv]])
                mk.op("pe", lambda e, c=c, pg=pg, lhs=lhs: e.matmul(ps[pg][:, 0:48], lhsT=lhs, rhs=Wtok[:, c, 512:560],
                                                                     start=(c == 0), stop=(c == 15)),
                      reads=[b_Wtok, b_yT[tt // 2]], writes=[psb[pg]])
            evac(mk, tt, Vst[k][:, :, :, 0:64], ps[pv][:].rearrange("p (a g d) -> p a g d", a=2, g=4),
                 [psb[pv]], [b_vst[k]])
            mk.op("dve", lambda e, k=k, pg=pg: e.tensor_tensor(out=gtmp[k][:], in0=ps[pg][:, 0:48], in1=bgate[:],
                                                                op=ALU.add),
                  reads=[psb[pg], b_bg], writes=[b_gtmp[k]])
            mk.op("act", lambda e, k=k: e.activation(out=gst[k][:], in_=gtmp[k][:], func=AF.Sigmoid),
                  reads=[b_gtmp[k]], writes=[b_gst[k]])
            for (dst, src, sbuf_, key) in ((X["VSW"][s, tt], Vst[k][:], b_vst[k], "VSW"),
                                           (X["GATE"][s, tt], gst[k][:], b_gst[k], "GATE")):
                dmab = Buf()
                mk.dma("sp", dst, src, reads=[sbuf_], writes=[dmab])
                db = P.xb[key][s]
                for kk, vv in dmab.w.items():
                    db.w[kk] = max(db.w.get(kk, 0), vv)
        if s == P.nseq - 1:
            issue_casts(P, 1000)
        mk.flush()


def pc(v, nchunk):
    return np.ascontiguousarray(np.asarray(v, np.float32).reshape(nchunk, 128).T)


def make_in_maps(inp, n_cores=N_CORES):
    f = lambda k: np.asarray(inp[k], np.float32)
    shared = {}
    shared["w_in"] = np.ascontiguousarray(f("w_in")[0])
    shared["w_out"] = np.ascontiguousarray(f("w_out")[0])
    shared["w_g"] = np.ascontiguousarray(f("w_ffn_gate")[0])
    shared["w_u"] = np.ascontiguousarray(f("w_ffn_up")[0])
    shared["w_d"] = np.ascontiguousarray(f("w_ffn_down")[0])
    shared["g_mix"] = pc(f("mix_norm_g")[0], 16)
    shared["g_ffn"] = pc(f("ffn_norm_g")[0], 16)
    shared["g_fin"] = pc(f("final_norm_g"), 16)
    shared["b_gate_bc"] = np.ascontiguousarray(np.broadcast_to(f("b_gate")[0][None, :], (128, 48)))
    shared["g_attn_bc"] = np.ascontiguousarray(np.broadcast_to(f("attn_out_g")[0][None, :], (128, 1024)))
    rv = np.zeros((128, 8, 10), np.float32)
    cw = f("rnn_conv_w")[0]
    for k in range(4):
        rv[:, :, k] = pc(cw[k], 8)
    rv[:, :, 4] = pc(f("rnn_conv_b")[0], 8)
    rv[:, :, 5] = pc(f("rg_a_b")[0], 8)
    rv[:, :, 6] = pc(f("rg_x_b")[0], 8)
    rv[:, :, 7] = pc(f("rg_lambda")[0], 8)
    rv[:, :, 8] = pc(f("rnn_out_g")[0], 8)
    shared["rnn_vec"] = rv
    shared["rg_a_w"] = np.ascontiguousarray(f("rg_a_w")[0])
    shared["rg_x_w"] = np.ascontiguousarray(f("rg_x_w")[0])
    fv = np.zeros((128, NFC, 4), np.float32)
    fw = f("ffn_conv_w")[0]
    for k in range(3):
        fv[:, :, k] = pc(fw[k], NFC)
    fv[:, :, 3] = pc(f("ffn_conv_b")[0], NFC)
    shared["ffn_vec"] = fv
    shared["rel_bias"] = np.ascontiguousarray(f("rel_bias"))
    w1 = np.stack([f("cmp_k_w1")[0], f("cmp_v_w1")[0]])
    shared["cmp_w1"] = np.ascontiguousarray(w1.reshape(2, 32, 64, 256).transpose(0, 2, 1, 3))
    w2 = np.stack([f("cmp_k_w2")[0], f("cmp_v_w2")[0]])
    shared["cmp_w2"] = np.ascontiguousarray(w2.reshape(2, 2, 128, 64).transpose(0, 2, 1, 3))
    pe = np.stack([f("cmp_pe_k")[0], f("cmp_pe_v")[0]])
    shared["cmp_peT"] = np.ascontiguousarray(pe.transpose(0, 2, 1))
    shared.update(host_constants())
    x = f("x")
    maps = []
    for c in range(n_cores):
        m = dict(shared)
        m["xT"] = np.ascontiguousarray(x[c * NSEQ:(c + 1) * NSEQ].transpose(0, 2, 1))
        maps.append(m)
    return maps


_NC_CACHE = {}


def kernel(**inputs):
    if "nc" not in _NC_CACHE:
        _NC_CACHE["nc"] = build()
    nc = _NC_CACHE["nc"]
    maps = make_in_maps(inputs)
    res = run_bass_kernel_spmd(nc, maps, core_ids=list(range(N_CORES)))
    outs = [np.asarray(r["outT"]).transpose(0, 2, 1) for r in res.results]
    return np.ascontiguousarray(np.concatenate(outs, axis=0).astype(np.float32))


import os
P2E = os.environ.get("P2E", "dve")


def phase2(P, s):
    nc, mk, I, W, X, C = P.nc, P.mk, P.I, P.W, P.X, P.C
    mk.barrier()
    with ExitStack() as ts:
        def sb(name, shape, dt):
            return ts.enter_context(nc.sbuf_tensor(f"p2_{s}_{name}", shape, dt))

        rv = sb("rv", [128, 8, 10], F32)
        cl = sb("cl", [128, 8, 2], F32)
        tmp8 = sb("tmp8", [128, 8], F32)
        cst1 = sb("cst1", [128, 2], F32)
        BDa = sb("BDa", [128, 8, 128], BF16)
        BDx = sb("BDx", [128, 8, 128], BF16)
        rxp = [sb(f"rxp{i}", [128, 3 + S], F32) for i in range(2)]
        ryt = [sb(f"ryt{i}", [128, S], F32) for i in range(3)]
        xr_ = [sb(f"xr{i}", [128, S], F32) for i in range(2)]
        xrb_ = [sb(f"xrb{i}", [128, S], BF16) for i in range(2)]
        rr_ = [sb(f"rr{i}", [128, S], F32) for i in range(2)]
        gi_ = [sb(f"gi{i}", [128, S], F32) for i in range(2)]
        aa_ = [sb(f"aa{i}", [128, S], F32) for i in range(2)]
        mm_ = [sb(f"mm{i}", [128, S], F32) for i in range(2)]
        hh_ = [sb(f"hh{i}", [128, S], F32) for i in range(2)]
        sqo_ = [sb(f"sqo{i}", [128, S], BF16) for i in range(2)]
        mst = [sb(f"mst{i}", [128, S], BF16) for i in range(2)]
        rstdr = sb("rstdr", [128, S], F32)
        ps = [ts.enter_context(nc.psum_tensor(f"p2_{s}_ps{i}", [128, 512], F32)) for i in range(8)]
        psb = bufs(8, "ps")

        b_rv = Buf("rv")
        mk.dma("sp", rv[:], I["rnn_vec"][:, :, :], writes=[b_rv])
        b_cst = Buf("cst")
        mk.op("dve", lambda e: e.memset(cst1[:, 0:1], 1.0), writes=[b_cst])
        mk.op("dve", lambda e: e.memset(cst1[:, 1:2], EPS), writes=[b_cst])
        b_cl = Buf("cl")
        b_t8 = Buf("t8")
        mk.op("act", lambda e: e.activation(out=tmp8[:], in_=rv[:, :, 7], func=AF.Exp, scale=-1.0),
              reads=[b_rv], writes=[b_t8])
        mk.op("act", lambda e: e.activation(out=tmp8[:], in_=tmp8[:], func=AF.Ln, bias=cst1[:, 0:1]),
              reads=[b_t8, b_cst], writes=[b_t8])
        mk.op("dve", lambda e: e.tensor_scalar(out=cl[:, :, 0], in0=tmp8[:], scalar1=-8.0, scalar2=None, op0=ALU.mult),
              reads=[b_t8], writes=[b_cl])
        mk.op("dve", lambda e: e.tensor_scalar(out=cl[:, :, 1], in0=tmp8[:], scalar1=-16.0, scalar2=None, op0=ALU.mult),
              reads=[b_t8], writes=[b_cl])
        b_bd = Buf("bd")
        mk.op("dve", lambda e: e.memset(BDa[:], 0.0), writes=[b_bd])
        mk.op("dve", lambda e: e.memset(BDx[:], 0.0), writes=[b_bd])
        for (bd, key) in ((BDa, "rg_a_w"), (BDx, "rg_x_w")):
            src = I[key].rearrange("(c two) i j -> two i c j", two=2)
            mk.dma("pool", bd[0:64, :, 0:64], src[0], writes=[b_bd])
            mk.dma("pool", bd[64:128, :, 64:128], src[1], writes=[b_bd])
        b_rxp = bufs(2, "rxp")
        b_ry = bufs(3, "ry")
        for i in range(2):
            mk.op("dve", lambda e, i=i: e.memset(rxp[i][:, 0:3], 0.0), writes=[b_rxp[i]])
        b_xr_, b_xrb_, b_rr_, b_gi_, b_aa_, b_mm_, b_hh_, b_sqo_ = (bufs(2, n) for n in
                                                                      ("xr", "xrb", "rr", "gi", "aa", "mm", "hh", "sqo"))
        b_mst = bufs(2, "mst")

        def load(c):
            k = c % 2
            mk.dma("sp", rxp[k][:, 3:3 + S], X["RX"][s, c * 128:(c + 1) * 128, :], reads=[P.xb["RX"][s]],
                   writes=[b_rxp[k]])
            mk.dma("sp", ryt[c % 3][:], X["RY"][s, c * 128:(c + 1) * 128, :], reads=[P.xb["RY"][s]],
                   writes=[b_ry[c % 3]])

        load(0)

        def front(c):
            k = c % 2
            if c + 1 < 8:
                load(c + 1)
            xr, xrb, rr, gi, aa, mm, hh, sqo = (t_[k] for t_ in (xr_, xrb_, rr_, gi_, aa_, mm_, hh_, sqo_))
            b_xr, b_xrb, b_rr, b_gi, b_aa, b_mm, b_hh, b_sqo = (t_[k] for t_ in (b_xr_, b_xrb_, b_rr_, b_gi_, b_aa_,
                                                                                  b_mm_, b_hh_, b_sqo_))
            mk.op("act", lambda e, k=k, c=c: e.activation(out=xr[:], in_=rxp[k][:, 3:3 + S], func=AF.Identity,
                                                          scale=rv[:, c, 3:4], bias=rv[:, c, 4:5]),
                  reads=[b_rxp[k], b_rv], writes=[b_xr])
            for kk in range(3):
                mk.op("dve", lambda e, k=k, c=c, kk=kk: e.scalar_tensor_tensor(
                    out=xr[:], in0=rxp[k][:, kk:kk + S], scalar=rv[:, c, kk:kk + 1], in1=xr[:],
                    op0=ALU.mult, op1=ALU.add), reads=[b_rxp[k], b_rv, b_xr], writes=[b_xr])
            mk.op("dve", lambda e: e.tensor_copy(out=xrb[:], in_=xr[:]), reads=[b_xr], writes=[b_xrb])
            for tg in range(4):
                sl = slice(tg * 512, (tg + 1) * 512)
                pr = tg % 2
                pg = 2 + tg % 2
                mk.op("pe", lambda e, c=c, sl=sl, pr=pr: e.matmul(ps[pr][:], lhsT=BDa[:, c, :], rhs=xrb[:, sl],
                                                                 start=True, stop=True),
                      reads=[b_bd, b_xrb], writes=[psb[pr]])
                mk.op("pe", lambda e, c=c, sl=sl, pg=pg: e.matmul(ps[pg][:], lhsT=BDx[:, c, :], rhs=xrb[:, sl],
                                                                 start=True, stop=True),
                      reads=[b_bd, b_xrb], writes=[psb[pg]])
                mk.op("act", lambda e, c=c, sl=sl, pr=pr: e.activation(out=rr[:, sl], in_=ps[pr][:], func=AF.Sigmoid,
                                                                      bias=rv[:, c, 5:6]),
                      reads=[psb[pr], b_rv], writes=[b_rr])
                mk.op("act", lambda e, c=c, sl=sl, pg=pg: e.activation(out=gi[:, sl], in_=ps[pg][:], func=AF.Sigmoid,
                                                                      bias=rv[:, c, 6:7]),
                      reads=[psb[pg], b_rv], writes=[b_gi])
            mk.op("act", lambda e, c=c: e.activation(out=aa[:], in_=rr[:], func=AF.Exp, scale=cl[:, c, 0:1]),
                  reads=[b_rr, b_cl], writes=[b_aa])
            mk.op("act", lambda e, c=c: e.activation(out=mm[:], in_=rr[:], func=AF.Exp, scale=cl[:, c, 1:2]),
                  reads=[b_rr, b_cl], writes=[b_mm])
            mk.op("act", lambda e: e.activation(out=mm[:], in_=mm[:], func=AF.Sqrt, scale=-1.0, bias=cst1[:, 0:1]),
                  reads=[b_mm, b_cst], writes=[b_mm])

        def back(c):
            k = c % 2
            xr, xrb, rr, gi, aa, mm, hh, sqo = (t_[k] for t_ in (xr_, xrb_, rr_, gi_, aa_, mm_, hh_, sqo_))
            b_xr, b_xrb, b_rr, b_gi, b_aa, b_mm, b_hh, b_sqo = (t_[k] for t_ in (b_xr_, b_xrb_, b_rr_, b_gi_, b_aa_,
                                                                                  b_mm_, b_hh_, b_sqo_))
            mk.op(P2E, lambda e: e.tensor_tensor(out=gi[:], in0=gi[:], in1=xr[:], op=ALU.mult),
                  reads=[b_gi, b_xr], writes=[b_gi])
            mk.op(P2E, lambda e: e.tensor_tensor(out=gi[:], in0=gi[:], in1=mm[:], op=ALU.mult),
                  reads=[b_gi, b_mm], writes=[b_gi])
            mk.op("dve", lambda e: e.tensor_tensor_scan(out=hh[:], data0=aa[:], data1=gi[:], initial=0.0,
                                                        op0=ALU.mult, op1=ALU.add),
                  reads=[b_aa, b_gi], writes=[b_hh])
            mk.op("dve", lambda e, c=c: e.tensor_tensor(out=hh[:], in0=hh[:], in1=ryt[c % 3][:], op=ALU.mult),
                  reads=[b_hh, b_ry[c % 3]], writes=[b_hh])
            mk.op("act", lambda e: e.activation(out=sqo[:], in_=hh[:], func=AF.Square), reads=[b_hh], writes=[b_sqo])
            for tg in range(4):
                sl = slice(tg * 512, (tg + 1) * 512)
                mk.op("pe", lambda e, c=c, sl=sl, tg=tg: e.matmul(ps[4 + tg][:], lhsT=C["ones"][:], rhs=sqo[:, sl],
                                                                 start=(c == 0), stop=(c == 7)),
                      reads=[b_sqo, P.cb], writes=[psb[4 + tg]])
            mk.op("dve", lambda e, k=k, c=c: e.tensor_scalar(out=mst[k][:], in0=hh[:], scalar1=rv[:, c, 8:9],
                                                             scalar2=None, op0=ALU.mult),
                  reads=[b_hh, b_rv], writes=[b_mst[k]])
            dmab = Buf()
            mk.dma("pool", X["MIXR"][s, c * 128:(c + 1) * 128, :], mst[k][:], reads=[b_mst[k]], writes=[dmab])
            db = P.xb["MIXR"][s]
            for kk_, vv in dmab.w.items():
                db.w[kk_] = max(db.w.get(kk_, 0), vv)
        front(0)
        for c in range(8):
            if c + 1 < 8:
                front(c + 1)
            back(c)
        b_rs = Buf("rstdr")
        for tg in range(4):
            sl = slice(tg * 512, (tg + 1) * 512)
            mk.op("act", lambda e, sl=sl, tg=tg: e.activation(out=rstdr[:, sl], in_=ps[4 + tg][:], func=AF.Sqrt,
                                                              scale=1.0 / 1024.0, bias=cst1[:, 1:2]),
                  reads=[psb[4 + tg], b_cst], writes=[b_rs])
        mk.op("dve", lambda e: e.reciprocal(out=rstdr[:], in_=rstdr[:]), reads=[b_rs], writes=[b_rs])
        mk.dma("pool", X["RSTDR"][s], rstdr[:], reads=[b_rs], writes=[P.xb["RSTDR"][s]])
        mk.flush()


def merge_ev(dst_buf, src_buf):
    for kk, vv in src_buf.w.items():
        dst_buf.w[kk] = max(dst_buf.w.get(kk, 0), vv)


def phase3_setup(P):
    pass


def phase3(P, s):
    if s == 0:
        phase3_all(P)


def phase3_all(P):
    nc, mk, I, W, X, C = P.nc, P.mk, P.I, P.W, P.X, P.C
    nseq = P.nseq
    mk.barrier()
    with ExitStack() as ts:
        def sb(name, shape, dt):
            return ts.enter_context(nc.sbuf_tensor(f"p3_{name}", shape, dt))

        DNb = sb("DNb", [128, 16, 2, 128], BF16)
        DN4b = sb("DN4b", [128, 4, 128], BF16)
        Mb = sb("Mb", [16, 16, 128], BF16)
        zc = sb("zc", [16, 272], BF16)
        W1 = sb("W1", [64, 2, 32, 256], BF16)
        W2 = sb("W2", [128, 2, 2, 64], BF16)
        peT = sb("peT", [64, 2, 34], BF16)
        pebias = sb("pebias", [128, 2, 2], F32)
        VAL = sb("VAL", [128, 16, 32], F32)
        ADD = sb("ADD", [128, 16, 32], F32)
        gbc = sb("gbc", [128, 1024], F32)
        KsA = sb("KsA", [96, S], BF16)
        VCA = sb("VCA", [128, 4, 97], BF16)
        NSP = [sb(f"NSP{i}", [128, 96], BF16) for i in range(2)]
        cst1 = sb("cst1", [128, 2], F32)
        ts2 = ExitStack()

        def sb2(name, shape, dt):
            return ts2.enter_context(nc.sbuf_tensor(f"p3t_{name}", shape, dt))

        tab = sb2("tab", [32, 16], F32)
        tabb = [sb2(f"tabb{i}", [32, 128], F32) for i in range(2)]
        oh = sb2("oh", [32, GW], F32)
        rst = [sb2(f"rst{i}", [128, GW], F32) for i in range(2)]
        dn01 = sb2("dn01", [128, 16, 2, 128], F32)
        m0 = sb2("m0", [128, 128], F32)
        negm0 = sb2("negm0", [128, 128], F32)
        m4 = sb2("m4", [128, 128], F32)
        mbf = sb2("mbf", [16, 16, 128], F32)
        cvm = sb2("cvm", [16, 128], F32)
        negcv = sb2("negcv", [16, 128], F32)
        ps = [ts.enter_context(nc.psum_tensor(f"p3_ps{i}", [128, 512], F32)) for i in range(8)]
        psb = bufs(8, "ps")

        b_c = Buf("p3c")
        for (t_, src) in ((tab, I["rel_bias"]), (oh, I["oh"]), (m0, I["mask_d0"]), (m4, I["mask_d4"]),
                          (cvm, I["cmp_valid"]), (zc, I["zc"]), (VAL, I["sel_val"]), (ADD, I["sel_add"]),
                          (gbc, I["g_attn_bc"])):
            b = Buf()
            mk.dma("sp", t_[:], src, writes=[b])
            merge_ev(b_c, b)
        b = Buf()
        mk.dma("sp", KsA[64:96, :], I["e_rows"][:, :], writes=[b])
        merge_ev(b_c, b)
        for i in range(2):
            b = Buf()
            mk.op("dve", lambda e, i=i: e.memset(NSP[i][:], 0.0), writes=[b])
            merge_ev(b_c, b)
        b = Buf()
        mk.op("dve", lambda e: e.memset(cst1[:, 0:1], 1.0), writes=[b])
        mk.op("dve", lambda e: e.memset(cst1[:, 1:2], EPS), writes=[b])
        merge_ev(b_c, b)
        b_vca = Buf("vca")
        mk.op("dve", lambda e: e.memset(VCA[:], 1.0), writes=[b_vca])
        for g in range(4):
            mk.dma("pool", VCA[0:NCMP, g, 65:97], I["overlap"][:, :], writes=[b_vca])
        b_w1 = Buf("w1")
        mk.dma("pool", W1[:, 0], I["cmp_w1"][0], writes=[b_w1])
        mk.dma("pool", W1[:, 1], I["cmp_w1"][1], writes=[b_w1])
        mk.dma("pool", W2[:], I["cmp_w2"].rearrange("kv p m e -> p kv m e"), writes=[b_w1])
        mk.op("dve", lambda e: e.memset(peT[:], 0.0), writes=[b_w1])
        mk.dma("pool", peT[:, :, 0:32], I["cmp_peT"].rearrange("kv d l -> d kv l"), writes=[b_w1])

        b_tabb = bufs(2, "tabb")
        b_rst = bufs(2, "rst")
        b_rtab = Buf("rtab")
        for h in range(16):
            k = h % 2
            mk.op("dve", lambda e, k=k, h=h: e.tensor_copy(out=tabb[k][:], in_=tab[:, h:h + 1].to_broadcast([32, 128])),
                  reads=[b_c], writes=[b_tabb[k]])
            mk.op("pe", lambda e, k=k: e.matmul(ps[k][:, 0:GW], lhsT=tabb[k][:], rhs=oh[:], start=True, stop=True),
                  reads=[b_tabb[k], b_c], writes=[psb[k]])
            mk.op("act", act_copy(rst[k][:], ps[k][:, 0:GW]), reads=[psb[k]], writes=[b_rst[k]])
            b = Buf()
            mk.dma("sp", X["RTAB"][h], rst[k][:], reads=[b_rst[k]], writes=[b])
            merge_ev(b_rtab, b)
        b_dn = Buf("dn")
        rt_t = X["RTAB"].tensor
        mk.dma("sp", dn01[:], bass.AP(rt_t, 127, [[GW - 1, 128], [128 * GW, 16], [128, 2], [1, 128]]),
               reads=[b_rtab], writes=[b_dn])
        b_mb = Buf("mbf")
        mk.dma("sp", mbf[:], bass.AP(rt_t, 224, [[GW - 16, 16], [128 * GW, 16], [1, 128]]), reads=[b_rtab],
               writes=[b_mb])
        b_m = Buf("masks")
        mk.op("dve", lambda e: e.tensor_scalar(out=negm0[:], in0=m0[:], scalar1=-1.0, scalar2=-NEG, op0=ALU.add,
                                               op1=ALU.mult), reads=[b_c], writes=[b_m])
        mk.op("dve", lambda e: e.tensor_scalar(out=m4[:], in0=m4[:], scalar1=-1.0, scalar2=-NEG, op0=ALU.add,
                                               op1=ALU.mult), reads=[b_c], writes=[b_m])
        mk.op("dve", lambda e: e.tensor_scalar(out=negcv[:], in0=cvm[:], scalar1=-1.0, scalar2=-NEG, op0=ALU.add,
                                               op1=ALU.mult), reads=[b_c], writes=[b_m])
        b_DN = Buf("DN")
        mk.op("dve", lambda e: e.tensor_tensor(out=dn01[:, :, 0, :], in0=dn01[:, :, 0, :],
                                               in1=m0[:].unsqueeze(1).to_broadcast([128, 16, 128]), op=ALU.mult),
              reads=[b_dn, b_c], writes=[b_dn])
        mk.op("dve", lambda e: e.tensor_tensor(out=DNb[:, :, 0, :], in0=dn01[:, :, 0, :],
                                               in1=negm0[:].unsqueeze(1).to_broadcast([128, 16, 128]), op=ALU.add),
              reads=[b_dn, b_m], writes=[b_DN])
        mk.op("dve", lambda e: e.tensor_copy(out=DNb[:, :, 1, :], in_=dn01[:, :, 1, :]), reads=[b_dn], writes=[b_DN])
        mk.op("dve", lambda e: e.tensor_copy(out=DN4b[:], in_=m4[:].unsqueeze(1).to_broadcast([128, 4, 128])),
              reads=[b_m], writes=[b_DN])
        mk.op("dve", lambda e: e.tensor_tensor(out=mbf[:], in0=mbf[:],
                                               in1=cvm[:].unsqueeze(1).to_broadcast([16, 16, 128]), op=ALU.mult),
              reads=[b_mb, b_c], writes=[b_mb])
        mk.op("dve", lambda e: e.tensor_tensor(out=Mb[:], in0=mbf[:],
                                               in1=negcv[:].unsqueeze(1).to_broadcast([16, 16, 128]), op=ALU.add),
              reads=[b_mb, b_m], writes=[b_DN])
        b_pb = Buf("pebias")
        for kv in range(2):
            for mc in range(2):
                pb = 2 + mc
                for l in range(32):
                    mk.op("pe", lambda e, kv=kv, mc=mc, l=l, pb=pb: e.matmul(
                        ps[pb][:, 0:2], lhsT=W1[:, kv, l, mc * 128:(mc + 1) * 128], rhs=peT[:, kv, l:l + 2],
                        start=(l == 0), stop=(l == 31)), reads=[b_w1], writes=[psb[pb]])
                mk.op("act", act_copy(pebias[:, kv, mc:mc + 1], ps[pb][:, 0:1]), reads=[psb[pb]], writes=[b_pb])

        if "DBGDN" in X:
            mk.dma("sp", X["DBGDN"], DNb[:], reads=[b_DN], writes=[Buf()])
            mk.dma("sp", X["DBGMB"], Mb[:], reads=[b_DN], writes=[Buf()])
        mk.flush()
        ts2.close()
        L = dict(locals())
        for s in range(nseq):
            attention_seq(P, s, L)


def attention_seq(P, s, L):
    nc, mk, I, W, X, C = P.nc, P.mk, P.I, P.W, P.X, P.C
    ps, psb = L["ps"], L["psb"]
    b_c, b_DN, b_w1, b_pb, b_vca = L["b_c"], L["b_DN"], L["b_w1"], L["b_pb"], L["b_vca"]
    W1, W2, pebias, VCA, KsA, zc, Mb, DNb, DN4b = (L[k] for k in ("W1", "W2", "pebias", "VCA", "KsA", "zc", "Mb",
                                                                 "DNb", "DN4b"))
    VAL, ADD, gbc, NSP, cst1 = L["VAL"], L["ADD"], L["gbc"], L["NSP"], L["cst1"]
    ident = C["ident"]
    with ExitStack() as ts:
        def sb(name, shape, dt):
            return ts.enter_context(nc.sbuf_tensor(f"p3s_{s}_{name}", shape, dt))

        big = sb("big", [128, 16384], BF16)
        KCt = big[0:64, :].rearrange("p (kv g t) -> p kv g t", kv=2, g=4)
        HT = sb("HT", [128, 2, 2, 508], BF16)
        KCMP = sb("KCMP", [64, 4, NCMP], BF16)
        Gt = sb("Gt", [128, 16, 48], F32)
        QA = sb("QA", [96, 4, S], BF16)
        KwT = sb("KwT", [64, S], BF16)
        Vsw = sb("Vsw", [128, 16, 2, 65], BF16)
        PTc = [sb(f"PTc{i}", [128, 512], BF16) for i in range(2)]
        PTs = [big[:, i * 8192:(i + 1) * 8192].rearrange("p (k n) -> p k n", k=16) for i in range(2)]
        PTw = [sb(f"PTw{i}", [128, 5, 512], BF16) for i in range(2)]
        ocmp = sb("ocmp", [128, 16, 4, 64], F32)
        oacc = [sb(f"oacc{i}", [128, 4, 64], F32) for i in range(2)]
        rs = sb("rs", [128, 4], F32)
        rinv = sb("rinv", [128, 4], F32)
        coef = sb("coef", [128, 4], F32)
        coefc = [sb(f"coefc{i}", [128, 4], F32) for i in range(2)]
        imp = sb("imp", [128, 32], F32)
        score = sb("score", [128, 32], F32)
        sc2 = sb("sc2", [128, 32], F32)
        m8a = sb("m8a", [128, 8], F32)
        m8b = sb("m8b", [128, 8], F32)
        thr = sb("thr", [128, 1], F32)
        selt = sb("selt", [128, 32], F32)
        oat = [sb(f"oat{i}", [128, 1024], F32) for i in range(2)]
        junk = sb("junk", [128, 1024], BF16)
        ssq = sb("ssq", [128, 1], F32)
        rstd = sb("rstd", [128, 1], F32)
        mtok = [sb(f"mtok{i}", [128, 1024], BF16) for i in range(2)]
        mixst = [sb(f"mixst{i}", [128, 8, 128], BF16) for i in range(2)]
        pst = [ts.enter_context(nc.psum_tensor(f"p3s_{s}_pst{i}", [128, 8, 128], BF16)) for i in range(0)]

        mk.barrier()
        b_kct = Buf("kct")
        mk.dma("sp", KCt, X["KC"][s].rearrange("(kv g d) t -> d kv g t", kv=2, g=4), reads=[P.xb["KC"][s]],
               writes=[b_kct])
        b_G = Buf("G")
        mk.dma("sp", Gt[:], X["GATE"][s].rearrange("tt p e -> p tt e"), reads=[P.xb["GATE"][s]], writes=[b_G])
        b_ht = bufs(4, "ht")
        for kv in range(2):
            for mc in range(2):
                pb = (2 * kv + mc) % 4
                for l in range(32):
                    mk.op("pe", lambda e, kv=kv, mc=mc, l=l, pb=pb: e.matmul(
                        ps[pb][:, 0:508].rearrange("p (g c) -> p g c", g=4),
                        lhsT=W1[:, kv, l, mc * 128:(mc + 1) * 128], rhs=KCt[:, kv, :, l:l + 2017:16],
                        start=(l == 0), stop=(l == 31)), reads=[b_w1, b_kct], writes=[psb[pb]])
                mk.op("act", lambda e, kv=kv, mc=mc, pb=pb: e.activation(
                    out=HT[:, kv, mc, :], in_=ps[pb][:, 0:508], func=AF.Gelu_apprx_tanh, bias=pebias[:, kv, mc:mc + 1]),
                    reads=[psb[pb], b_pb], writes=[b_ht[2 * kv + mc]])
        b_kcmp = Buf("kcmp")
        for mc in range(2):
            mk.op("pe", lambda e, mc=mc: e.matmul(ps[4][0:64, 0:508], lhsT=W2[:, 0, mc, :], rhs=HT[:, 0, mc, :],
                                                  start=(mc == 0), stop=(mc == 1)),
                  reads=[b_w1, b_ht[0], b_ht[1]], writes=[psb[4]])
        mk.op("act", act_copy(KCMP[:], ps[4][0:64, 0:508].rearrange("p (g c) -> p g c", g=4)), reads=[psb[4]],
              writes=[b_kcmp])
        b_vc = Buf("vcmp")
        merge_ev(b_vc, b_vca)
        for g in range(4):
            pb = 5 + g % 2
            for mc in range(2):
                mk.op("pe", lambda e, mc=mc, g=g, pb=pb: e.matmul(
                    ps[pb][0:NCMP, 0:64], lhsT=HT[:, 1, mc, g * NCMP:(g + 1) * NCMP], rhs=W2[:, 1, mc, :],
                    start=(mc == 0), stop=(mc == 1)), reads=[b_w1, b_ht[2], b_ht[3]], writes=[psb[pb]])
            bb = Buf()
            mk.op("dve", lambda e, g=g, pb=pb: e.tensor_copy(out=VCA[0:NCMP, g, 0:64], in_=ps[pb][0:NCMP, 0:64]),
                  reads=[psb[pb], b_vca], writes=[bb])
            merge_ev(b_vc, bb)
        b_vca.r = {}
        L["b_vca_last"] = b_vc

        mk.barrier()
        b_qa = Buf("qa")
        b_qsel = bufs(16, "qsel")
        b_ks = Buf("ks")
        b_kw = Buf("kw")
        b_v = Buf("v")
        b_ptc = bufs(2, "ptc")
        b_pts = [bufs(16, f"pts{i}_") for i in range(2)]
        b_ptw = [bufs(5, f"ptw{i}_") for i in range(2)]
        b_ocmp = bufs(16, "ocmp")
        b_oacc = bufs(2, "oacc")
        b_t = Buf("dvetmp")
        b_coefc = bufs(2, "coefc")
        b_nsp = bufs(2, "nsp")
        for i in range(2):
            merge_ev(b_nsp[i], b_c)
        scnt = [0]

        def sbank():
            b = scnt[0] % 3
            scnt[0] += 1
            return b

        for g in range(4):
            mk.dma("sp", QA[0:64, :, :], X["QT"][s, g * 256:(g + 1) * 256, :].rearrange("(r d) t -> d r t", r=4),
                   reads=[P.xb["QT"][s]], writes=[b_qa] + b_qsel)
            mk.dma("sp", KsA[0:64, :], X["KS"][s, g * 64:(g + 1) * 64, :], reads=[P.xb["KS"][s]], writes=[b_ks])
            mk.dma("sp", KwT[:], X["KW"][s, g * 64:(g + 1) * 64, :], reads=[P.xb["KW"][s]], writes=[b_kw])
            for a_ in range(2):
                mk.dma("sp", Vsw[:, :, a_, :], X["VSW"][s][:, :, a_, g, :].rearrange("tt p e -> p tt e"),
                       reads=[P.xb["VSW"][s]], writes=[b_v])

            def la1(qt):
                nk = min(NCMP, 8 * qt + 7)
                qs = slice(qt * 128, (qt + 1) * 128)
                sbk = sbank()
                k2 = qt % 2
                off = 136 - 8 * qt
                mk.op("pe", lambda e, g=g, nk=nk, qs=qs, sbk=sbk: e.matmul(
                    ps[sbk][0:nk, :].rearrange("p (r i) -> p r i", r=4), lhsT=KCMP[:, g, 0:nk], rhs=QA[0:64, :, qs],
                    start=True, stop=False), reads=[b_kcmp, b_qa], writes=[psb[sbk]])
                mk.op("pe", lambda e, g=g, nk=nk, off=off, sbk=sbk: e.matmul(
                    ps[sbk][0:nk, :].rearrange("p (r i) -> p r i", r=4), lhsT=zc[0:16, off:off + nk],
                    rhs=Mb[0:16, 4 * g:4 * g + 4, :], start=False, stop=True), reads=[b_c, b_DN], writes=[psb[sbk]])
                mk.op("act", lambda e, nk=nk, sbk=sbk, k2=k2: e.activation(out=PTc[k2][0:nk, :], in_=ps[sbk][0:nk, :],
                                                                          func=AF.Exp),
                      reads=[psb[sbk]], writes=[b_ptc[k2]])
                ob = 3
                for r in range(4):
                    mk.op("pe", lambda e, r=r, nk=nk, g=g, k2=k2, ob=ob: e.matmul(
                        ps[ob][:, r * 128:r * 128 + 97], lhsT=PTc[k2][0:nk, r * 128:(r + 1) * 128], rhs=VCA[0:nk, g, :],
                        start=True, stop=True), reads=[b_ptc[k2], b_vc], writes=[psb[ob]])

            def la2a(qt):
                k2 = qt % 2
                qs = slice(qt * 128, (qt + 1) * 128)
                ob = 3
                O = ps[ob][:, :].rearrange("p (r e) -> p r e", r=4)
                mk.op("dve", lambda e, O=O: e.tensor_scalar(out=rs[:], in0=O[:, :, 64], scalar1=1e-30, scalar2=None,
                                                            op0=ALU.max), reads=[psb[ob]], writes=[b_t])
                mk.op("dve", lambda e: e.reciprocal(out=rinv[:], in_=rs[:]), reads=[b_t], writes=[b_t])
                mk.op("dve", lambda e, O=O: e.tensor_scalar(out=imp[:], in0=O[:, 0, 65:97], scalar1=rinv[:, 0:1],
                                                            scalar2=None, op0=ALU.mult),
                      reads=[psb[ob], b_t], writes=[b_t])
                for r in range(1, 4):
                    mk.op("dve", lambda e, O=O, r=r: e.scalar_tensor_tensor(
                        out=imp[:], in0=O[:, r, 65:97], scalar=rinv[:, r:r + 1], in1=imp[:], op0=ALU.mult,
                        op1=ALU.add), reads=[psb[ob], b_t], writes=[b_t])
                mk.op("dve", lambda e, qt=qt, g=g: e.tensor_tensor(out=coef[:], in0=rinv[:],
                                                                  in1=Gt[:, qt, g * 12:g * 12 + 12:3], op=ALU.mult),
                      reads=[b_t, b_G], writes=[b_t])
                for r in range(4):
                    mk.op("dve", lambda e, O=O, r=r, qt=qt: e.tensor_scalar(
                        out=ocmp[:, qt, r, :], in0=O[:, r, 0:64], scalar1=coef[:, r:r + 1], scalar2=None,
                        op0=ALU.mult), reads=[psb[ob], b_t], writes=[b_ocmp[qt]])
                mk.op("dve", lambda e, qt=qt: e.tensor_tensor(out=score[:], in0=imp[:], in1=VAL[:, qt, :], op=ALU.mult),
                      reads=[b_t, b_c], writes=[b_t])
                mk.op("dve", lambda e, qt=qt: e.tensor_tensor(out=score[:], in0=score[:], in1=ADD[:, qt, :], op=ALU.add),
                      reads=[b_t, b_c], writes=[b_t])
                mk.op("dve", lambda e: e.max(out=m8a[:], in_=score[:]), reads=[b_t], writes=[b_t])
                mk.op("dve", lambda e: e.match_replace(out=sc2[:], in_to_replace=m8a[:], in_values=score[:],
                                                       imm_value=-1e30), reads=[b_t], writes=[b_t])
                mk.op("dve", lambda e: e.max(out=m8b[:], in_=sc2[:]), reads=[b_t], writes=[b_t])
                mk.op("dve", lambda e: e.tensor_scalar(out=thr[:], in0=m8b[:, 7:8], scalar1=0.0, scalar2=None,
                                                       op0=ALU.max), reads=[b_t], writes=[b_t])
                mk.op("dve", lambda e: e.tensor_scalar(out=selt[:], in0=score[:], scalar1=thr[:, 0:1], scalar2=None,
                                                       op0=ALU.is_ge), reads=[b_t], writes=[b_t])
                mk.op("dve", lambda e, k2=k2: e.tensor_scalar(out=NSP[k2][:, 64:96], in0=selt[:], scalar1=-1.0,
                                                              scalar2=-NEG, op0=ALU.add, op1=ALU.mult),
                      reads=[b_t], writes=[b_nsp[k2]])

            def la2b(qt):
                k2 = qt % 2
                qs = slice(qt * 128, (qt + 1) * 128)
                tb = sbank()
                mk.op("pe", lambda e, k2=k2, tb=tb: e.matmul(ps[tb][0:96, 0:128], lhsT=NSP[k2][:, 0:96], rhs=ident[:],
                                                             start=True, stop=True),
                      reads=[b_nsp[k2], P.cb], writes=[psb[tb]])
                mk.op("act", lambda e, tb=tb, qs=qs: e.activation(
                    out=QA[64:96, :, qs], in_=ps[tb][64:96, 0:128].unsqueeze(1).to_broadcast([32, 4, 128]),
                    func=AF.Copy), reads=[psb[tb]], writes=[b_qsel[qt]])


            def qk(qt):
                k2 = qt % 2
                qs = slice(qt * 128, (qt + 1) * 128)
                for kt in range(0, qt + 1):
                    yield
                    sbk = sbank()
                    ks_ = slice(kt * 128, (kt + 1) * 128)
                    near = kt >= qt - 1
                    mk.op("pe", lambda e, sbk=sbk, ks_=ks_, qs=qs, near=near: e.matmul(
                        ps[sbk][:, :].rearrange("p (r i) -> p r i", r=4), lhsT=KsA[0:96, ks_], rhs=QA[0:96, :, qs],
                        start=True, stop=(not near)), reads=[b_ks, b_c, b_qa, b_qsel[qt]], writes=[psb[sbk]])
                    if near:
                        mk.op("pe", lambda e, sbk=sbk, g=g, dl=qt - kt: e.matmul(
                            ps[sbk][:, :].rearrange("p (r i) -> p r i", r=4), lhsT=ident[:],
                            rhs=DNb[:, 4 * g:4 * g + 4, dl, :], start=False, stop=True),
                            reads=[P.cb, b_DN], writes=[psb[sbk]])
                    mk.op("act", lambda e, sbk=sbk, k2=k2, kt=kt: e.activation(out=PTs[k2][:, kt, :], in_=ps[sbk][:, :],
                                                                              func=AF.Exp),
                          reads=[psb[sbk]], writes=[b_pts[k2][kt]])
                for wi, kt in enumerate(range(max(0, qt - 4), qt + 1)):
                    yield
                    sbk = sbank()
                    ks_ = slice(kt * 128, (kt + 1) * 128)
                    dl = qt - kt
                    sp_ = dl in (0, 1, 4)
                    mk.op("pe", lambda e, sbk=sbk, ks_=ks_, qs=qs, sp_=sp_: e.matmul(
                        ps[sbk][:, :].rearrange("p (r i) -> p r i", r=4), lhsT=KwT[0:64, ks_], rhs=QA[0:64, :, qs],
                        start=True, stop=(not sp_)), reads=[b_kw, b_qa], writes=[psb[sbk]])
                    if sp_:
                        rhs = DN4b[:] if dl == 4 else DNb[:, 4 * g:4 * g + 4, dl, :]
                        mk.op("pe", lambda e, sbk=sbk, rhs=rhs: e.matmul(
                            ps[sbk][:, :].rearrange("p (r i) -> p r i", r=4), lhsT=ident[:], rhs=rhs, start=False,
                            stop=True), reads=[P.cb, b_DN], writes=[psb[sbk]])
                    mk.op("act", lambda e, sbk=sbk, k2=k2, wi=wi: e.activation(out=PTw[k2][:, wi, :], in_=ps[sbk][:, :],
                                                                              func=AF.Exp),
                          reads=[psb[sbk]], writes=[b_ptw[k2][wi]])

            def pv(qt):
                k2 = qt % 2
                obs = 4 + 2 * k2
                obw = 5 + 2 * k2
                for r in range(4):
                    for kt in range(0, qt + 1):
                        if kt % 4 == 0:
                            yield
                        mk.op("pe", lambda e, r=r, kt=kt, k2=k2, obs=obs, qt=qt: e.matmul(
                            ps[obs][:, r * 128:r * 128 + 65], lhsT=PTs[k2][:, kt, r * 128:(r + 1) * 128],
                            rhs=Vsw[:, kt, 0, :], start=(kt == 0), stop=(kt == qt)),
                            reads=[b_pts[k2][kt], b_v], writes=[psb[obs]])
                kts = list(range(max(0, qt - 4), qt + 1))
                for r in range(4):
                    yield
                    for wi, kt in enumerate(kts):
                        mk.op("pe", lambda e, r=r, kt=kt, wi=wi, k2=k2, obw=obw: e.matmul(
                            ps[obw][:, r * 128:r * 128 + 65], lhsT=PTw[k2][:, wi, r * 128:(r + 1) * 128],
                            rhs=Vsw[:, kt, 1, :], start=(wi == 0), stop=(wi == len(kts) - 1)),
                            reads=[b_ptw[k2][wi], b_v], writes=[psb[obw]])
                for bi, ob in ((1, obs), (2, obw)):
                    O = ps[ob][:, :].rearrange("p (r e) -> p r e", r=4)
                    mk.op("dve", lambda e, O=O: e.reciprocal(out=rinv[:], in_=O[:, :, 64]), reads=[psb[ob]], writes=[b_t])
                    mk.op("dve", lambda e, qt=qt, bi=bi, g=g: e.tensor_tensor(
                        out=coef[:], in0=rinv[:], in1=Gt[:, qt, g * 12 + bi:g * 12 + 12:3], op=ALU.mult),
                        reads=[b_t, b_G], writes=[b_t])
                    for r in range(4):
                        src1 = ocmp[:, qt, r, :] if bi == 1 else oacc[k2][:, r, :]
                        mk.op("dve", lambda e, O=O, r=r, src1=src1, k2=k2: e.scalar_tensor_tensor(
                            out=oacc[k2][:, r, :], in0=O[:, r, 0:64], scalar=coef[:, r:r + 1], in1=src1, op0=ALU.mult,
                            op1=ALU.add), reads=[psb[ob], b_t, b_ocmp[qt], b_oacc[k2]], writes=[b_oacc[k2]])
                bb = Buf()
                mk.dma("pool", X["OATT"][s, qt, :, g * 256:(g + 1) * 256], oacc[k2][:].rearrange("p r e -> p (r e)"),
                       reads=[b_oacc[k2]], writes=[bb])
                merge_ev(P.xb["OATT"][s], bb)

            for v in range(-3, 16):
                if 0 <= v + 3 < 16:
                    la1(v + 3)
                    la2a(v + 3)
                if 0 <= v + 2 < 16:
                    la2b(v + 2)
                gq = qk(v + 1) if 0 <= v + 1 < 16 else iter(())
                gp = pv(v) if 0 <= v < 16 else iter(())
                live = [gq, gp]
                while live:
                    for g_ in list(live):
                        try:
                            next(g_)
                        except StopIteration:
                            live.remove(g_)

        b_oat = bufs(2, "oat")
        b_n = Buf("normtmp")
        b_mtok = bufs(2, "mtok")
        b_mixst = bufs(2, "mixst")
        for qt in range(16):
            k2 = qt % 2
            mk.dma("sp", oat[k2][:], X["OATT"][s, qt], reads=[P.xb["OATT"][s]], writes=[b_oat[k2]])
            mk.op("act", lambda e, k2=k2: e.activation(out=junk[:], in_=oat[k2][:], func=AF.Square, accum_out=ssq[:]),
                  reads=[b_oat[k2]], writes=[b_n])
            mk.op("act", lambda e: e.activation(out=rstd[:], in_=ssq[:], func=AF.Sqrt, scale=1.0 / 1024.0,
                                                bias=cst1[:, 1:2]), reads=[b_n, b_c], writes=[b_n])
            mk.op("dve", lambda e: e.reciprocal(out=rstd[:], in_=rstd[:]), reads=[b_n], writes=[b_n])
            mk.op("dve", lambda e, k2=k2: e.scalar_tensor_tensor(out=mtok[k2][:], in0=oat[k2][:], scalar=rstd[:, 0:1],
                                                                 in1=gbc[:], op0=ALU.mult, op1=ALU.mult),
                  reads=[b_oat[k2], b_n, b_c], writes=[b_mtok[k2]])
            tb = 2 * k2
            for c in range(8):
                mk.op("pe", lambda e, c=c, k2=k2, tb=tb: e.matmul(
                    ps[tb + c // 4][:, (c % 4) * 128:(c % 4 + 1) * 128], lhsT=mtok[k2][:, c * 128:(c + 1) * 128],
                    rhs=ident[:], start=True, stop=True), reads=[b_mtok[k2], P.cb], writes=[psb[tb + c // 4]])
            for hh_ in range(2):
                evac(mk, hh_, mixst[k2][:, 4 * hh_:4 * hh_ + 4, :],
                     ps[tb + hh_][:, :].rearrange("p (c t) -> p c t", c=4), [psb[tb + hh_]], [b_mixst[k2]])
            bb = Buf()
            mk.dma("pool", X["MIXA"][s, :, qt * 128:(qt + 1) * 128].rearrange("(c p) t -> p c t", p=128), mixst[k2][:],
                   reads=[b_mixst[k2]], writes=[bb])
            merge_ev(P.xb["MIXA"][s], bb)
        mk.flush()


def phase4(P, nseq):
    nc, mk, I, W, X, C = P.nc, P.mk, P.I, P.W, P.X, P.C
    mk.barrier()
    TG = 512
    with ExitStack() as ts:
        def sb(name, shape, dt):
            return ts.enter_context(nc.sbuf_tensor(f"p4_{name}", shape, dt))

        hx = sb("hx", [128, 16, TG], F32)
        mixT = sb("mixT", [128, 16, TG], BF16)
        rstr = sb("rstr", [128, TG], F32)
        y2T = sb("y2T", [128, 16, TG], BF16)
        actT = sb("actT", [128, NFC, TG], BF16)
        NW16, NWD = 5, 3
        W16 = [sb(f"W16_{i}", [128, 16, 256], BF16) for i in range(NW16)]
        WD = [sb(f"WD_{i}", [128, NFC, 128], BF16) for i in range(NWD)]
        gpre = [sb(f"gpre{i}", [128, TG + 2], F32) for i in range(2)]
        cv = [sb(f"cv{i}", [128, TG], F32) for i in range(2)]
        ge = [sb(f"ge{i}", [128, TG], F32) for i in range(2)]
        GC = sb("GC", [128, NFC, 2], F32)
        rt = sb("rt", [128, TG], F32)
        rstd = sb("rstd", [128, TG], F32)
        fv = sb("fv", [128, NFC, 4], F32)
        gffn = sb("gffn", [128, 16], F32)
        gfin = sb("gfin", [128, 16], F32)
        epst = sb("eps", [128, 1], F32)
        ps = [ts.enter_context(nc.psum_tensor(f"p4_ps{i}", [128, 512], F32)) for i in range(8)]
        psb = bufs(8, "ps")
        ones = C["ones"]

        b_c = Buf("p4c")
        for (t_, src) in ((fv, I["ffn_vec"]), (gffn, I["g_ffn"]), (gfin, I["g_fin"])):
            b = Buf()
            mk.dma("pool", t_[:], src, writes=[b])
            merge_ev(b_c, b)
        b = Buf()
        mk.op("dve", lambda e: e.memset(epst[:], EPS), writes=[b])
        merge_ev(b_c, b)
        P_EPS[0] = epst[:]
        P_EPS[1] = b_c

        b_hx = bufs(16, "hx")
        b_mix = bufs(16, "mix")
        b_rstr = Buf("rstr")
        b_y2 = bufs(16, "y2")
        b_act = bufs(NFC, "act")
        b_w16 = bufs(NW16, "w16")
        b_wd = bufs(NWD, "wd")
        b_gpre = bufs(2, "gpre")
        b_cv = bufs(2, "cv")
        b_ge = bufs(2, "ge")
        b_gc = bufs(NFC, "gc")
        b_rt = Buf("rt")
        b_rstd = Buf("rstd")

        groups = [(s, j) for s in range(nseq) for j in range(S // TG)]
        items16 = []
        for gi_ in range(len(groups)):
            for dcp in range(8):
                items16.append((W["w_out"][dcp], P.wb["w_out"]))
            for fp in range(NFC // 2):
                items16.append((W["w_g"][fp], P.wb["w_g"]))
                items16.append((W["w_u"][fp], P.wb["w_u"]))
        st16 = [0]

        def need16(n):
            while st16[0] <= min(n + NW16 - 2, len(items16) - 1):
                i = st16[0]
                src, wb_ = items16[i]
                mk.dma("sp", W16[i % NW16][:].rearrange("p c n -> p (c n)"), src, reads=[wb_], writes=[b_w16[i % NW16]])
                st16[0] += 1

        itemsd = []
        for gi_ in range(len(groups)):
            for dc in range(16):
                itemsd.append(W["w_d"][dc])
        std = [0]

        def needd(n):
            while std[0] <= min(n + NWD - 1, len(itemsd) - 1):
                i = std[0]
                mk.dma("pool", WD[i % NWD][:].rearrange("p f n -> p (f n)"), itemsd[i], reads=[P.wb["w_d"]],
                       writes=[b_wd[i % NWD]])
                std[0] += 1

        pcnt = [0]

        def bank():
            b = pcnt[0] % 8
            pcnt[0] += 1
            return b

        def norm_stats(n_feat):
            pb = bank()
            for c in range(16):
                mk.op("act", lambda e, c=c: e.activation(out=y2T[:, c, :], in_=hx[:, c, :], func=AF.Square),
                      reads=[b_hx[c]], writes=[b_y2[c]])
                mk.op("pe", lambda e, c=c, pb=pb: e.matmul(ps[pb][:, :], lhsT=ones[:], rhs=y2T[:, c, :], start=(c == 0),
                                                           stop=(c == 15)), reads=[b_y2[c], P.cb], writes=[psb[pb]])
            rms_rstd(mk, ps[pb][:, :], psb[pb], rt[:], b_rt, rstd[:], b_rstd, float(n_feat))

        def load_mix(gi2):
            s2, j2 = groups[gi2]
            tsl2 = slice(j2 * TG, (j2 + 1) * TG)
            mk.dma("pool", mixT[:, 0:8, :], X["MIXA"][s2].rearrange("(c p) t -> p c t", p=128)[:, :, tsl2],
                   reads=[P.xb["MIXA"][s2]], writes=b_mix[0:8])
            mk.dma("pool", mixT[:, 8:16, :], X["MIXR"][s2].rearrange("(c p) t -> p c t", p=128)[:, :, tsl2],
                   reads=[P.xb["MIXR"][s2]], writes=b_mix[8:16])
            mk.dma("pool", rstr[:], X["RSTDR"][s2][:, tsl2], reads=[P.xb["RSTDR"][s2]], writes=[b_rstr])
            for c in range(8, 16):
                mk.op("dve", lambda e, c=c: e.tensor_tensor(out=mixT[:, c, :], in0=mixT[:, c, :], in1=rstr[:],
                                                            op=ALU.mult), reads=[b_mix[c], b_rstr], writes=[b_mix[c]])

        i16 = 0
        idn = 0
        import os
        P4S = int(os.environ.get("P4_STOP", "9"))
        CPE = os.environ.get("P4_CPE", "dve")
        if P4S < 9:
            groups = groups[:1]
        for gi_, (s, j) in enumerate(groups):
            t0 = j * TG
            tsl = slice(t0, t0 + TG)
            need16(i16)
            xsrc = I["xT"][s].rearrange("(c p) t -> p c t", p=128)
            for hf in range(8):
                cs = slice(hf * 2, hf * 2 + 2)
                mk.dma("pool", hx[:, cs, :], xsrc[:, cs, tsl], writes=b_hx[hf * 2:hf * 2 + 2])
            if gi_ == 0:
                load_mix(0)
            if j == 0:
                for fc in range(NFC):
                    mk.op("dve", lambda e, fc=fc: e.memset(GC[:, fc, :], 0.0), writes=[b_gc[fc]])
            if P4S <= 1:
                break
            for dcp in range(8):
                need16(i16)
                slot = i16 % NW16
                for dd in range(2):
                    dc = 2 * dcp + dd
                    pb = bank()
                    for c in range(16):
                        mk.op("pe", lambda e, c=c, pb=pb, slot=slot, dd=dd: e.matmul(
                            ps[pb][:, :], lhsT=W16[slot][:, c, dd * 128:(dd + 1) * 128], rhs=mixT[:, c, :],
                            start=(c == 0), stop=(c == 15)), reads=[b_w16[slot], b_mix[c]], writes=[psb[pb]])
                    mk.op("dve", lambda e, dc=dc, pb=pb: e.tensor_tensor(out=hx[:, dc, :], in0=ps[pb][:, :],
                                                                        in1=hx[:, dc, :], op=ALU.add),
                          reads=[psb[pb], b_hx[dc]], writes=[b_hx[dc]])
                i16 += 1
            if P4S <= 2:
                break
            norm_stats(D)
            for c in range(16):
                mk.op("dve", lambda e, c=c: e.scalar_tensor_tensor(out=y2T[:, c, :], in0=hx[:, c, :],
                                                                   scalar=gffn[:, c:c + 1], in1=rstd[:],
                                                                   op0=ALU.mult, op1=ALU.mult),
                      reads=[b_hx[c], b_rstd, b_c], writes=[b_y2[c]])
            if P4S <= 3:
                break
            for fp in range(NFC // 2):
                need16(i16)
                sg = i16 % NW16
                su = (i16 + 1) % NW16
                for ff in range(2):
                    fc = 2 * fp + ff
                    k = fc % 2
                    pg = bank()
                    pu = bank()
                    for c in range(16):
                        mk.op("pe", lambda e, c=c, pg=pg, sg=sg, ff=ff: e.matmul(
                            ps[pg][:, :], lhsT=W16[sg][:, c, ff * 128:(ff + 1) * 128], rhs=y2T[:, c, :],
                            start=(c == 0), stop=(c == 15)), reads=[b_w16[sg], b_y2[c]], writes=[psb[pg]])
                    for c in range(16):
                        mk.op("pe", lambda e, c=c, pu=pu, su=su, ff=ff: e.matmul(
                            ps[pu][:, :], lhsT=W16[su][:, c, ff * 128:(ff + 1) * 128], rhs=y2T[:, c, :],
                            start=(c == 0), stop=(c == 15)), reads=[b_w16[su], b_y2[c]], writes=[psb[pu]])
                    mk.op(CPE, lambda e, k=k, fc=fc: e.tensor_copy(out=gpre[k][:, 0:2], in_=GC[:, fc, :]),
                          reads=[b_gc[fc]], writes=[b_gpre[k]])
                    mk.op("act", act_copy(gpre[k][:, 2:TG + 2], ps[pg][:, :]), reads=[psb[pg]], writes=[b_gpre[k]])
                    mk.op("act", lambda e, k=k, fc=fc, pg=pg: e.activation(
                        out=cv[k][:], in_=ps[pg][:, :], func=AF.Identity, scale=fv[:, fc, 2:3], bias=fv[:, fc, 3:4]),
                        reads=[psb[pg], b_c], writes=[b_cv[k]])
                    mk.op(CPE, lambda e, k=k, fc=fc: e.tensor_copy(out=GC[:, fc, :], in_=gpre[k][:, TG:TG + 2]),
                          reads=[b_gpre[k]], writes=[b_gc[fc]])
                    for kk in (1, 0):
                        mk.op("dve", lambda e, k=k, fc=fc, kk=kk: e.scalar_tensor_tensor(
                            out=cv[k][:], in0=gpre[k][:, kk:kk + TG], scalar=fv[:, fc, kk:kk + 1], in1=cv[k][:],
                            op0=ALU.mult, op1=ALU.add), reads=[b_gpre[k], b_cv[k], b_c], writes=[b_cv[k]])
                    mk.op("act", lambda e, k=k: e.activation(out=ge[k][:], in_=cv[k][:], func=AF.Gelu_apprx_tanh),
                          reads=[b_cv[k]], writes=[b_ge[k]])
                    mk.op("dve", lambda e, k=k, fc=fc, pu=pu: e.tensor_tensor(out=actT[:, fc, :], in0=ps[pu][:, :],
                                                                              in1=ge[k][:], op=ALU.mult),
                          reads=[psb[pu], b_ge[k]], writes=[b_act[fc]])
                i16 += 2
            if P4S <= 4:
                break
            if gi_ + 1 < len(groups):
                load_mix(gi_ + 1)
            for dc in range(16):
                needd(idn)
                slot = idn % NWD
                pb = bank()
                for fc in range(NFC):
                    mk.op("pe", lambda e, fc=fc, pb=pb, slot=slot: e.matmul(
                        ps[pb][:, :], lhsT=WD[slot][:, fc, :], rhs=actT[:, fc, :], start=(fc == 0),
                        stop=(fc == NFC - 1)), reads=[b_wd[slot], b_act[fc]], writes=[psb[pb]])
                mk.op("dve", lambda e, dc=dc, pb=pb: e.tensor_tensor(out=hx[:, dc, :], in0=ps[pb][:, :],
                                                                    in1=hx[:, dc, :], op=ALU.add),
                      reads=[psb[pb], b_hx[dc]], writes=[b_hx[dc]])
                idn += 1
            if P4S <= 5:
                break
            norm_stats(D)
            for c in range(16):
                mk.op("dve", lambda e, c=c: e.scalar_tensor_tensor(out=hx[:, c, :], in0=hx[:, c, :],
                                                                   scalar=gfin[:, c:c + 1], in1=rstd[:],
                                                                   op0=ALU.mult, op1=ALU.mult),
                      reads=[b_hx[c], b_rstd, b_c], writes=[b_hx[c]])
            osrc = P.outT[s].rearrange("(c p) t -> p c t", p=128)
            for hf in range(8):
                cs = slice(hf * 2, hf * 2 + 2)
                mk.dma("pool", osrc[:, cs, tsl], hx[:, cs, :], reads=b_hx[hf * 2:hf * 2 + 2], writes=[Buf()])
        mk.flush()
```
